# Optimizing a Trainium2 kernel written in Bass

```python
import jax
import jax.numpy as jnp
from jax import lax
import numpy as np


D_MODEL = 1024
BATCH = 8
SEQ = 4096
DEPTH = 2

HEAD_DIM = 64
Q_BLK = 128
ROPE_THETA = 10000.0
LN_EPS = 1e-5
RMS_EPS = 1e-6

NSA_HEADS = 6
NSA_KV_HEADS = 2
NSA_GROUP = NSA_HEADS // NSA_KV_HEADS
NSA_WIDTH = NSA_HEADS * HEAD_DIM
CMP_LEN = 32
CMP_STRIDE = 16
SEL_BLOCK = 64
SEL_TOP = 16
WINDOW = 512
FORCE_BONUS = 1e4

DSA_HEADS = 6
DSA_WIDTH = DSA_HEADS * HEAD_DIM
DSA_LATENT = 128
IDX_HEADS = 4
IDX_DIM = 32
IDX_TOPK_MAX = 256

SGU_GROUPS = 4
SGU_CHUNK = 128
SGU_WIDTH = SGU_GROUPS * HEAD_DIM

MIX_WIDTH = NSA_WIDTH + DSA_WIDTH + SGU_WIDTH

SPLIT_SIZES = (NSA_WIDTH, 6 * NSA_KV_HEADS * HEAD_DIM, 3 * NSA_HEADS, DSA_WIDTH, DSA_LATENT, IDX_HEADS * IDX_DIM, IDX_DIM, IDX_HEADS, 2 * SGU_WIDTH)
IN_WIDTH = sum(SPLIT_SIZES)

D_FF = 2816
N_EXPERTS = 8
TOP_K = 2
EXPERT_FF = 3584
N_DENSE = (DEPTH + 1) // 2
N_MOE = DEPTH // 2

ALPHA = (2 * DEPTH) ** 0.25
BETA = (8 * DEPTH) ** -0.25
POS_OFFSET_MAX = 1024

kernel_name = 'hybrid_nsa_dsa_sgu_deepnorm_adaln_moe'


def standardize(x):
    xf = x.astype(jnp.float32)
    mu = jnp.mean(xf, -1, keepdims=True)
    var = jnp.mean(jnp.square(xf - mu), -1, keepdims=True)
    return ((xf - mu) * lax.rsqrt(var + LN_EPS)).astype(x.dtype)


def layer_norm(x, g, b):
    return standardize(x) * g + b


def rms_norm(x, g):
    xf = x.astype(jnp.float32)
    y = xf * lax.rsqrt(jnp.mean(jnp.square(xf), -1, keepdims=True) + RMS_EPS)
    return (y * g).astype(x.dtype)


def modulate(x, shift, scale):
    return standardize(x) * (1.0 + scale) + shift


def rope_tables(positions, dim):
    inv = ROPE_THETA ** (-jnp.arange(0, dim, 2, dtype=jnp.float32) / dim)
    ang = positions.astype(jnp.float32)[..., None] * inv
    return jnp.cos(ang), jnp.sin(ang)


def apply_rope(x, cos, sin):
    x1, x2 = jnp.split(x, 2, axis=-1)
    c = cos[:, :, None, :]
    s = sin[:, :, None, :]
    return jnp.concatenate([x1 * c - x2 * s, x2 * c + x1 * s], axis=-1).astype(x.dtype)


def masked_softmax(scores, mask):
    s = jnp.where(mask, scores.astype(jnp.float32), -jnp.inf)
    m = jnp.max(s, axis=-1, keepdims=True)
    m = jnp.where(jnp.isfinite(m), m, 0.0)
    p = jnp.exp(s - m)
    return p / jnp.maximum(jnp.sum(p, axis=-1, keepdims=True), 1e-30)


def nsa_mixer(q, kv, gate_logits, cos, sin, cmp_pos, cmp_w1, cmp_w2):
    B, S = q.shape[0], q.shape[1]
    G, R, dh = NSA_KV_HEADS, NSA_GROUP, HEAD_DIM
    n_cmp = (S - CMP_LEN) // CMP_STRIDE + 1
    n_blk = S // SEL_BLOCK
    n_sel = min(SEL_TOP, n_blk)
    n_q = S // Q_BLK
    scale = dh ** -0.5

    kv = kv.reshape(B, S, 6, G, dh)
    k_sel = apply_rope(kv[:, :, 2], cos, sin)
    v_sel = kv[:, :, 3]
    k_win = apply_rope(kv[:, :, 4], cos, sin)
    v_win = kv[:, :, 5]
    q_rot = apply_rope(q, cos, sin)
    gates = jax.nn.sigmoid(gate_logits.astype(jnp.float32)).astype(q.dtype)
    gates = gates.reshape(B, S, G, R, 3)

    win_idx = jnp.arange(n_cmp)[:, None] * CMP_STRIDE + jnp.arange(CMP_LEN)[None, :]

    def compress(x_tok, j):
        blocks = x_tok[:, win_idx] + cmp_pos[j][:, None, :]
        hid = jax.nn.gelu(jnp.einsum('bnlgd,lde->bnge', blocks, cmp_w1[j]))
        return jnp.einsum('bnge,ef->bngf', hid, cmp_w2[j])

    k_cmp = compress(kv[:, :, 0], 0)
    v_cmp = compress(kv[:, :, 1], 1)
    cmp_start = jnp.arange(n_cmp) * CMP_STRIDE
    cmp_end = cmp_start + CMP_LEN - 1
    blk_start = jnp.arange(n_blk) * SEL_BLOCK
    overlap = ((cmp_start[:, None] < blk_start[None, :] + SEL_BLOCK)
               & (cmp_start[:, None] + CMP_LEN > blk_start[None, :])).astype(jnp.float32)

    k_sel_blk = k_sel.reshape(B, n_blk, SEL_BLOCK, G, dh).transpose(0, 3, 1, 2, 4)
    v_sel_blk = v_sel.reshape(B, n_blk, SEL_BLOCK, G, dh).transpose(0, 3, 1, 2, 4)
    gather_blocks = jax.vmap(jax.vmap(lambda kb, ib: kb[ib]))

    pad = jnp.zeros((B, WINDOW, G, dh), k_win.dtype)
    k_win_pad = jnp.concatenate([pad, k_win], axis=1)
    v_win_pad = jnp.concatenate([pad, v_win], axis=1)
    blk_ids = jnp.arange(n_blk)

    def block(qb):
        t0 = qb * Q_BLK
        t = t0 + jnp.arange(Q_BLK)
        qc = lax.dynamic_slice_in_dim(q, t0, Q_BLK, 1).reshape(B, Q_BLK, G, R, dh)
        qr = lax.dynamic_slice_in_dim(q_rot, t0, Q_BLK, 1).reshape(B, Q_BLK, G, R, dh)

        s_c = jnp.einsum('bqgrd,bngd->bqgrn', qc, k_cmp) * scale
        p_c = masked_softmax(s_c, (cmp_end[None, :] <= t[:, None])[None, :, None, None, :])
        o_c = jnp.einsum('bqgrn,bngd->bqgrd', p_c.astype(v_cmp.dtype), v_cmp)

        imp = jnp.einsum('bqgrn,nj->bqgj', p_c, overlap)
        cur = t // SEL_BLOCK
        valid = blk_ids[None, :] <= cur[:, None]
        forced = ((blk_ids[None, :] == 0) | (blk_ids[None, :] == cur[:, None])
                  | (blk_ids[None, :] == cur[:, None] - 1)).astype(jnp.float32)
        score = jnp.where(valid[None, :, None, :], imp + FORCE_BONUS * forced[None, :, None, :], -jnp.inf)
        _, idx = lax.top_k(score, n_sel)
        idx_bg = idx.transpose(0, 2, 1, 3)
        k_g = gather_blocks(k_sel_blk, idx_bg)
        v_g = gather_blocks(v_sel_blk, idx_bg)
        key_pos = idx_bg[..., None] * SEL_BLOCK + jnp.arange(SEL_BLOCK)
        m_s = (key_pos <= t[None, None, :, None, None]).transpose(0, 2, 1, 3, 4)
        m_s = m_s.reshape(B, Q_BLK, G, 1, n_sel * SEL_BLOCK)
        s_s = jnp.einsum('bqgrd,bgqnsd->bqgrns', qr, k_g) * scale
        p_s = masked_softmax(s_s.reshape(B, Q_BLK, G, R, n_sel * SEL_BLOCK), m_s)
        p_s = p_s.reshape(B, Q_BLK, G, R, n_sel, SEL_BLOCK)
        o_s = jnp.einsum('bqgrns,bgqnsd->bqgrd', p_s.astype(v_g.dtype), v_g)

        k_w = lax.dynamic_slice_in_dim(k_win_pad, t0, WINDOW + Q_BLK, 1)
        v_w = lax.dynamic_slice_in_dim(v_win_pad, t0, WINDOW + Q_BLK, 1)
        s_pos = t0 - WINDOW + jnp.arange(WINDOW + Q_BLK)
        diff = t[:, None] - s_pos[None, :]
        m_w = (diff >= 0) & (diff < WINDOW) & (s_pos[None, :] >= 0)
        s_w = jnp.einsum('bqgrd,bsgd->bqgrs', qr, k_w) * scale
        p_w = masked_softmax(s_w, m_w[None, :, None, None, :])
        o_w = jnp.einsum('bqgrs,bsgd->bqgrd', p_w.astype(v_w.dtype), v_w)

        g = lax.dynamic_slice_in_dim(gates, t0, Q_BLK, 1)
        o = g[..., 0:1] * o_c + g[..., 1:2] * o_s + g[..., 2:3] * o_w
        return o.reshape(B, Q_BLK, NSA_WIDTH)

    out = lax.map(block, jnp.arange(n_q))
    return out.transpose(1, 0, 2, 3).reshape(B, S, NSA_WIDTH)


def dsa_mixer(q, c_kv, iq, ik, iw, cos, sin, cos_i, sin_i, kv_norm_g, w_uk, w_uv):
    B, S = q.shape[0], q.shape[1]
    k_top = min(IDX_TOPK_MAX, S // 4)
    n_q = S // Q_BLK
    scale = HEAD_DIM ** -0.5
    ckv = rms_norm(c_kv, kv_norm_g)
    k = apply_rope((ckv @ w_uk)[:, :, None, :], cos, sin)[:, :, 0]
    v = ckv @ w_uv
    q = apply_rope(q, cos, sin)
    iq = apply_rope(iq.reshape(B, S, IDX_HEADS, IDX_DIM), cos_i, sin_i)
    ik = apply_rope(ik[:, :, None, :], cos_i, sin_i)[:, :, 0]
    iw = iw * (IDX_HEADS ** -0.5)
    key_ids = jnp.arange(S)
    gather_tok = jax.vmap(lambda kb, ib: kb[ib])

    def block(qb):
        t0 = qb * Q_BLK
        t = t0 + jnp.arange(Q_BLK)
        iq_b = lax.dynamic_slice_in_dim(iq, t0, Q_BLK, 1)
        iw_b = lax.dynamic_slice_in_dim(iw, t0, Q_BLK, 1)
        logits = jnp.einsum('bqhe,bse->bqhs', iq_b, ik)
        i_score = jnp.einsum('bqh,bqhs->bqs', iw_b, jax.nn.relu(logits)).astype(jnp.float32)
        causal = key_ids[None, :] <= t[:, None]
        i_score = jnp.where(causal[None], i_score, -jnp.inf)
        _, idx = lax.top_k(i_score, k_top)
        k_g = gather_tok(k, idx)
        v_g = gather_tok(v, idx)
        q_b = lax.dynamic_slice_in_dim(q, t0, Q_BLK, 1)
        s = jnp.einsum('bqhd,bqkd->bqhk', q_b, k_g) * scale
        p = masked_softmax(s, (idx <= t[None, :, None])[:, :, None, :])
        o = jnp.einsum('bqhk,bqkd->bqhd', p.astype(v_g.dtype), v_g)
        return o.reshape(B, Q_BLK, DSA_WIDTH)

    out = lax.map(block, jnp.arange(n_q))
    return out.transpose(1, 0, 2, 3).reshape(B, S, DSA_WIDTH)


def sgu_mixer(z, norm_g, norm_b, w_s, b_s):
    B, S = z.shape[0], z.shape[1]
    z = jax.nn.gelu(z)
    u, v = jnp.split(z, 2, axis=-1)
    v = layer_norm(v.reshape(B, S, SGU_GROUPS, HEAD_DIM), norm_g, norm_b)
    v = v.reshape(B, S // SGU_CHUNK, SGU_CHUNK, SGU_GROUPS, HEAD_DIM)
    causal = jnp.tril(jnp.ones((SGU_CHUNK, SGU_CHUNK), w_s.dtype))
    s = jnp.einsum('gts,bnsgd->bntgd', w_s * causal, v) + jnp.swapaxes(b_s, 0, 1)[None, None, :, :, None]
    return u * s.reshape(B, S, SGU_WIDTH)


def swiglu(h, w_gate, w_up, w_down):
    return (jax.nn.silu(h @ w_gate) * (h @ w_up)) @ w_down


def moe_swiglu(h, w_router, b_router, w_gate, w_up, w_down):
    logits = (h @ w_router + b_router).astype(jnp.float32)
    top_val, top_idx = lax.top_k(logits, TOP_K)
    top_w = jax.nn.softmax(top_val, axis=-1)
    gates = jnp.sum(jax.nn.one_hot(top_idx, N_EXPERTS, dtype=jnp.float32) * top_w[..., None], axis=-2)
    gates = gates.astype(h.dtype)
    out = jnp.zeros_like(h)
    for e in range(N_EXPERTS):
        out = out + gates[..., e:e + 1] * swiglu(h, w_gate[e], w_up[e], w_down[e])
    return out


def setup_inputs(seed: int = 0) -> dict:
    key = jax.random.key(seed)
    ks = iter(jax.random.split(key, 40))
    D, dh = D_MODEL, HEAD_DIM

    def nrm(shape, scale):
        return jax.random.normal(next(ks), shape, jnp.float32) * scale

    x = nrm((BATCH, SEQ, D), 1.0)
    c = nrm((BATCH, D), 1.0)
    offs = jax.random.randint(next(ks), (BATCH, 1), 0, POS_OFFSET_MAX, dtype=jnp.int32)
    positions = (jnp.arange(SEQ, dtype=jnp.int32)[None, :] + offs).astype(jnp.int32)
    return {
        'x': x,
        'c': c,
        'positions': positions,
        'w_ada': nrm((DEPTH, D, 6 * D), D ** -0.5),
        'b_ada': nrm((DEPTH, 6 * D), 0.02),
        'w_in': nrm((DEPTH, D, IN_WIDTH), D ** -0.5),
        'nsa_cmp_pos': nrm((DEPTH, 2, CMP_LEN, dh), 0.5),
        'nsa_cmp_w1': nrm((DEPTH, 2, CMP_LEN, dh, dh), (CMP_LEN * dh) ** -0.5),
        'nsa_cmp_w2': nrm((DEPTH, 2, dh, dh), dh ** -0.5),
        'dsa_kv_norm': 1.0 + nrm((DEPTH, DSA_LATENT), 0.02),
        'dsa_w_uk': nrm((DEPTH, DSA_LATENT, dh), DSA_LATENT ** -0.5),
        'dsa_w_uv': nrm((DEPTH, DSA_LATENT, dh), DSA_LATENT ** -0.5),
        'sgu_norm_g': 1.0 + nrm((DEPTH, SGU_GROUPS, dh), 0.02),
        'sgu_norm_b': nrm((DEPTH, SGU_GROUPS, dh), 0.02),
        'sgu_w': nrm((DEPTH, SGU_GROUPS, SGU_CHUNK, SGU_CHUNK), SGU_CHUNK ** -0.5),
        'sgu_b': 1.0 + nrm((DEPTH, SGU_GROUPS, SGU_CHUNK), 0.02),
        'w_out': nrm((DEPTH, MIX_WIDTH, D), BETA * MIX_WIDTH ** -0.5),
        'ln1_g': 1.0 + nrm((DEPTH, D), 0.02),
        'ln1_b': nrm((DEPTH, D), 0.02),
        'ln2_g': 1.0 + nrm((DEPTH, D), 0.02),
        'ln2_b': nrm((DEPTH, D), 0.02),
        'ffn_w_gate': nrm((N_DENSE, D, D_FF), D ** -0.5),
        'ffn_w_up': nrm((N_DENSE, D, D_FF), D ** -0.5),
        'ffn_w_down': nrm((N_DENSE, D_FF, D), BETA * D_FF ** -0.5),
        'moe_w_router': nrm((N_MOE, D, N_EXPERTS), D ** -0.5),
        'moe_b_router': nrm((N_MOE, N_EXPERTS), 0.01),
        'moe_w_gate': nrm((N_MOE, N_EXPERTS, D, EXPERT_FF), D ** -0.5),
        'moe_w_up': nrm((N_MOE, N_EXPERTS, D, EXPERT_FF), D ** -0.5),
        'moe_w_down': nrm((N_MOE, N_EXPERTS, EXPERT_FF, D), BETA * EXPERT_FF ** -0.5),
    }


def reference(x, c, positions, w_ada, b_ada, w_in, nsa_cmp_pos, nsa_cmp_w1, nsa_cmp_w2,
              dsa_kv_norm, dsa_w_uk, dsa_w_uv, sgu_norm_g, sgu_norm_b, sgu_w, sgu_b,
              w_out, ln1_g, ln1_b, ln2_g, ln2_b, ffn_w_gate, ffn_w_up, ffn_w_down,
              moe_w_router, moe_b_router, moe_w_gate, moe_w_up, moe_w_down):
    B, S = x.shape[0], x.shape[1]
    cos_h, sin_h = rope_tables(positions, HEAD_DIM)
    cos_i, sin_i = rope_tables(positions, IDX_DIM)
    split_points = np.cumsum(SPLIT_SIZES)[:-1].tolist()
    c_act = jax.nn.silu(c)
    for layer in range(DEPTH):
        mod = (c_act @ w_ada[layer] + b_ada[layer])[:, None, :]
        shift1, scale1, gate1, shift2, scale2, gate2 = jnp.split(mod, 6, axis=-1)

        h = modulate(x, shift1, scale1)
        z = h @ w_in[layer]
        nsa_q, nsa_kv, nsa_g, dsa_q, dsa_ckv, idx_q, idx_k, idx_w, sgu_z = jnp.split(z, split_points, axis=-1)
        o_a = nsa_mixer(nsa_q.reshape(B, S, NSA_HEADS, HEAD_DIM), nsa_kv, nsa_g, cos_h, sin_h,
                        nsa_cmp_pos[layer], nsa_cmp_w1[layer], nsa_cmp_w2[layer])
        o_b = dsa_mixer(dsa_q.reshape(B, S, DSA_HEADS, HEAD_DIM), dsa_ckv, idx_q, idx_k, idx_w,
                        cos_h, sin_h, cos_i, sin_i, dsa_kv_norm[layer], dsa_w_uk[layer], dsa_w_uv[layer])
        o_c = sgu_mixer(sgu_z, sgu_norm_g[layer], sgu_norm_b[layer], sgu_w[layer], sgu_b[layer])
        mix = jnp.concatenate([o_a, o_b, o_c], axis=-1) @ w_out[layer]
        x = layer_norm(ALPHA * x + gate1 * mix, ln1_g[layer], ln1_b[layer])

        h = modulate(x, shift2, scale2)
        if layer % 2 == 0:
            j = layer // 2
            f = swiglu(h, ffn_w_gate[j], ffn_w_up[j], ffn_w_down[j])
        else:
            j = layer // 2
            f = moe_swiglu(h, moe_w_router[j], moe_b_router[j], moe_w_gate[j], moe_w_up[j], moe_w_down[j])
        x = layer_norm(ALPHA * x + gate2 * f, ln2_g[layer], ln2_b[layer])
    return x
```

```python
import math
import numpy as np
from contextlib import ExitStack
import concourse.bass as bass
import concourse.mybir as mybir
from concourse.bass_utils import run_bass_kernel_spmd

F32 = mybir.dt.float32
BF16 = mybir.dt.bfloat16
I32 = mybir.dt.int32
AF = mybir.ActivationFunctionType
ALU = mybir.AluOpType
AX = mybir.AxisListType

D = 1024
DEPTH = 2
ALPHA = (2 * DEPTH) ** 0.25
Q0, KV0, G0, DQ0, CKV0, IQ0, IK0, IW0, SGU0, INW = 0, 384, 1152, 1170, 1554, 1682, 1810, 1842, 1846, 2358
D_FF = 2816
E_FF = 3584
NEXP = 8
NEG = -30000.0
FILL = -1.0e30
NBIS = 16


class Buf:
    __slots__ = ("name", "w", "r", "excl")

    def __init__(self, name="", excl=False):
        self.name = name
        self.w = None
        self.r = {}
        self.excl = excl


def PBuf():
    return Buf("psum", True)


class Eng:
    def __init__(self, key, eng, is_pe=False):
        self.key, self.eng, self.is_pe = key, eng, is_pe
        self.sem = None
        self.count = 0
        self.seen = {}
        self.epoch = 0
        self.dma_sems, self.dma_vals, self.dma_rr = [], [], 0
        self.n_inst = 0
        self.n_wait = 0


class Sched:
    EPOCH = 30000

    def __init__(self, nc, stack, n_dma_sems=10):
        self.nc, self.stack = nc, stack
        self.pe = Eng("pe", nc.tensor, True)
        self.act = Eng("act", nc.scalar)
        self.dve = Eng("dve", nc.vector)
        self.pool = Eng("pool", nc.gpsimd)
        self.sp = Eng("sp", nc.sync)
        self.engs = [self.pe, self.act, self.dve, self.pool, self.sp]
        self.nsem = 0
        for e in self.engs:
            e.sem = self._newsem(e.key + "_p0")
        for e in (self.sp, self.pool):
            for i in range(n_dma_sems):
                e.dma_sems.append(self._newsem(f"{e.key}_d{i}"))
                e.dma_vals.append(0)
        self.out_tokens = []

    def _newsem(self, name):
        self.nsem += 1
        return self.stack.enter_context(self.nc.semaphore(name))

    def _wait(self, E, tok):
        sem, val, src = tok
        if src == E.key and E.is_pe:
            return
        k = id(sem)
        if E.seen.get(k, 0) >= val:
            return
        E.eng.wait_ge(sem, val)
        E.n_wait += 1
        E.seen[k] = val

    def _deps(self, E, reads, writes):
        for b in reads:
            if b.w is not None:
                self._wait(E, b.w)
            if b.excl:
                for k, t in b.r.items():
                    if k != E.key:
                        self._wait(E, t)
        for b in writes:
            if b.w is not None:
                self._wait(E, b.w)
            for t in b.r.values():
                self._wait(E, t)

    def _mark(self, tok, reads, writes):
        for b in reads:
            b.r[tok[2]] = tok
        for b in writes:
            b.w = tok
            b.r = {}

    def op(self, E, fn, reads=(), writes=()):
        self._deps(E, reads, writes)
        if E.count >= self.EPOCH:
            E.epoch += 1
            E.sem = self._newsem(f"{E.key}_p{E.epoch}")
            E.count = 0
        inst = fn(E.eng)
        E.count += 1
        E.n_inst += 1
        inst.then_inc(E.sem, 1)
        tok = (E.sem, E.count, E.key)
        self._mark(tok, reads, writes)
        return tok

    def dma(self, Q, out, in_, reads=(), writes=(), is_output=False, **kw):
        self._deps(Q, reads, writes)
        i = Q.dma_rr
        Q.dma_rr = (Q.dma_rr + 1) % len(Q.dma_sems)
        sem = Q.dma_sems[i]
        if Q.dma_vals[i] > 0:
            self._wait(Q, (sem, Q.dma_vals[i], "dma"))
        inst = Q.eng.dma_start(out=out, in_=in_, **kw)
        Q.dma_vals[i] += 16
        inst.then_inc(sem, 16)
        Q.n_inst += 1
        tok = (sem, Q.dma_vals[i], "dma%s%d" % (Q.key, i))
        self._mark(tok, reads, writes)
        if is_output:
            self.out_tokens.append(tok)
        return tok

    def all_tokens(self):
        toks = []
        for Q in (self.sp, self.pool):
            for i, sem in enumerate(Q.dma_sems):
                if Q.dma_vals[i] > 0:
                    toks.append((sem, Q.dma_vals[i], "dma"))
        for e in self.engs:
            if e.count > 0:
                toks.append((e.sem, e.count, e.key + "_bar"))
        return toks

    def barrier(self):
        toks = self.all_tokens()
        for E in self.engs:
            for t in toks:
                if t[2] == E.key + "_bar":
                    continue
                self._wait(E, t)

    def finish(self):
        for t in self.all_tokens():
            self._wait(self.sp, t)


class Prog:
    def __init__(self, S_len, n_layers=DEPTH, stop_after=None, debug=False):
        self.debug = debug
        self.KTOP = min(256, S_len // 4)
        self.SL = S_len
        self.NT = S_len // 128
        self.NG = S_len // 512
        self.n_layers = n_layers
        self.stop_after = stop_after
        self.nc = bass.Bass("TRN2", target_bir_lowering=False)
        self.build()

    def sb(self, st, name, shape, dt):
        self._uid = getattr(self, "_uid", 0) + 1
        return st.enter_context(self.nc.sbuf_tensor(f"{name}_{self._uid}", shape, dt))

    def ps(self, st, name, shape, dt):
        self._uid = getattr(self, "_uid", 0) + 1
        return st.enter_context(self.nc.psum_tensor(f"{name}_{self._uid}", shape, dt))

    def MM(self, out, lhsT, rhs, start, R, W, stop=True):
        self.S.op(self.S.pe, lambda e: e.matmul(out, lhsT=lhsT, rhs=rhs, start=start, stop=stop,
                                                skip_group_check=True), R, W)

    def TR(self, out, in_, ident, R, W):
        self.S.op(self.S.pe, lambda e: e.transpose(out=out, in_=in_, identity=ident), R, W)

    def A(self, out, in_, func, R, W, **kw):
        self.S.op(self.S.act, lambda e: e.activation(out=out, in_=in_, func=func, **kw), R, W)

    def V(self, name, R, W, **kw):
        self.S.op(self.S.dve, lambda e: getattr(e, name)(**kw), R, W)

    def G(self, name, R, W, **kw):
        if name == "affine_select" and isinstance(kw.get("fill"), (int, float)) and kw["fill"] != 0.0:
            regs = self.__dict__.setdefault("_fillregs", {})
            v = float(kw["fill"])
            if v not in regs:
                regs[v] = self.nc.gpsimd.to_reg(v)
            kw["fill"] = regs[v]
        self.S.op(self.S.pool, lambda e: getattr(e, name)(**kw), R, W)

    def LD(self, out, in_, W, R=(), q=None, **kw):
        self.S.dma(q or self.S.sp, out, in_, reads=R, writes=W, **kw)

    def dram_in(self, name, shape, dt=F32):
        return self.nc.dram_tensor(name, list(shape), dt, kind="ExternalInput").ap()

    def build(self):
        nc = self.nc
        SL, NT, NG = self.SL, self.NT, self.NG
        I = {}
        I["x"] = self.dram_in("x", [SL, D])
        I["c"] = self.dram_in("c", [1, D])
        I["pos"] = self.dram_in("pos", [1, SL], I32)
        I["w_ada"] = self.dram_in("w_ada", [DEPTH, D, 6 * D])
        I["b_ada"] = self.dram_in("b_ada", [DEPTH, 6 * D])
        I["w_in"] = self.dram_in("w_in", [DEPTH, D, INW])
        I["cmp_pos"] = self.dram_in("nsa_cmp_pos", [DEPTH, 2, 32, 64])
        I["cmp_w1"] = self.dram_in("nsa_cmp_w1", [DEPTH, 2, 32, 64, 64])
        I["cmp_w2"] = self.dram_in("nsa_cmp_w2", [DEPTH, 2, 64, 64])
        I["kvn"] = self.dram_in("dsa_kv_norm", [DEPTH, 128])
        I["w_uk"] = self.dram_in("dsa_w_uk", [DEPTH, 128, 64])
        I["w_uv"] = self.dram_in("dsa_w_uv", [DEPTH, 128, 64])
        I["sgu_g"] = self.dram_in("sgu_norm_g", [DEPTH, 256])
        I["sgu_b"] = self.dram_in("sgu_norm_b", [DEPTH, 256])
        I["sgu_w"] = self.dram_in("sgu_w", [DEPTH, 4, 128, 128])
        I["sgu_bs"] = self.dram_in("sgu_b", [DEPTH, 4, 128])
        I["w_out"] = self.dram_in("w_out", [DEPTH, D, D])
        for n in ("ln1_g", "ln1_b", "ln2_g", "ln2_b"):
            I[n] = self.dram_in(n, [DEPTH, D])
        I["ffn_wg"] = self.dram_in("ffn_w_gate", [1, D, D_FF])
        I["ffn_wu"] = self.dram_in("ffn_w_up", [1, D, D_FF])
        I["ffn_wd"] = self.dram_in("ffn_w_down", [1, D_FF, D])
        I["moe_wr"] = self.dram_in("moe_w_router", [1, D, NEXP])
        I["moe_br"] = self.dram_in("moe_b_router", [1, NEXP])
        I["moe_wg"] = self.dram_in("moe_w_gate", [1, NEXP, D, E_FF])
        I["moe_wu"] = self.dram_in("moe_w_up", [1, NEXP, D, E_FF])
        I["moe_wd"] = self.dram_in("moe_w_down", [1, NEXP, E_FF, D])
        self.I = I
        self.y = nc.dram_tensor("y", [SL, D], F32, kind="ExternalOutput").ap()
        if self.debug:
            self.dbg = nc.dram_tensor("dbg", [SL, D], F32, kind="ExternalOutput").ap()
        self.rope_d = nc.dram_tensor("rope_d", [4, 128, SL], F32, kind="Internal").ap()
        self.xa = nc.dram_tensor("xa", [SL, D], F32, kind="Internal").ap()
        self.xb = nc.dram_tensor("xb", [SL, D], F32, kind="Internal").ap()
        self.b_rope = Buf("rope_d")
        self.b_xa = [Buf() for _ in range(NT)]
        self.b_xb = [Buf() for _ in range(NT)]
        self.b_y = [Buf() for _ in range(NT)]

        with ExitStack() as st0:
            self.S = Sched(nc, st0)
            self.setup_consts(st0)
            self.setup_mod(st0)
            self.setup_rope()
            xin, bxin = I["x"], [Buf() for _ in range(NT)]
            for l in range(self.n_layers):
                last = (l == self.n_layers - 1)
                self.S.barrier()
                with ExitStack() as st:
                    self.mixer(st, l, xin, bxin, self.xa, self.b_xa)
                if self.stop_after == ("mix", l):
                    self.copy_out(self.xa, self.b_xa)
                    break
                self.S.barrier()
                dst, bdst = (self.y, self.b_y) if last else (self.xb, self.b_xb)
                with ExitStack() as st:
                    if l % 2 == 0:
                        self.ffn_dense(st, l, self.xa, self.b_xa, dst, bdst, is_out=last)
                    else:
                        self.ffn_moe(st, l, self.xa, self.b_xa, dst, bdst, is_out=last)
                if (not last) and self.stop_after == ("ffn", l):
                    self.copy_out(self.xb, self.b_xb)
                    break
                xin, bxin = self.xb, self.b_xb
            self.S.finish()
            self.stats = {e.key: (e.n_inst, e.n_wait) for e in self.S.engs}

    def copy_out(self, src, bsrc):
        self.S.barrier()
        for t in range(self.NT):
            self.S.dma(self.S.sp, self.y[t * 128:(t + 1) * 128, :], src[t * 128:(t + 1) * 128, :],
                       reads=[bsrc[t]], writes=[self.b_y[t]], is_output=True)

    def setup_consts(self, st):
        C = self.C = {}
        B = self.B = {}

        def mk(name, shape, dt):
            C[name] = self.sb(st, name, shape, dt)
            B[name] = Buf(name)
            return C[name], B[name]

        idf, b = mk("ident_f", [128, 128], F32)
        self.G("memset", [], [b], ap=idf[:], constant=0.0)
        self.G("affine_select", [b], [b], out=idf[:], in_=idf[:], pattern=[[-1, 128]],
               compare_op=ALU.not_equal, fill=1.0, base=0, channel_multiplier=1)
        idb, b2 = mk("ident_b", [128, 128], BF16)
        self.V("tensor_copy", [b], [b2], out=idb[:], in_=idf[:])
        ones, b3 = mk("ones", [128, 128], F32)
        self.G("memset", [], [b3], ap=ones[:], constant=1.0)
        onesb, b3b = mk("onesb", [128, 128], BF16)
        self.G("memset", [], [b3b], ap=onesb[:], constant=1.0)
        zf, bz = mk("ztmp", [128, 384], F32)
        ntc, b4 = mk("negtri_c", [128, 384], BF16)
        self.G("memset", [], [bz], ap=zf[:], constant=0.0)
        self.G("affine_select", [bz], [bz], out=zf[:], in_=zf[:], pattern=[[0, 3], [1, 128]],
               compare_op=ALU.is_ge, fill=NEG, base=0, channel_multiplier=-1)
        self.V("tensor_copy", [bz], [b4], out=ntc[:], in_=zf[:])
        ntw, b5 = mk("negtri_w", [128, 384], BF16)
        self.G("memset", [b4], [bz], ap=zf[:], constant=0.0)
        self.G("affine_select", [bz], [bz], out=zf[:], in_=zf[:], pattern=[[0, 3], [-1, 128]],
               compare_op=ALU.is_ge, fill=NEG, base=-1, channel_multiplier=1)
        self.V("tensor_copy", [bz], [b5], out=ntw[:], in_=zf[:])
        rel, b6 = mk("rel", [128, 128], F32)
        self.G("memset", [], [b6], ap=rel[:], constant=0.0)
        for half in range(2):
            sl = rel[half * 64:(half + 1) * 64, :]
            self.G("memset", [b6], [b6], ap=rel[half * 64:(half + 1) * 64, 62 + half:64 + half], constant=1.0e4)
            if 64 + half < 128:
                self.G("memset", [b6], [b6], ap=rel[half * 64:(half + 1) * 64, 64 + half:128], constant=FILL)
        col0, b7 = mk("col0", [128, 64], F32)
        self.G("memset", [], [b7], ap=col0[:], constant=0.0)
        self.G("memset", [b7], [b7], ap=col0[:, 0:1], constant=1.0e4)
        ovf, b8 = mk("ovf", [128, 2, 64], F32)
        ov, b9 = mk("overlap", [128, 2, 64], BF16)
        self.G("memset", [], [b8], ap=ovf[:], constant=1.0)
        for nt in range(2):
            self.G("affine_select", [b8], [b8], out=ovf[:, nt, :], in_=ovf[:, nt, :], pattern=[[-4, 64]],
                   compare_op=ALU.is_ge, fill=0.0, base=128 * nt + 1, channel_multiplier=1)
            self.G("affine_select", [b8], [b8], out=ovf[:, nt, :], in_=ovf[:, nt, :], pattern=[[4, 64]],
                   compare_op=ALU.is_ge, fill=0.0, base=3 - 128 * nt, channel_multiplier=-1)
        self.V("tensor_copy", [b8], [b9], out=ov[:], in_=ovf[:])
        for nm, dh in (("rot_h", 64), ("rot_i", 32)):
            rm, brm = mk(nm, [128, 128], F32)
            hh = dh // 2
            self.G("memset", [], [brm], ap=rm[:], constant=0.0)
            for c0 in range(0, 128, dh):
                self.G("affine_select", [brm], [brm], out=rm[:, c0:c0 + hh], in_=rm[:, c0:c0 + hh], pattern=[[-1, hh]],
                       compare_op=ALU.not_equal, fill=-1.0, base=-(c0 + hh), channel_multiplier=1)
                self.G("affine_select", [brm], [brm], out=rm[:, c0 + hh:c0 + dh], in_=rm[:, c0 + hh:c0 + dh], pattern=[[-1, hh]],
                       compare_op=ALU.not_equal, fill=1.0, base=-c0, channel_multiplier=1)
        p2, b10 = mk("pow2", [128, NBIS], F32)
        for k in range(NBIS):
            self.G("memset", [b10], [b10], ap=p2[:, k:k + 1], constant=2.0 ** -(k + 1))

    def setup_mod(self, st):
        nc, S, C, B, I = self.nc, self.S, self.C, self.B, self.I
        modcol = self.sb(st, "modcol", [128, DEPTH, 4, 8], F32)
        self.gate_d = nc.dram_tensor("gate_d", [DEPTH * 2, D], F32, kind="Internal").ap()
        self.b_gate = Buf("gate_d")
        self.modcol = modcol
        self.b_mod = Buf("mod")
        with ExitStack() as s2:
            crow = self.sb(s2, "crow", [1, D], F32); b_crow = Buf()
            ccol = self.sb(s2, "ccol", [128, 8], F32); b_ccol = Buf()
            modrow = self.sb(s2, "modrow", [1, 6 * D], F32); b_modrow = Buf()
            brow = self.sb(s2, "brow", [1, 6 * D], F32); b_brow = Buf()
            wa = [self.sb(s2, f"wa{i}", [128, 8, 512], F32) for i in range(2)]
            b_wa = [Buf(), Buf()]
            pc = self.ps(s2, "pc", [128, 512], F32); b_pc = PBuf()
            pr = [self.ps(s2, f"pr{i}", [128, 512], F32) for i in range(2)]
            b_pr = [PBuf(), PBuf()]
            self.LD(crow[:], I["c"], [b_crow])
            for k in range(8):
                self.MM(pc[:, k:k + 1], crow[0:1, k * 128:(k + 1) * 128], C["ones"][0:1, 0:1], True,
                        [b_crow, B["ones"]], [b_pc])
            self.A(ccol[:], pc[:, 0:8], AF.Silu, [b_pc], [b_ccol])
            cnt = 0
            for l in range(self.n_layers):
                self.LD(brow[:], I["b_ada"][l:l + 1, :], [b_brow])
                for j in range(12):
                    w = wa[cnt % 2]; bw = b_wa[cnt % 2]; p = pr[cnt % 2]; bp = b_pr[cnt % 2]
                    cnt += 1
                    self.LD(w[:], I["w_ada"][l, :, j * 512:(j + 1) * 512].rearrange("(k p) n -> p k n", p=128), [bw])
                    for k in range(8):
                        self.MM(p[0:1, :], ccol[:, k:k + 1], w[:, k, :], k == 0, [b_ccol, bw], [bp])
                    self.V("tensor_tensor", [bp, b_brow], [b_modrow], out=modrow[0:1, j * 512:(j + 1) * 512],
                           in0=p[0:1, :], in1=brow[0:1, j * 512:(j + 1) * 512], op=ALU.add)
                for vi, seg in enumerate((0, 1, 3, 4)):
                    for k in range(8):
                        self.MM(pc[:, 16 + vi * 8 + k:16 + vi * 8 + k + 1],
                                modrow[0:1, seg * D + k * 128:seg * D + (k + 1) * 128], C["ones"][0:1, 0:1], True,
                                [b_modrow, B["ones"]], [b_pc])
                for vi in range(4):
                    if vi % 2 == 0:
                        self.V("tensor_copy", [b_pc], [self.b_mod], out=modcol[:, l, vi, :], in_=pc[:, 16 + vi * 8:24 + vi * 8])
                    else:
                        self.V("tensor_scalar", [b_pc], [self.b_mod], out=modcol[:, l, vi, :], in0=pc[:, 16 + vi * 8:24 + vi * 8],
                               scalar1=1.0, scalar2=None, op0=ALU.add)
                self.LD(self.gate_d[2 * l:2 * l + 1, :], modrow[0:1, 2 * D:3 * D], [self.b_gate], R=[b_modrow])
                self.LD(self.gate_d[2 * l + 1:2 * l + 2, :], modrow[0:1, 5 * D:6 * D], [self.b_gate], R=[b_modrow])
            S.barrier()

    def setup_rope(self):
        S, I = self.S, self.I
        with ExitStack() as st:
            pidx = self.sb(st, "pidx", [128, 1], I32); b_p = Buf()
            pm = self.sb(st, "pm", [128, 2], I32); b_pm = Buf()
            pmf = self.sb(st, "pmf", [128, 2], F32); b_pmf = Buf()
            inv = self.sb(st, "inv", [128, 2], F32); b_inv = Buf()
            self.G("iota", [], [b_p], out=pidx[:], pattern=[[0, 1]], base=0, channel_multiplier=1)
            self.V("tensor_single_scalar", [b_p], [b_pm], out=pm[:, 0:1], in_=pidx[:], scalar=31, op=ALU.bitwise_and)
            self.V("tensor_single_scalar", [b_p], [b_pm], out=pm[:, 1:2], in_=pidx[:], scalar=15, op=ALU.bitwise_and)
            self.V("tensor_copy", [b_pm], [b_pmf], out=pmf[:], in_=pm[:])
            lnth = math.log(10000.0)
            self.A(inv[:, 0:1], pmf[:, 0:1], AF.Exp, [b_pmf], [b_inv], scale=-lnth / 32.0)
            self.A(inv[:, 1:2], pmf[:, 1:2], AF.Exp, [b_pmf], [b_inv], scale=-lnth / 16.0)
            posi = self.sb(st, "posi", [128, 512], I32); b_posi = Buf()
            posf = self.sb(st, "posf", [128, 512], F32); b_posf = Buf()
            u = self.sb(st, "ru", [128, 512], F32); b_u = Buf()
            ki = self.sb(st, "rki", [128, 512], I32); b_ki = Buf()
            kf = self.sb(st, "rkf", [128, 512], F32); b_kf = Buf()
            t1 = self.sb(st, "rt1", [128, 512], F32); b_t1 = Buf()
            tab = [self.sb(st, f"rtab{i}", [128, 512], F32) for i in range(2)]
            b_tab = [Buf(), Buf()]
            n = 0
            for c in range(self.NG):
                self.LD(posi[:], I["pos"][0:1, c * 512:(c + 1) * 512].to_broadcast([128, 512]), [b_posi])
                self.V("tensor_copy", [b_posi], [b_posf], out=posf[:], in_=posi[:])
                for ti in range(4):
                    fi = ti // 2
                    off = 0.25 if ti % 2 == 0 else 0.0
                    self.V("tensor_scalar", [b_posf, b_inv], [b_u], out=u[:], in0=posf[:], scalar1=inv[:, fi:fi + 1],
                           scalar2=1.0 / (2 * math.pi), op0=ALU.mult, op1=ALU.mult)
                    if off:
                        self.V("tensor_scalar", [b_u], [b_u], out=u[:], in0=u[:], scalar1=off, scalar2=None, op0=ALU.add)
                    self.V("tensor_copy", [b_u], [b_ki], out=ki[:], in_=u[:])
                    self.V("tensor_copy", [b_ki], [b_kf], out=kf[:], in_=ki[:])
                    self.V("tensor_tensor", [b_u, b_kf], [b_u], out=u[:], in0=u[:], in1=kf[:], op=ALU.subtract)
                    self.V("tensor_single_scalar", [b_u], [b_t1], out=t1[:], in_=u[:], scalar=0.5, op=ALU.is_gt)
                    self.V("tensor_tensor", [b_u, b_t1], [b_u], out=u[:], in0=u[:], in1=t1[:], op=ALU.subtract)
                    self.V("tensor_single_scalar", [b_u], [b_t1], out=t1[:], in_=u[:], scalar=-0.5, op=ALU.is_lt)
                    self.V("tensor_tensor", [b_u, b_t1], [b_u], out=u[:], in0=u[:], in1=t1[:], op=ALU.add)
                    tb = tab[n % 2]; bt = b_tab[n % 2]; n += 1
                    self.A(tb[:], u[:], AF.Sin, [b_u], [bt], scale=6.28318)
                    self.LD(self.rope_d[ti, :, c * 512:(c + 1) * 512], tb[:], [self.b_rope], R=[bt])
            S.barrier()

    def ln_stats(self, xt, b_x, mv, b_mv, rstd, stats, b_st, eps=1e-5):
        for c2 in range(2):
            self.V("bn_stats", [b_x], [b_st], out=stats[:, c2, :], in_=xt[:, c2 * 512:(c2 + 1) * 512])
        self.V("bn_aggr", [b_st], [b_mv], out=mv[:, 0:2], in_=stats[:].rearrange("p a b -> p (a b)"))
        self.V("tensor_scalar", [b_mv], [b_mv], out=rstd, in0=mv[:, 1:2], scalar1=eps, scalar2=None, op0=ALU.add)
        self.A(rstd, rstd, AF.Sqrt, [b_mv], [b_mv])
        self.V("reciprocal", [b_mv], [b_mv], out=rstd, in_=rstd)

    def mixer(self, st, l, xin, bxin, xout, bxout):
        nc, S, C, B, I = self.nc, self.S, self.C, self.B, self.I
        SL, NT, NG = self.SL, self.NT, self.NG
        sb = lambda n, s, d: self.sb(st, n, s, d)
        MM, TR, A, V, G, LD = self.MM, self.TR, self.A, self.V, self.G, self.LD
        PQ = S.pool
        SA = max(SL, 4096)

        pA = self.ps(st, "pA", [128, 512], F32); b_pA = PBuf()
        pB = self.ps(st, "pB", [128, 512], F32); b_pB = PBuf()
        pS = [self.ps(st, f"pS{i}", [128, 512], F32) for i in range(2)]; b_pS = [PBuf(), PBuf()]
        pM = self.ps(st, "pM", [128, 1024], BF16); _bpm = PBuf(); b_pM = [_bpm, _bpm]
        pO = [self.ps(st, f"pO{i}", [128, 512], F32) for i in range(3)]; b_pO = [PBuf(), PBuf(), PBuf()]

        acc = sb("acc", [128, SA], F32); b_acc = Buf()
        dmask = sb("dmask", [128, SA], BF16); b_dm = Buf()
        win = sb("win", [128, 8, INW], BF16); b_win = Buf()
        for (a, b) in ((384, 1179), (1179, INW)):
            LD(win[:, :, a:b], I["w_in"][l, :, a:b].rearrange("(k p) n -> p k n", p=128), [b_win], q=PQ)
        for k in range(8):
            for r in range(3):
                LD(win[:, k, r * 128:(r + 1) * 128].rearrange("p (g d) -> p g d", g=2),
                   I["w_in"][l, k * 128:(k + 1) * 128, 0:384].rearrange("p (g r d) -> p g r d", g=2, r=3)[:, :, r, :], [b_win], q=PQ)
        ik4 = sb("ik4", [128, 8, 128], BF16); b_ik4 = Buf()
        for k in range(8):
            for r4 in range(4):
                (V if r4 % 2 == 0 else G)("tensor_copy", [b_win], [b_ik4], out=ik4[:, k, r4 * 32:(r4 + 1) * 32], in_=win[:, k, IK0:IK0 + 32])
        wout = sb("wout", [128, 8, D], BF16); b_wout = Buf()
        LD(wout[:], I["w_out"][l].rearrange("(k p) n -> p k n", p=128), [b_wout], q=PQ)
        lng = sb("lng", [128, D], F32); lnb = sb("lnb", [128, D], F32); b_ln = Buf()
        LD(lng[:], I["ln1_g"][l:l + 1, :].to_broadcast([128, D]), [b_ln])
        LD(lnb[:], I["ln1_b"][l:l + 1, :].to_broadcast([128, D]), [b_ln])
        w1 = sb("cw1", [128, 2, 32, 64], BF16); w2 = sb("cw2", [128, 2, 64], BF16); b_cw = Buf()
        for g in range(2):
            LD(w1[64 * g:64 * g + 64], I["cmp_w1"][l].rearrange("j l d e -> d j l e"), [b_cw], q=PQ)
            LD(w2[64 * g:64 * g + 64], I["cmp_w2"][l].rearrange("j e f -> e j f"), [b_cw], q=PQ)
        cbias = sb("cbias", [128, 2], F32); b_cb = Buf()
        wuk = sb("wuk", [128, 128], BF16); wuv = sb("wuv", [128, 64], BF16); b_wu = Buf()
        for hf in range(2):
            LD(wuk[:, hf * 64:(hf + 1) * 64], I["w_uk"][l], [b_wu], q=PQ)
        LD(wuv[:], I["w_uv"][l], [b_wu], q=PQ)
        kvg = sb("kvg", [128, 128], F32); b_kvg = Buf()
        LD(kvg[:], I["kvn"][l:l + 1, :].to_broadcast([128, 128]), [b_kvg])
        sgg = sb("sgg", [128, 256], F32); sgb = sb("sgb", [128, 256], F32); b_sg = Buf()
        LD(sgg[:], I["sgu_g"][l:l + 1, :].to_broadcast([128, 256]), [b_sg])
        LD(sgb[:], I["sgu_b"][l:l + 1, :].to_broadcast([128, 256]), [b_sg])
        wsT = sb("wsT", [128, 4, 128], BF16); b_ws = Buf()
        bs = sb("sgbs", [128, 4], F32); b_bs = Buf()
        with ExitStack() as s2:
            gbc = self.sb(s2, "gbc", [128, D], F32); b_gbc = Buf()
            LD(gbc[:], self.gate_d[2 * l:2 * l + 1, :].to_broadcast([128, D]), [b_gbc], R=[self.b_gate])
            for k in range(8):
                V("tensor_tensor", [b_wout, b_gbc], [b_wout], out=wout[:, k, :], in0=wout[:, k, :], in1=gbc[:], op=ALU.mult)
            w1n = self.sb(s2, "w1n", [32, 2, 64, 64], F32); posn = self.sb(s2, "posn", [32, 2, 64], F32); b_n = Buf()
            LD(w1n[:], I["cmp_w1"][l].rearrange("j l d e -> l j d e"), [b_n])
            LD(posn[:], I["cmp_pos"][l].rearrange("j l d -> l j d"), [b_n])
            for j in range(2):
                for g in range(2):
                    for d in range(64):
                        MM(pA[64 * g:64 * g + 64, j:j + 1], w1n[:, j, d, :], posn[:, j, d:d + 1], d == 0, [b_n], [b_pA])
            V("tensor_copy", [b_pA], [b_cb], out=cbias[:], in_=pA[:, 0:2])
            wsn = self.sb(s2, "wsn", [128, 4, 128], F32); b_wsn = Buf()
            wsf = self.sb(s2, "wsf", [128, 4, 128], F32); b_wsf = Buf()
            bsn = self.sb(s2, "bsn", [4, 128], F32); b_bsn = Buf()
            LD(wsn[:], I["sgu_w"][l].rearrange("g t s -> t g s"), [b_wsn])
            LD(bsn[:], I["sgu_bs"][l], [b_bsn])
            for g in range(4):
                TR(pB[:, g * 128:(g + 1) * 128], wsn[:, g, :], C["ident_f"][:], [b_wsn, B["ident_f"]], [b_pB])
            V("tensor_copy", [b_pB], [b_wsf], out=wsf[:].rearrange("p g t -> p (g t)"), in_=pB[:, :])
            G("affine_select", [b_wsf], [b_wsf], out=wsf[:], in_=wsf[:], pattern=[[0, 4], [1, 128]],
              compare_op=ALU.is_ge, fill=0.0, base=0, channel_multiplier=-1)
            V("tensor_copy", [b_wsf], [b_ws], out=wsT[:], in_=wsf[:])
            TR(pS[0][:, 0:4], bsn[:, :], C["ident_f"][0:4, 0:4], [b_bsn, B["ident_f"]], [b_pS[0]])
            V("tensor_copy", [b_pS[0]], [b_bs], out=bs[:], in_=pS[0][:, 0:4])
            S.barrier()

        xcT = sb("xcT", [128, 2, 528], BF16); b_xcT = Buf()
        kselT = sb("kselT", [128, SL], BF16); kwinT = sb("kwinT", [128, 1024], BF16)
        kdsT = sb("kdsT", [128, SL], BF16); ikT = sb("ikT", [128, SL], BF16)
        vsel = sb("vsel", [128, NT, 2, 65], BF16); vwin = sb("vwin", [128, 8, 2, 65], BF16)
        vds = sb("vds", [128, NT, 65], BF16)
        b_kv = [Buf() for _ in range(NG)]
        b_kw = [Buf(), Buf()]
        b_kv0 = Buf()
        G("memset", [], [b_kv0], ap=vsel[:, :, :, 64:65], constant=1.0)
        G("memset", [], [b_kv0], ap=vwin[:, :, :, 64:65], constant=1.0)
        G("memset", [], [b_kv0], ap=vds[:, :, 64:65], constant=1.0)
        hidT = sb("hidT", [128, 2, 256], BF16); kcT = sb("kcT", [128, 256], BF16)
        vcmp = sb("vcmp", [128, 2, 2, 129], BF16); b_cmp = Buf()
        G("memset", [], [b_cmp], ap=hidT[:], constant=0.0)
        G("memset", [b_cmp], [b_cmp], ap=kcT[:], constant=0.0)
        G("memset", [b_cmp], [b_cmp], ap=vcmp[:], constant=0.0)
        G("memset", [b_cmp], [b_cmp], ap=vcmp[:, :, :, 64:65], constant=1.0)
        for g in range(2):
            G("tensor_copy", [b_cmp, B["overlap"]], [b_cmp], out=vcmp[:, :, g, 65:129], in_=C["overlap"][:])

        xs = [sb("xs0", [128, D], F32)] * 2; _bxs = Buf(); b_xs = [_bxs, _bxs]
        xr = [sb("xr0", [128, D], F32)] * 2; _bxr = Buf(); b_xr = [_bxr, _bxr]
        stats2 = sb("stats2", [128, 2, 6], F32); b_st2 = Buf()
        mv2 = sb("mv2", [128, 4], F32); b_mv2 = Buf()
        stats = sb("stats", [128, 2, 6], F32); b_st = Buf()
        mv = sb("mv", [128, 4], F32); b_mv = Buf()
        mv3 = sb("mv3", [128, 2], F32); b_mv3 = Buf()
        rt = acc[:, 2048:4096].rearrange("p (a t) -> p a t", a=4); b_rt = b_acc
        junk, b_junk = dmask, b_dm
        hT = dmask[:, 0:4096].rearrange("p (k t) -> p k t", k=8); b_hT = b_dm
        qf = sb("qf", [128, 512], F32); b_qf = Buf()
        rl = [sb(f"rl{i}", [128, 512], F32) for i in range(2)]; b_rl = [Buf(), Buf()]
        t1, b_t1, t2, b_t2 = rl[0], b_rl[0], rl[1], b_rl[1]
        qrT = sb("qrT", [128, 3, 512], BF16); qropT = sb("qropT", [128, 3, 512], BF16)
        dqT = sb("dqT", [128, 3, 512], BF16); iqT = sb("iqT", [64, 2, 512], BF16); b_q = Buf()
        gl = sb("gl", [128, 4, 24], F32); b_gl = Buf()
        ckv = sb("ckv", [128, 128], F32); b_ckv = Buf()
        ckvT = sb("ckvT", [128, 512], BF16); b_ckvT = Buf()
        zt, b_zt = qf, b_qf
        ocs = sb("ocs", [128, 4, 256], BF16); b_ocs = Buf()
        cat = [sb(f"cat{i}", [128, D], BF16) for i in range(2)]; b_cat = [Buf(), Buf()]
        catT = sb("catT", [128, 8, 128], BF16); b_catT = Buf()
        E = [sb(f"E{i}", [128, 768], BF16) for i in range(2)]; b_E = [Buf(), Buf()]
        Pm = [sb(f"Pm{i}", [128, 768], BF16) for i in range(2)]; b_Pm = [Buf(), Buf()]
        sm = sb("sm", [128, 64], F32); b_sm = Buf()
        sm_n = sb("sm_n", [128, 64], F32); b_smn = Buf()
        sm_d = sb("sm_d", [128, 64], F32); b_smd = Buf()
        Ed = [sb(f"Ed{i}", [128, 768], BF16) for i in range(2)]; b_Ed = [Buf(), Buf()]
        b_Eh = [Buf() for _ in range(4)]; b_Ph = [Buf() for _ in range(4)]
        b_Edh = [Buf() for _ in range(4)]; b_Pdh = [Buf(), Buf()]
        Pd = [sb("Pd0", [128, 768], BF16)] * 2; _bpd = Buf(); b_Pd = [_bpd, _bpd]
        oc = sb("oc", [128, 6, 64], F32); b_oc = Buf()
        imp = sb("imp", [128, 6, 64], F32); b_imp = Buf()
        sc = sb("sc", [128, 2, 64], F32); sc2 = sb("sc2", [128, 2, 64], F32); b_sc = Buf()
        m8 = sb("m8", [128, 2, 8], F32); b_m8 = Buf()
        bm = sb("bm", [128, 2, 64], BF16); b_bm = Buf()
        bx = sb("bx", [128, 2, 128], BF16); b_bx = [Buf(), Buf()]
        bst = sb("bst", [128, 8 + NBIS], F32); b_bst = Buf()
        wsg = sb("wsg", [128, 12], F32); b_wsg = Buf()
        sg = sb("sgt", [128, 4, 256], F32); b_sgt = Buf()
        vln = sb("vln", [128, 256], BF16); b_vln = Buf()

        nE = [0]

        def nextE():
            i = nE[0] % 2
            nE[0] += 1
            return i

        nEd = [0]

        def nextEd():
            i_ = nEd[0] % 2
            nEd[0] += 1
            return i_

        nxs = 0
        import os
        MS = float(os.environ.get("MIXSTOP", "9"))
        for gi in range(NG):
            gs = slice(gi * 512, (gi + 1) * 512)
            wsl = slice((gi % 2) * 512, (gi % 2) * 512 + 512)
            KVW = [b_kv[gi]]
            KWW = [b_kw[gi % 2]]
            LD(rt, self.rope_d[:, :, gs].rearrange("a p t -> p a t"), [b_rt], R=[self.b_rope])
            for tl in range(4):
                t = gi * 4 + tl
                xi = nxs % 2; nxs += 1
                LD(xs[xi][:], xin[t * 128:(t + 1) * 128, :], [b_xs[xi]], R=[bxin[t]])
                self.ln_stats(xs[xi], b_xs[xi], mv, b_mv, mv[:, 2:3], stats, b_st)
                V("tensor_scalar", [b_xs[xi], b_mv], [b_xs[xi]], out=xs[xi][:], in0=xs[xi][:], scalar1=mv[:, 0:1], scalar2=mv[:, 2:3],
                  op0=ALU.subtract, op1=ALU.mult)
                for hf in range(2):
                    pp, bpp = (pA, b_pA) if hf == 0 else (pB, b_pB)
                    for k4 in range(4):
                        TR(pp[:, k4 * 128:(k4 + 1) * 128], xs[xi][:, (hf * 4 + k4) * 128:(hf * 4 + k4 + 1) * 128], C["ident_f"][:],
                           [b_xs[xi], B["ident_f"]], [bpp])
                    for k4 in range(4):
                        k = hf * 4 + k4
                        A(hT[:, k, tl * 128:(tl + 1) * 128], pp[:, k4 * 128:(k4 + 1) * 128], AF.Identity, [bpp, self.b_mod], [b_hT],
                          bias=self.modcol[:, l, 0, k:k + 1], scale=self.modcol[:, l, 1, k:k + 1])

            if MS <= 1:
                S.barrier(); return
            def proj_fm(wcols_fn, M, pp, bpp):
                for k in range(8):
                    MM(pp[0:M, :], wcols_fn(k), hT[:, k, :], k == 0, [b_win, b_ik4, b_hT], [bpp])

            def rope_from_pA(dst, M, tab, Rm, RB):
                RO = int(os.environ.get("ROPE_OFF", "0"))
                if RO == 1:
                    A(dst[0], pA[0:M, :], AF.Copy, [b_pA], dst[1]); return
                A(qf[0:M, :], pA[0:M, :], AF.Copy, [b_pA], [b_qf])
                if RO == 2:
                    A(dst[0], pA[0:M, :], AF.Copy, [b_pA], dst[1]); return
                if RO == 3:
                    MM(pB[0:M, :], Rm[0:M, 0:M], qf[0:M, :], True, [RB, b_qf], [b_pB])
                    A(dst[0], pB[0:M, :], AF.Copy, [b_pB], dst[1]); return
                MM(pB[0:M, :], Rm[0:M, 0:M], qf[0:M, :], True, [RB, b_qf], [b_pB])
                V("tensor_tensor", [b_pA, b_rt], [b_t1], out=t1[0:M, :], in0=pA[0:M, :], in1=rt[0:M, tab, :], op=ALU.mult)
                if RO == 4:
                    A(dst[0], t1[0:M, :], AF.Copy, [b_t1], dst[1]); return
                V("tensor_tensor", [b_pB, b_rt], [b_t2], out=t2[0:M, :], in0=pB[0:M, :], in1=rt[0:M, tab + 1, :], op=ALU.mult)
                if RO == 5:
                    A(dst[0], t2[0:M, :], AF.Copy, [b_t2], dst[1]); return
                G("tensor_tensor", [b_t1, b_t2], dst[1], out=dst[0], in0=t1[0:M, :], in1=t2[0:M, :], op=ALU.add)

            def rope_fm(dst, wp, M, tab):
                proj_fm(wp, M, pA, b_pA)
                if tab == 0:
                    rope_from_pA(dst, M, 0, C["rot_h"], B["rot_h"])
                else:
                    rope_from_pA(dst, M, 2, C["rot_i"], B["rot_i"])

            def run_il(gens):
                gens = [g_ for g_ in gens if g_ is not None]
                while gens:
                    for g_ in list(gens):
                        try:
                            next(g_)
                        except StopIteration:
                            gens.remove(g_)

            def g_proj():
                for r in range(3):
                    proj_fm(lambda k: win[:, k, Q0 + r * 128:Q0 + (r + 1) * 128], 128, pA, b_pA)
                    A(qrT[:, r, :], pA[:, :], AF.Copy, [b_pA], [b_q])
                    rope_from_pA((qropT[:, r, :], [b_q]), 128, 0, C["rot_h"], B["rot_h"])
                    yield
                    rope_fm((dqT[:, r, :], [b_q]), lambda k: win[:, k, DQ0 + r * 128:DQ0 + (r + 1) * 128], 128, 0)
                    yield
                rope_fm((kselT[:, gs], KVW), lambda k: win[:, k, KV0 + 256:KV0 + 384], 128, 0)
                yield
                rope_fm((kwinT[:, wsl], KWW), lambda k: win[:, k, KV0 + 512:KV0 + 640], 128, 0)
                yield
                for hh2 in range(2):
                    rope_fm((iqT[:, hh2, :], [b_q]), lambda k: win[:, k, IQ0 + hh2 * 64:IQ0 + hh2 * 64 + 64], 64, 2)
                    yield
                rope_fm((ikT[:, gs], KVW), lambda k: ik4[:, k, :], 128, 2)
                yield
                if gi > 0:
                    G("tensor_copy", [b_xcT], [b_xcT], out=xcT[:, :, 0:16], in_=xcT[:, :, 512:528])
                for j in range(2):
                    proj_fm(lambda k: win[:, k, KV0 + j * 128:KV0 + (j + 1) * 128], 128, pA, b_pA)
                    A(xcT[:, j, 16:528], pA[:, :], AF.Copy, [b_pA], [b_xcT])
                    yield

            def g_tok():
                pT_, bT_ = pO[0], b_pO[0]
                for tl in range(4):
                    t = gi * 4 + tl
                    ts = slice(tl * 128, (tl + 1) * 128)
                    for k in range(8):
                        MM(pT_[:, 0:128], hT[:, k, ts], win[:, k, KV0 + 384:KV0 + 512], k == 0, [b_hT, b_win], [bT_])
                    for k in range(8):
                        MM(pT_[:, 128:256], hT[:, k, ts], win[:, k, KV0 + 640:KV0 + 768], False, [b_hT, b_win], [bT_])
                    for k in range(8):
                        MM(pT_[:, 256:384], hT[:, k, ts], win[:, k, CKV0:CKV0 + 128], False, [b_hT, b_win], [bT_])
                    for k in range(8):
                        MM(pT_[:, 384:402], hT[:, k, ts], win[:, k, G0:G0 + 18], False, [b_hT, b_win], [bT_])
                    for k in range(8):
                        MM(pT_[:, 402:406], hT[:, k, ts], win[:, k, IW0:IW0 + 4], False, [b_hT, b_win], [bT_])
                    yield
                    A(vsel[:, t, :, 0:64], pT_[:, 0:128].rearrange("p (g d) -> p g d", g=2), AF.Copy, [bT_], KVW)
                    A(vwin[:, t % 8, :, 0:64], pT_[:, 128:256].rearrange("p (g d) -> p g d", g=2), AF.Copy, [bT_], KWW)
                    A(gl[:, tl, 0:18], pT_[:, 384:402], AF.Sigmoid, [bT_], [b_gl])
                    V("tensor_scalar", [bT_], [b_gl], out=gl[:, tl, 18:22], in0=pT_[:, 402:406], scalar1=0.5, scalar2=None, op0=ALU.mult)
                    A(ckv[:], pT_[:, 256:384], AF.Square, [bT_], [b_ckv, b_mv3], accum_out=mv3[:, 0:1])
                    V("tensor_scalar", [b_ckv, b_mv3], [b_mv3], out=mv3[:, 0:1], in0=mv3[:, 0:1], scalar1=1.0 / 128, scalar2=1e-6, op0=ALU.mult, op1=ALU.add)
                    A(mv3[:, 0:1], mv3[:, 0:1], AF.Sqrt, [b_mv3], [b_mv3])
                    V("reciprocal", [b_mv3], [b_mv3], out=mv3[:, 0:1], in_=mv3[:, 0:1])
                    V("scalar_tensor_tensor", [bT_, b_mv3, b_kvg], [b_ckv], out=ckv[:], in0=pT_[:, 256:384], scalar=mv3[:, 0:1], in1=kvg[:],
                      op0=ALU.mult, op1=ALU.mult)
                    yield
                    TR(pO[1][:, 0:128], ckv[:], C["ident_f"][:], [b_ckv, B["ident_f"]], [b_pO[1]])
                    A(ckvT[:, ts], pO[1][:, 0:128], AF.Copy, [b_pO[1]], [b_ckvT])
                    MM(pS[0][:, 0:64], ckvT[:, ts], wuv[:], True, [b_ckvT, b_wu], [b_pS[0]])
                    A(vds[:, t, 0:64], pS[0][:, 0:64], AF.Copy, [b_pS[0]], KVW)
                    yield

            def g_sgu():
                pZ, bZ = pO[2], b_pO[2]
                for tl in range(4):
                    ts = slice(tl * 128, (tl + 1) * 128)
                    for k in range(8):
                        MM(pZ[:, :], hT[:, k, ts], win[:, k, SGU0:SGU0 + 512], k == 0, [b_hT, b_win], [bZ])
                    yield
                    z = pZ[:, :]
                    sg01 = sg[:, 0:2, :].rearrange("p a b -> p (a b)")
                    A(sg01, z, AF.Square, [bZ], [b_sgt])
                    V("tensor_scalar", [b_sgt], [b_sgt], out=sg01, in0=sg01, scalar1=0.044715, scalar2=1.0, op0=ALU.mult, op1=ALU.add)
                    V("tensor_tensor", [b_sgt, bZ], [b_sgt], out=sg01, in0=sg01, in1=z, op=ALU.mult)
                    A(sg01, sg01, AF.Sigmoid, [b_sgt], [b_sgt], scale=1.5957691216)
                    V("tensor_tensor", [b_sgt, bZ], [b_sgt], out=sg01, in0=sg01, in1=z, op=ALU.mult)
                    yield
                    u_ = sg[:, 0, :]; v_ = sg[:, 1, :]; v3 = v_.rearrange("p (g d) -> p g d", g=4)
                    s2v = sg[:, 2, :].rearrange("p (g d) -> p g d", g=4)
                    s3v = sg[:, 3, :].rearrange("p (g d) -> p g d", g=4)
                    V("tensor_reduce", [b_sgt], [b_sm], out=sm[:, 32:36], in_=v3, axis=AX.X, op=ALU.add)
                    V("tensor_scalar", [b_sm], [b_sm], out=sm[:, 32:36], in0=sm[:, 32:36], scalar1=1.0 / 64, scalar2=None, op0=ALU.mult)
                    V("tensor_tensor", [b_sgt, b_sm], [b_sgt], out=s2v, in0=v3, in1=sm[:, 32:36].unsqueeze(2).to_broadcast([128, 4, 64]), op=ALU.subtract)
                    V("tensor_tensor", [b_sgt], [b_sgt], out=sg[:, 3, :], in0=sg[:, 2, :], in1=sg[:, 2, :], op=ALU.mult)
                    V("tensor_reduce", [b_sgt], [b_sm], out=sm[:, 36:40], in_=s3v, axis=AX.X, op=ALU.add)
                    V("tensor_scalar", [b_sm], [b_sm], out=sm[:, 36:40], in0=sm[:, 36:40], scalar1=1.0 / 64, scalar2=1e-5, op0=ALU.mult, op1=ALU.add)
                    A(sm[:, 36:40], sm[:, 36:40], AF.Sqrt, [b_sm], [b_sm])
                    V("reciprocal", [b_sm], [b_sm], out=sm[:, 36:40], in_=sm[:, 36:40])
                    yield
                    V("tensor_tensor", [b_sgt, b_sm], [b_sgt], out=s2v, in0=s2v, in1=sm[:, 36:40].unsqueeze(2).to_broadcast([128, 4, 64]), op=ALU.mult)
                    V("tensor_tensor", [b_sgt, b_sg], [b_sgt], out=sg[:, 2, :], in0=sg[:, 2, :], in1=sgg[:], op=ALU.mult)
                    V("tensor_tensor", [b_sgt, b_sg], [b_vln], out=vln[:], in0=sg[:, 2, :], in1=sgb[:], op=ALU.add)
                    for g4 in range(4):
                        MM(pS[1][:, g4 * 64:(g4 + 1) * 64], wsT[:, g4, :], vln[:, g4 * 64:(g4 + 1) * 64], g4 == 0, [b_ws, b_vln], [b_pS[1]])
                    V("tensor_tensor", [b_pS[1], b_bs], [b_sgt], out=s3v, in0=pS[1][:, 0:256].rearrange("p (g d) -> p g d", g=4),
                      in1=bs[:, 0:4].unsqueeze(2).to_broadcast([128, 4, 64]), op=ALU.add)
                    V("tensor_tensor", [b_sgt], [b_ocs], out=ocs[:, tl, :], in0=sg[:, 3, :], in1=u_, op=ALU.mult)
                    yield

            run_il([g_proj(), g_tok(), g_sgu()])
            if MS <= 1.6:
                S.barrier(); return
            MM(pA[:, :], wuk[:, :], ckvT[:, :], True, [b_wu, b_ckvT], [b_pA])
            rope_from_pA((kdsT[:, gs], KVW), 128, 0, C["rot_h"], B["rot_h"])

            if MS <= 2:
                S.barrier(); return
            n0 = max(0, 32 * gi - 1)
            n1 = 32 * gi + 31
            nn = n1 - n0
            lo0 = 16 * (n0 - 32 * gi + 1)
            for j in range(2):
                for g in range(2):
                    pq, bpq = (pA, b_pA) if g == 0 else (pB, b_pB)
                    for ll in range(32):
                        rhs = xcT[64 * g:64 * g + 64, j, lo0 + ll:lo0 + ll + 16 * (nn - 1) + 1:16]
                        MM(pq[64 * g:64 * g + 64, j * 32:j * 32 + nn], w1[64 * g:64 * g + 64, j, ll, :], rhs, ll == 0,
                           [b_cw, b_xcT], [bpq])
            for j in range(2):
                xx = sg[:, 0, 0:nn]
                V("tensor_scalar", [b_pA, b_cb], [b_sgt], out=sg[0:64, 0, 0:nn], in0=pA[0:64, j * 32:j * 32 + nn], scalar1=cbias[0:64, j:j + 1], scalar2=None, op0=ALU.add)
                V("tensor_scalar", [b_pB, b_cb], [b_sgt], out=sg[64:128, 0, 0:nn], in0=pB[64:128, j * 32:j * 32 + nn], scalar1=cbias[64:128, j:j + 1], scalar2=None, op0=ALU.add)
                V("tensor_tensor", [b_sgt], [b_sgt], out=sg[:, 1, 0:nn], in0=xx, in1=xx, op=ALU.mult)
                V("tensor_scalar", [b_sgt], [b_sgt], out=sg[:, 1, 0:nn], in0=sg[:, 1, 0:nn], scalar1=0.044715, scalar2=1.0, op0=ALU.mult, op1=ALU.add)
                V("tensor_tensor", [b_sgt], [b_sgt], out=sg[:, 1, 0:nn], in0=sg[:, 1, 0:nn], in1=xx, op=ALU.mult)
                A(sg[:, 1, 0:nn], sg[:, 1, 0:nn], AF.Sigmoid, [b_sgt], [b_sgt], scale=1.5957691216)
                V("tensor_tensor", [b_sgt], [b_cmp], out=hidT[:, j, n0:n1], in0=sg[:, 1, 0:nn], in1=xx, op=ALU.mult)
            for g in range(2):
                MM(pS[g][64 * g:64 * g + 64, 0:nn], w2[64 * g:64 * g + 64, 0, :], hidT[64 * g:64 * g + 64, 0, n0:n1], True, [b_cw, b_cmp], [b_pS[g]])
                V("tensor_copy", [b_pS[g]], [b_cmp], out=kcT[64 * g:64 * g + 64, n0:n1], in_=pS[g][64 * g:64 * g + 64, 0:nn])
            for a in ((32 * gi - 32, 32 * gi), (32 * gi, 32 * gi + 32)):
                if a[0] < 0:
                    continue
                nt_, po = a[0] // 128, a[0] % 128
                for g in range(2):
                    if po < 96:
                        MM(pS[g][po:po + 32, 64:128], hidT[64 * g:64 * g + 64, 1, a[0]:a[1]], w2[64 * g:64 * g + 64, 1, :], True,
                           [b_cw, b_cmp], [b_pS[g]])
                        V("tensor_copy", [b_pS[g]], [b_cmp], out=vcmp[po:po + 32, nt_, g, 0:64], in_=pS[g][po:po + 32, 64:128])
                    else:
                        MM(pS[g][64:128, 64:128], hidT[64 * g:64 * g + 64, 1, a[0] - 32:a[1]], w2[64 * g:64 * g + 64, 1, :], True,
                           [b_cw, b_cmp], [b_pS[g]])
                        V("tensor_copy", [b_pS[g]], [b_cmp], out=vcmp[64:128, nt_, g, 0:64], in_=pS[g][64:128, 64:128])

            if MS <= 2.5:
                S.barrier(); return
            def nsa_chain(i, tl, ci):
                ts = slice(tl * 128, (tl + 1) * 128)
                KVR = [b_kv[x] for x in range(gi + 1)] + [b_kv0]
                sm = sm_n; b_sm = b_smn
                catc, b_catc = cat[ci], b_cat[ci]
                n_nt = 1 if (8 * i + 7) <= 128 else 2
                first = [True, True]
                for nt_ in range(n_nt):
                    ei = nextE()
                    bE2 = [b_Eh[2 * ei], b_Eh[2 * ei + 1]]
                    for g in range(2):
                        MM(pS[g][:, 0:384].rearrange("p (r t) -> p r t", r=3), kcT[64 * g:64 * g + 64, nt_ * 128:(nt_ + 1) * 128],
                           qrT[64 * g:64 * g + 64, :, ts], True, [b_cmp, b_q], [b_pS[g]])
                        A(E[ei][:, g * 384:(g + 1) * 384], pS[g][:, 0:384], AF.Exp, [b_pS[g]], [bE2[g]], scale=0.125)
                    G("affine_select", bE2, bE2, out=E[ei][:, :], in_=E[ei][:, :], pattern=[[0, 6], [1, 128]],
                      compare_op=ALU.is_ge, fill=0.0, base=128 * i - 2048 * nt_ - 31, channel_multiplier=-16)
                    for g in range(2):
                        for r in range(3):
                            MM(pO[g][:, r * 129:(r + 1) * 129], E[ei][:, (g * 3 + r) * 128:(g * 3 + r + 1) * 128], vcmp[:, nt_, g, :],
                               first[g], [bE2[g], b_cmp], [b_pO[g]])
                            first[g] = False
                    yield
                for g in range(2):
                    po3 = pO[g][:, 0:387].rearrange("p (r c) -> p r c", r=3)
                    V("tensor_scalar", [b_pO[g]], [b_sm], out=sm[:, 40 + g * 3:43 + g * 3], in0=po3[:, :, 64], scalar1=0.0, scalar2=None, op0=ALU.is_equal)
                    V("tensor_tensor", [b_pO[g], b_sm], [b_sm], out=sm[:, g * 3:g * 3 + 3], in0=po3[:, :, 64], in1=sm[:, 40 + g * 3:43 + g * 3], op=ALU.add)
                V("reciprocal", [b_sm], [b_sm], out=sm[:, 0:6], in_=sm[:, 0:6])
                glv = gl[:, tl, 0:18].rearrange("p (h b) -> p h b", b=3)
                V("tensor_tensor", [b_sm, b_gl], [b_sm], out=sm[:, 6:12], in0=sm[:, 0:6], in1=glv[:, :, 0], op=ALU.mult)
                for g in range(2):
                    po3 = pO[g][:, 0:387].rearrange("p (r c) -> p r c", r=3)
                    V("tensor_tensor", [b_pO[g], b_sm], [b_imp], out=imp[:, g * 3:g * 3 + 3, :], in0=po3[:, :, 65:129],
                      in1=sm[:, g * 3:g * 3 + 3].unsqueeze(2).to_broadcast([128, 3, 64]), op=ALU.mult)
                    V("tensor_tensor", [b_pO[g], b_sm], [b_oc], out=oc[:, g * 3:g * 3 + 3, :], in0=po3[:, :, 0:64],
                      in1=sm[:, 6 + g * 3:9 + g * 3].unsqueeze(2).to_broadcast([128, 3, 64]), op=ALU.mult)
                yield
                for g in range(2):
                    V("tensor_tensor", [b_imp], [b_sc], out=sc[:, g, :], in0=imp[:, g * 3, :], in1=imp[:, g * 3 + 1, :], op=ALU.add)
                    V("tensor_tensor", [b_imp, b_sc], [b_sc], out=sc[:, g, :], in0=sc[:, g, :], in1=imp[:, g * 3 + 2, :], op=ALU.add)
                    V("tensor_tensor", [b_sc, B["rel"]], [b_sc], out=sc[:, g, :], in0=sc[:, g, :], in1=C["rel"][:, 63 - 2 * i:127 - 2 * i], op=ALU.add)
                    V("tensor_tensor", [b_sc, B["col0"]], [b_sc], out=sc[:, g, :], in0=sc[:, g, :], in1=C["col0"][:], op=ALU.add)
                    V("max", [b_sc], [b_m8], out=m8[:, g, :], in_=sc[:, g, :])
                    V("match_replace", [b_sc, b_m8], [b_sc], out=sc2[:, g, :], in_to_replace=m8[:, g, :], in_values=sc[:, g, :], imm_value=-3.0e38)
                    V("max", [b_sc], [b_m8], out=m8[:, g, :], in_=sc2[:, g, :])
                    V("tensor_scalar", [b_sc, b_m8], [b_bm], out=bm[:, g, :], in0=sc[:, g, :], scalar1=m8[:, g, 7:8], scalar2=None, op0=ALU.is_ge)
                yield
                stO = [True]

                def sel_front(it):
                    j2, g, k = it
                    eh = k % 4
                    Eh = E[eh // 2][:, (eh % 2) * 384:(eh % 2 + 1) * 384]
                    MM(pS[g][:, 0:384].rearrange("p (r t) -> p r t", r=3), kselT[64 * g:64 * g + 64, j2 * 128:(j2 + 1) * 128],
                       qropT[64 * g:64 * g + 64, :, ts], True, KVR + [b_q], [b_pS[g]])
                    if j2 == i:
                        MM(pS[g][:, 0:384], C["ident_b"][:], C["negtri_c"][:], False, [B["ident_b"], B["negtri_c"]], [b_pS[g]])
                    A(Eh, pS[g][:, 0:384], AF.Exp, [b_pS[g]], [b_Eh[eh]], scale=0.125)
                    V("tensor_copy", [b_bm], [b_bx[g]], out=bx[:, g, :].rearrange("p (a b) -> p a b", a=2),
                      in_=bm[:, g, 2 * j2:2 * j2 + 2].unsqueeze(2).to_broadcast([128, 2, 64]))
                    TR(pM[:, g * 128:(g + 1) * 128], bx[:, g, :], C["ident_b"][:], [b_bx[g], B["ident_b"]], [b_pM[0]])

                def sel_back(it):
                    j2, g, k = it
                    eh = k % 4
                    Eh = E[eh // 2][:, (eh % 2) * 384:(eh % 2 + 1) * 384]
                    Ph = Pm[eh // 2][:, (eh % 2) * 384:(eh % 2 + 1) * 384]
                    V("tensor_tensor", [b_Eh[eh], b_pM[0]], [b_Ph[eh]], out=Ph.rearrange("p (r t) -> p r t", r=3),
                      in0=Eh.rearrange("p (r t) -> p r t", r=3),
                      in1=pM[:, g * 128:(g + 1) * 128].unsqueeze(1).to_broadcast([128, 3, 128]), op=ALU.mult)
                    for r in range(3):
                        h = g * 3 + r
                        MM(pO[2][:, h * 65:(h + 1) * 65], Ph[:, r * 128:(r + 1) * 128], vsel[:, j2, g, :], stO[0],
                           [b_Ph[eh]] + KVR, [b_pO[2]])
                        stO[0] = False

                items = [(j2, g, 2 * j2 + g) for j2 in range(i + 1) for g in range(2)]
                prev = None
                for it in items:
                    sel_front(it)
                    if prev is not None:
                        sel_back(prev)
                        yield
                    prev = it
                sel_back(prev)
                yield
                po6 = pO[2][:, 0:390].rearrange("p (h c) -> p h c", h=6)
                V("tensor_scalar", [b_pO[2]], [b_sm], out=sm[:, 12:18], in0=po6[:, :, 64], scalar1=1e-30, scalar2=None, op0=ALU.max)
                V("reciprocal", [b_sm], [b_sm], out=sm[:, 12:18], in_=sm[:, 12:18])
                V("tensor_tensor", [b_sm, b_gl], [b_sm], out=sm[:, 12:18], in0=sm[:, 12:18], in1=glv[:, :, 1], op=ALU.mult)
                V("tensor_tensor", [b_pO[2], b_sm], [b_imp], out=imp[:], in0=po6[:, :, 0:64],
                  in1=sm[:, 12:18].unsqueeze(2).to_broadcast([128, 6, 64]), op=ALU.mult)
                V("tensor_tensor", [b_imp, b_oc], [b_oc], out=oc[:], in0=oc[:], in1=imp[:], op=ALU.add)
                yield
                stW = [True]

                def win_front(it):
                    j2, g, k = it
                    eh = k % 4
                    Eh = E[eh // 2][:, (eh % 2) * 384:(eh % 2 + 1) * 384]
                    KWR = [b_kw[(j2 // 4) % 2], b_kv0]
                    wc = slice((j2 % 8) * 128, (j2 % 8 + 1) * 128)
                    MM(pS[g][:, 0:384].rearrange("p (r t) -> p r t", r=3), kwinT[64 * g:64 * g + 64, wc],
                       qropT[64 * g:64 * g + 64, :, ts], True, KWR + [b_q], [b_pS[g]])
                    if j2 == i:
                        MM(pS[g][:, 0:384], C["ident_b"][:], C["negtri_c"][:], False, [B["ident_b"], B["negtri_c"]], [b_pS[g]])
                    if j2 == i - 4:
                        MM(pS[g][:, 0:384], C["ident_b"][:], C["negtri_w"][:], False, [B["ident_b"], B["negtri_w"]], [b_pS[g]])
                    A(Eh, pS[g][:, 0:384], AF.Exp, [b_pS[g]], [b_Eh[eh]], scale=0.125)

                def win_back(it):
                    j2, g, k = it
                    eh = k % 4
                    Eh = E[eh // 2][:, (eh % 2) * 384:(eh % 2 + 1) * 384]
                    KWR = [b_kw[(j2 // 4) % 2], b_kv0]
                    for r in range(3):
                        h = g * 3 + r
                        MM(pO[0][:, h * 65:(h + 1) * 65], Eh[:, r * 128:(r + 1) * 128], vwin[:, j2 % 8, g, :], stW[0],
                           [b_Eh[eh]] + KWR, [b_pO[0]])
                        stW[0] = False

                items = [(j2, g, 2 * jj + g) for jj, j2 in enumerate(range(max(0, i - 4), i + 1)) for g in range(2)]
                prev = None
                for it in items:
                    win_front(it)
                    if prev is not None:
                        win_back(prev)
                        yield
                    prev = it
                win_back(prev)
                yield
                po6 = pO[0][:, 0:390].rearrange("p (h c) -> p h c", h=6)
                V("tensor_scalar", [b_pO[0]], [b_sm], out=sm[:, 18:24], in0=po6[:, :, 64], scalar1=1e-30, scalar2=None, op0=ALU.max)
                V("reciprocal", [b_sm], [b_sm], out=sm[:, 18:24], in_=sm[:, 18:24])
                V("tensor_tensor", [b_sm, b_gl], [b_sm], out=sm[:, 18:24], in0=sm[:, 18:24], in1=glv[:, :, 2], op=ALU.mult)
                V("tensor_tensor", [b_pO[0], b_sm], [b_imp], out=imp[:], in0=po6[:, :, 0:64],
                  in1=sm[:, 18:24].unsqueeze(2).to_broadcast([128, 6, 64]), op=ALU.mult)
                V("tensor_tensor", [b_imp, b_oc], [b_catc], out=catc[:, 0:384].rearrange("p (h d) -> p h d", h=6), in0=oc[:], in1=imp[:], op=ALU.add)
                G("tensor_copy", [b_ocs], [b_catc], out=catc[:, 768:1024], in_=ocs[:, tl, :])
                yield

            def dsa_chain(i, tl, ci):
                ts = slice(tl * 128, (tl + 1) * 128)
                KVR = [b_kv[x] for x in range(gi + 1)] + [b_kv0]
                nk = 128 * (i + 1)
                sm = sm_d; b_sm = b_smd
                catc, b_catc = cat[ci], b_cat[ci]
                V("tensor_single_scalar", [b_gl], [b_wsg], out=wsg[:, 8:12], in_=gl[:, tl, 18:22], scalar=-1.0, op=ALU.mult)
                V("tensor_tensor", [b_gl, b_wsg], [b_wsg], out=wsg[:, 0:4], in0=gl[:, tl, 18:22], in1=wsg[:, 8:12], op=ALU.max)
                V("tensor_single_scalar", [b_gl], [b_wsg], out=wsg[:, 4:8], in_=gl[:, tl, 18:22], scalar=0.0, op=ALU.is_gt)
                V("tensor_single_scalar", [b_gl], [b_wsg], out=wsg[:, 8:12], in_=gl[:, tl, 18:22], scalar=0.0, op=ALU.is_lt)
                V("tensor_tensor", [b_wsg], [b_wsg], out=wsg[:, 4:8], in0=wsg[:, 4:8], in1=wsg[:, 8:12], op=ALU.subtract)
                for c0 in range(0, nk, 512):
                    cw = min(512, nk - c0)
                    for h in range(4):
                        pp, bpp = (pA, b_pA) if h % 2 == 0 else (pB, b_pB)
                        ri = h % 2
                        MM(pp[:, 0:cw], iqT[32 * (h % 2):32 * (h % 2) + 32, h // 2, ts], ikT[32 * (h % 2):32 * (h % 2) + 32, c0:c0 + cw], True, [b_q] + KVR, [bpp])
                        A(rl[ri][:, 0:cw], pp[:, 0:cw], AF.Relu, [bpp, b_wsg], [b_rl[ri]], scale=wsg[:, h:h + 1])
                        if h == 0:
                            V("tensor_scalar", [b_rl[ri], b_wsg], [b_acc], out=acc[:, c0:c0 + cw], in0=rl[ri][:, 0:cw], scalar1=wsg[:, 4:5], scalar2=None, op0=ALU.mult)
                        else:
                            V("scalar_tensor_tensor", [b_rl[ri], b_wsg, b_acc], [b_acc], out=acc[:, c0:c0 + cw], in0=rl[ri][:, 0:cw],
                              scalar=wsg[:, 4 + h:5 + h], in1=acc[:, c0:c0 + cw], op0=ALU.mult, op1=ALU.add)
                    yield
                if nk > self.KTOP:
                    V("tensor_reduce", [b_acc], [b_bst], out=bst[:, 1:2], in_=acc[:, 0:nk], axis=AX.X, op=ALU.max)
                    V("tensor_reduce", [b_acc], [b_bst], out=bst[:, 0:1], in_=acc[:, 0:nk], axis=AX.X, op=ALU.min)
                G("affine_select", [b_acc], [b_acc], out=acc[:, 128 * i:128 * i + 128], in_=acc[:, 128 * i:128 * i + 128], pattern=[[-1, 128]],
                  compare_op=ALU.is_ge, fill=FILL, base=0, channel_multiplier=1)
                yield
                if nk > self.KTOP:
                    V("tensor_scalar", [b_bst], [b_bst], out=bst[:, 0:1], in0=bst[:, 0:1], scalar1=-1.0, scalar2=None, op0=ALU.add)
                    V("tensor_tensor", [b_bst], [b_bst], out=bst[:, 2:3], in0=bst[:, 1:2], in1=bst[:, 0:1], op=ALU.subtract)
                    V("tensor_scalar", [b_bst, B["pow2"]], [b_bst], out=bst[:, 8:8 + NBIS], in0=C["pow2"][:], scalar1=bst[:, 2:3], scalar2=None, op0=ALU.mult)
                    for kb in range(NBIS):
                        V("tensor_tensor", [b_bst], [b_bst], out=bst[:, 3:4], in0=bst[:, 0:1], in1=bst[:, 8 + kb:9 + kb], op=ALU.add)
                        V("tensor_scalar", [b_acc, b_bst], [b_junk, b_bst], out=junk[:, 0:nk], in0=acc[:, 0:nk], scalar1=bst[:, 3:4], scalar2=None,
                          op0=ALU.is_gt, op1=ALU.add, accum_out=bst[:, 4:5])
                        V("tensor_scalar", [b_bst], [b_bst], out=bst[:, 5:6], in0=bst[:, 4:5], scalar1=self.KTOP - 0.5, scalar2=bst[:, 8 + kb:9 + kb],
                          op0=ALU.is_gt, op1=ALU.mult)
                        V("tensor_tensor", [b_bst], [b_bst], out=bst[:, 0:1], in0=bst[:, 0:1], in1=bst[:, 5:6], op=ALU.add)
                        yield
                    V("tensor_scalar", [b_acc, b_bst], [b_dm], out=dmask[:, 0:nk], in0=acc[:, 0:nk], scalar1=bst[:, 0:1], scalar2=None, op0=ALU.is_gt)
                else:
                    V("tensor_single_scalar", [b_acc], [b_dm], out=dmask[:, 0:nk], in_=acc[:, 0:nk], scalar=-1.0e29, op=ALU.is_gt)
                yield
                stD = [True]
                pSd = [pA, pB]; b_pSd = [b_pA, b_pB]

                def dsa_front(it):
                    j2, hb, k = it
                    eh = k % 4
                    Eh = Ed[eh // 2][:, (eh % 2) * 384:(eh % 2 + 1) * 384]
                    po_ = 64 * hb
                    MM(pSd[hb][:, 0:384].rearrange("p (r t) -> p r t", r=3), kdsT[po_:po_ + 64, j2 * 128:(j2 + 1) * 128],
                       dqT[po_:po_ + 64, :, ts], True, KVR + [b_q], [b_pSd[hb]])
                    A(Eh, pSd[hb][:, 0:384], AF.Exp, [b_pSd[hb]], [b_Edh[eh]], scale=0.125)
                    if hb == 0:
                        mo = 512 + 128 * (j2 % 2)
                        TR(pM[:, mo:mo + 128], dmask[:, j2 * 128:(j2 + 1) * 128], C["ident_b"][:], [b_dm, B["ident_b"]], [b_pM[1]])

                def dsa_back(it):
                    j2, hb, k = it
                    eh = k % 4
                    Eh = Ed[eh // 2][:, (eh % 2) * 384:(eh % 2 + 1) * 384]
                    Ph = Pd[0][:, (k % 2) * 384:(k % 2 + 1) * 384]
                    mo = 512 + 128 * (j2 % 2)
                    V("tensor_tensor", [b_Edh[eh], b_pM[1]], [b_Pdh[k % 2]], out=Ph.rearrange("p (r t) -> p r t", r=3),
                      in0=Eh.rearrange("p (r t) -> p r t", r=3),
                      in1=pM[:, mo:mo + 128].unsqueeze(1).to_broadcast([128, 3, 128]), op=ALU.mult)
                    for r in range(3):
                        hh = 2 * r + hb
                        MM(pO[1][:, hh * 65:(hh + 1) * 65], Ph[:, r * 128:(r + 1) * 128], vds[:, j2, :], stD[0], [b_Pdh[k % 2]] + KVR, [b_pO[1]])
                        stD[0] = False

                items = [(j2, hb, 2 * j2 + hb) for j2 in range(i + 1) for hb in range(2)]
                prev = None
                for it in items:
                    dsa_front(it)
                    if prev is not None:
                        dsa_back(prev)
                        yield
                    prev = it
                dsa_back(prev)
                yield
                po6 = pO[1][:, 0:390].rearrange("p (h c) -> p h c", h=6)
                V("tensor_scalar", [b_pO[1]], [b_sm], out=sm[:, 24:30], in0=po6[:, :, 64], scalar1=1e-30, scalar2=None, op0=ALU.max)
                V("reciprocal", [b_sm], [b_sm], out=sm[:, 24:30], in_=sm[:, 24:30])
                V("tensor_tensor", [b_pO[1], b_sm], [b_catc], out=catc[:, 384:768].rearrange("p (h d) -> p h d", h=6), in0=po6[:, :, 0:64],
                  in1=sm[:, 24:30].unsqueeze(2).to_broadcast([128, 6, 64]), op=ALU.mult)
                yield

            def out_chain(i, ci):
                catc, b_catc = cat[ci], b_cat[ci]
                xrc, b_xrc = xr[ci], b_xr[ci]
                if self.debug and l == 0:
                    self.S.dma(self.S.pool, self.dbg[i * 128:(i + 1) * 128, :], catc[:], reads=[b_catc], writes=[Buf()], is_output=True)
                LD(xrc[:], xin[i * 128:(i + 1) * 128, :], [b_xrc], R=[bxin[i]])
                yield
                for k in range(8):
                    TR(pM[:, k * 128:(k + 1) * 128], catc[:, k * 128:(k + 1) * 128], C["ident_b"][:], [b_catc, B["ident_b"]], [b_pM[0]])
                A(catT[:].rearrange("p k t -> p (k t)"), pM[:, :], AF.Copy, [b_pM[0]], [b_catT])
                yield
                for hf in range(2):
                    pp, bpp = (pA, b_pA) if hf == 0 else (pB, b_pB)
                    for k in range(8):
                        MM(pp[:, :], catT[:, k, :], wout[:, k, hf * 512:(hf + 1) * 512], k == 0, [b_catT, b_wout], [bpp])
                    V("scalar_tensor_tensor", [bpp, b_xrc], [b_xrc], out=xrc[:, hf * 512:(hf + 1) * 512], in0=xrc[:, hf * 512:(hf + 1) * 512],
                      scalar=ALPHA, in1=pp[:, :], op0=ALU.mult, op1=ALU.add)
                    yield
                self.ln_stats(xrc, b_xrc, mv2, b_mv2, mv2[:, 2:3], stats2, b_st2)
                V("tensor_scalar", [b_xrc, b_mv2], [b_xrc], out=xrc[:], in0=xrc[:], scalar1=mv2[:, 0:1], scalar2=mv2[:, 2:3], op0=ALU.subtract, op1=ALU.mult)
                yield
                G("tensor_tensor", [b_xrc, b_ln], [b_xrc], out=xrc[:], in0=xrc[:], in1=lng[:], op=ALU.mult)
                G("tensor_tensor", [b_xrc, b_ln], [b_xrc], out=xrc[:], in0=xrc[:], in1=lnb[:], op=ALU.add)
                LD(xout[i * 128:(i + 1) * 128, :], xrc[:], [bxout[i]], R=[b_xrc])
                yield

            def run_interleaved(gens):
                gens = [g_ for g_ in gens if g_ is not None]
                while gens:
                    for g_ in list(gens):
                        try:
                            next(g_)
                        except StopIteration:
                            gens.remove(g_)

            pend = None
            for tl in range(4):
                i = gi * 4 + tl
                ci = i % 2
                run_interleaved([dsa_chain(i, tl, ci), nsa_chain(i, tl, ci), pend])
                pend = out_chain(i, ci)
            run_interleaved([pend])
        S.barrier()

    def ffn_common(self, st, l):
        C, B, I = self.C, self.B, self.I
        sb = lambda n, s, d: self.sb(st, n, s, d)
        o = {}
        o["pA"] = self.ps(st, "fA", [128, 512], F32); o["bpA"] = PBuf()
        o["pB"] = self.ps(st, "fB", [128, 512], F32); o["bpB"] = PBuf()
        o["pT"] = [self.ps(st, f"fT{i}", [128, 512], F32) for i in range(2)]; o["bpT"] = [PBuf(), PBuf()]
        o["pD"] = [self.ps(st, f"fD{i}", [128, 512], F32) for i in range(2)]; o["bpD"] = [PBuf(), PBuf()]
        o["pR"] = self.ps(st, "fR", [128, 512], F32); o["bpR"] = PBuf()
        gbc = sb("gbc2", [128, D], F32); b_gbc = Buf()
        self.LD(gbc[:], self.gate_d[2 * l + 1:2 * l + 2, :].to_broadcast([128, D]), [b_gbc], R=[self.b_gate])
        o["gbc"], o["b_gbc"] = gbc, b_gbc
        lng = sb("lng2", [128, D], F32); lnb = sb("lnb2", [128, D], F32); b_ln = Buf()
        self.LD(lng[:], I["ln2_g"][l:l + 1, :].to_broadcast([128, D]), [b_ln])
        self.LD(lnb[:], I["ln2_b"][l:l + 1, :].to_broadcast([128, D]), [b_ln])
        o["lng"], o["lnb"], o["b_ln"] = lng, lnb, b_ln
        o["stats"] = sb("fstats", [128, 2, 6], F32); o["b_st"] = Buf()
        o["mv"] = sb("fmv", [128, 4], F32); o["b_mv"] = Buf()
        o["xn"] = sb("fxn", [128, D], F32); o["b_xn"] = Buf()
        o["ybuf"] = [sb("fy0", [128, D], F32)] * 2; _by = Buf(); o["b_y"] = [_by, _by]
        return o

    def ffn_ln_mod(self, o, l, xt, b_x, hT, b_hT, tl, hT32=None, b_hT32=None):
        C, B = self.C, self.B
        self.ln_stats(xt, b_x, o["mv"], o["b_mv"], o["mv"][:, 2:3], o["stats"], o["b_st"])
        self.V("tensor_scalar", [b_x, o["b_mv"]], [o["b_xn"]], out=o["xn"][:], in0=xt[:], scalar1=o["mv"][:, 0:1], scalar2=o["mv"][:, 2:3],
               op0=ALU.subtract, op1=ALU.mult)
        for hf in range(2):
            pp, bpp = o["pT"][hf], o["bpT"][hf]
            for k4 in range(4):
                self.TR(pp[:, k4 * 128:(k4 + 1) * 128], o["xn"][:, (hf * 4 + k4) * 128:(hf * 4 + k4 + 1) * 128], C["ident_f"][:],
                        [o["b_xn"], B["ident_f"]], [bpp])
            for k4 in range(4):
                k = hf * 4 + k4
                self.A(hT[:, k, tl * 128:(tl + 1) * 128], pp[:, k4 * 128:(k4 + 1) * 128], AF.Identity, [bpp, self.b_mod], [b_hT],
                       bias=self.modcol[:, l, 2, k:k + 1], scale=self.modcol[:, l, 3, k:k + 1])
                if hT32 is not None:
                    self.V("tensor_scalar", [bpp, self.b_mod], [b_hT32], out=hT32[:, k, :], in0=pp[:, k4 * 128:(k4 + 1) * 128],
                           scalar1=self.modcol[:, l, 3, k:k + 1], scalar2=self.modcol[:, l, 2, k:k + 1], op0=ALU.mult, op1=ALU.add)

    def ffn_finish_tile(self, o, yb, b_yb, dst_ap, b_dst, is_out):
        self.ln_stats(yb, b_yb, o["mv"], o["b_mv"], o["mv"][:, 2:3], o["stats"], o["b_st"])
        self.V("tensor_scalar", [b_yb, o["b_mv"]], [b_yb], out=yb[:], in0=yb[:], scalar1=o["mv"][:, 0:1], scalar2=o["mv"][:, 2:3],
               op0=ALU.subtract, op1=ALU.mult)
        self.G("tensor_tensor", [b_yb, o["b_ln"]], [b_yb], out=yb[:], in0=yb[:], in1=o["lng"][:], op=ALU.mult)
        self.G("tensor_tensor", [b_yb, o["b_ln"]], [b_yb], out=yb[:], in0=yb[:], in1=o["lnb"][:], op=ALU.add)
        self.S.dma(self.S.sp, dst_ap, yb[:], reads=[b_yb], writes=[b_dst], is_output=is_out)

    def ffn_dense(self, st, l, xin, bxin, xout, bxout, is_out):
        S, C, B, I = self.S, self.C, self.B, self.I
        sb = lambda n, s, d: self.sb(st, n, s, d)
        MM, A, V, G, LD = self.MM, self.A, self.V, self.G, self.LD
        j = l // 2
        NF = D_FF // 128
        o = self.ffn_common(st, l)
        wg = sb("wg", [128, 8, D_FF], BF16); wu = sb("wu", [128, 8, D_FF], BF16); wd = sb("wd", [128, NF, D], BF16)
        b_wg, b_wu, b_wd = Buf(), Buf(), Buf()
        for (a, b) in ((0, 1408), (1408, D_FF)):
            LD(wg[:, :, a:b], I["ffn_wg"][j, :, a:b].rearrange("(k p) n -> p k n", p=128), [b_wg], q=S.pool)
            LD(wu[:, :, a:b], I["ffn_wu"][j, :, a:b].rearrange("(k p) n -> p k n", p=128), [b_wu], q=S.pool)
        for (a, b) in ((0, 11), (11, NF)):
            LD(wd[:, a:b, :], I["ffn_wd"][j, a * 128:b * 128, :].rearrange("(k p) n -> p k n", p=128), [b_wd], q=S.pool)
        for k in range(NF):
            V("tensor_tensor", [b_wd, o["b_gbc"]], [b_wd], out=wd[:, k, :], in0=wd[:, k, :], in1=o["gbc"][:], op=ALU.mult)
        xt = [sb(f"fx{i}", [128, D], F32) for i in range(4)]; b_xt = [Buf() for _ in range(4)]
        hT = sb("fhT", [128, 8, 512], BF16); b_hT = Buf()
        hid = sb("hid", [128, NF, 512], BF16); b_hid = Buf()
        sg = [sb("fsg0", [128, 512], F32)] * 2; _bs = Buf(); b_sg = [_bs, _bs]
        ny = 0
        for gi in range(self.NG):
            for tl in range(4):
                t = gi * 4 + tl
                LD(xt[tl][:], xin[t * 128:(t + 1) * 128, :], [b_xt[tl]], R=[bxin[t]])
                self.ffn_ln_mod(o, l, xt[tl], b_xt[tl], hT, b_hT, tl)
            for fc in range(NF):
                for k in range(8):
                    MM(o["pA"][:, :], wg[:, k, fc * 128:(fc + 1) * 128], hT[:, k, :], k == 0, [b_wg, b_hT], [o["bpA"]])
                for k in range(8):
                    MM(o["pB"][:, :], wu[:, k, fc * 128:(fc + 1) * 128], hT[:, k, :], k == 0, [b_wu, b_hT], [o["bpB"]])
                si = fc % 2
                A(sg[si][:], o["pA"][:, :], AF.Silu, [o["bpA"]], [b_sg[si]])
                V("tensor_tensor", [b_sg[si], o["bpB"]], [b_hid], out=hid[:, fc, :], in0=sg[si][:], in1=o["pB"][:, :], op=ALU.mult)
            for tl in range(4):
                t = gi * 4 + tl
                yb, byb = o["ybuf"][ny % 2], o["b_y"][ny % 2]; ny += 1
                for hf in range(2):
                    pd, bpd = o["pD"][hf], o["bpD"][hf]
                    for fc in range(NF):
                        MM(pd[:, :], hid[:, fc, tl * 128:(tl + 1) * 128], wd[:, fc, hf * 512:(hf + 1) * 512], fc == 0, [b_hid, b_wd], [bpd])
                    V("scalar_tensor_tensor", [bpd, b_xt[tl]], [byb], out=yb[:, hf * 512:(hf + 1) * 512], in0=xt[tl][:, hf * 512:(hf + 1) * 512],
                      scalar=ALPHA, in1=pd[:, :], op0=ALU.mult, op1=ALU.add)
                self.ffn_finish_tile(o, yb, byb, xout[t * 128:(t + 1) * 128, :], bxout[t], is_out)
        S.barrier()

    def ffn_moe(self, st, l, xin, bxin, xout, bxout, is_out):
        S, C, B, I = self.S, self.C, self.B, self.I
        sb = lambda n, s, d: self.sb(st, n, s, d)
        MM, A, V, G, LD = self.MM, self.A, self.V, self.G, self.LD
        j = l // 2
        o = self.ffn_common(st, l)
        SGT = min(self.NT, 16)
        NSG = self.NT // SGT
        wr = sb("wr", [128, 8, NEXP], F32); b_wr = Buf()
        LD(wr[:], I["moe_wr"][j].rearrange("(k p) n -> p k n", p=128), [b_wr])
        brb = sb("brb", [128, NEXP], F32)
        LD(brb[:], I["moe_br"][j:j + 1, :].to_broadcast([128, NEXP]), [b_wr])
        hTa = sb("hTa", [128, 8, SGT * 128], BF16); b_hTa = [Buf() for _ in range(SGT // 4)]
        hT32 = sb("hT32", [128, 8, 128], F32); b_hT32 = Buf()
        accs = sb("accs", [128, SGT, D], F32); b_acc = [Buf() for _ in range(SGT)]
        gates = sb("gates", [128, SGT, NEXP], F32); b_gt = Buf()
        lg = sb("lg", [128, 40], F32); b_lg = Buf()
        xt = [sb(f"mx{i}", [128, D], F32) for i in range(2)]; b_xt = [Buf(), Buf()]
        wgc = [sb(f"wgc{i}", [128, 8, 512], BF16) for i in range(2)]
        wuc = [sb(f"wuc{i}", [128, 8, 512], BF16) for i in range(2)]
        wdc = [sb(f"wdc{i}", [128, 4, D], BF16) for i in range(2)]
        b_wc = [Buf(), Buf()]
        hid = sb("mhid", [128, 4, 512], BF16); b_hid = Buf()
        sg = [sb(f"msg{i}", [128, 512], F32) for i in range(2)]; b_sg = [Buf(), Buf()]
        nx = 0
        nw = 0
        for sgi in range(NSG):
            for tt in range(SGT):
                t = sgi * SGT + tt
                xi = nx % 2; nx += 1
                LD(xt[xi][:], xin[t * 128:(t + 1) * 128, :], [b_xt[xi]], R=[bxin[t]])
                self.ffn_ln_mod(o, l, xt[xi], b_xt[xi], hTa[:, :, (tt // 4) * 512:(tt // 4 + 1) * 512], b_hTa[tt // 4], tt % 4, hT32, b_hT32)
                for k in range(8):
                    MM(o["pR"][:, 0:NEXP], hT32[:, k, :], wr[:, k, :], k == 0, [b_hT32, b_wr], [o["bpR"]])
                V("tensor_tensor", [o["bpR"], b_wr], [b_lg], out=lg[:, 0:8], in0=o["pR"][:, 0:NEXP], in1=brb[:], op=ALU.add)
                V("max", [b_lg], [b_lg], out=lg[:, 8:16], in_=lg[:, 0:8])
                V("tensor_tensor", [b_lg], [b_lg], out=lg[:, 16:17], in0=lg[:, 9:10], in1=lg[:, 8:9], op=ALU.subtract)
                A(lg[:, 17:18], lg[:, 16:17], AF.Sigmoid, [b_lg], [b_lg])
                V("tensor_scalar", [b_lg], [b_lg], out=lg[:, 18:19], in0=lg[:, 17:18], scalar1=-1.0, scalar2=1.0, op0=ALU.mult, op1=ALU.add)
                V("tensor_scalar", [b_lg], [b_lg], out=lg[:, 24:32], in0=lg[:, 0:8], scalar1=lg[:, 8:9], scalar2=lg[:, 18:19], op0=ALU.is_equal, op1=ALU.mult)
                V("tensor_scalar", [b_lg], [b_lg], out=lg[:, 32:40], in0=lg[:, 0:8], scalar1=lg[:, 9:10], scalar2=lg[:, 17:18], op0=ALU.is_equal, op1=ALU.mult)
                V("tensor_tensor", [b_lg], [b_gt], out=gates[:, tt, :], in0=lg[:, 24:32], in1=lg[:, 32:40], op=ALU.add)
            import os
            MOES = int(os.environ.get("MOESTOP", "99"))
            for e in range(min(NEXP, MOES)):
                for fc in range(E_FF // 512 if MOES > 1 else 1):
                    wi = nw % 2; nw += 1
                    f0 = fc * 512
                    LD(wgc[wi][:], I["moe_wg"][j, e, :, f0:f0 + 512].rearrange("(k p) n -> p k n", p=128), [b_wc[wi]], q=S.pool)
                    LD(wuc[wi][:], I["moe_wu"][j, e, :, f0:f0 + 512].rearrange("(k p) n -> p k n", p=128), [b_wc[wi]], q=S.pool)
                    LD(wdc[wi][:], I["moe_wd"][j, e, f0:f0 + 512, :].rearrange("(k p) n -> p k n", p=128), [b_wc[wi]], q=S.pool)
                    for k in range(4):
                        G("tensor_tensor", [b_wc[wi], o["b_gbc"]], [b_wc[wi]], out=wdc[wi][:, k, :], in0=wdc[wi][:, k, :], in1=o["gbc"][:], op=ALU.mult)
                    first = (e == 0 and fc == 0)
                    for tg in range(SGT // 4):
                        hTg = hTa[:, :, tg * 512:(tg + 1) * 512]
                        for sc_ in range(4):
                            for k in range(8):
                                MM(o["pA"][:, :], wgc[wi][:, k, sc_ * 128:(sc_ + 1) * 128], hTg[:, k, :], k == 0, [b_wc[wi], b_hTa[tg]], [o["bpA"]])
                            for k in range(8):
                                MM(o["pB"][:, :], wuc[wi][:, k, sc_ * 128:(sc_ + 1) * 128], hTg[:, k, :], k == 0, [b_wc[wi], b_hTa[tg]], [o["bpB"]])
                            si = sc_ % 2
                            A(sg[si][:], o["pA"][:, :], AF.Silu, [o["bpA"]], [b_sg[si]])
                            V("tensor_tensor", [b_sg[si], o["bpB"]], [b_hid], out=hid[:, sc_, :], in0=sg[si][:], in1=o["pB"][:, :], op=ALU.mult)
                        for tl in range(4):
                            tt = tg * 4 + tl
                            for hf in range(2):
                                pd, bpd = o["pD"][hf], o["bpD"][hf]
                                for sc_ in range(4):
                                    MM(pd[:, :], hid[:, sc_, tl * 128:(tl + 1) * 128], wdc[wi][:, sc_, hf * 512:(hf + 1) * 512], sc_ == 0, [b_hid, b_wc[wi]], [bpd])
                                if first:
                                    V("tensor_scalar", [bpd, b_gt], [b_acc[tt]], out=accs[:, tt, hf * 512:(hf + 1) * 512], in0=pd[:, :],
                                      scalar1=gates[:, tt, e:e + 1], scalar2=None, op0=ALU.mult)
                                else:
                                    V("scalar_tensor_tensor", [bpd, b_gt, b_acc[tt]], [b_acc[tt]], out=accs[:, tt, hf * 512:(hf + 1) * 512], in0=pd[:, :],
                                      scalar=gates[:, tt, e:e + 1], in1=accs[:, tt, hf * 512:(hf + 1) * 512], op0=ALU.mult, op1=ALU.add)
            for tt in range(SGT):
                t = sgi * SGT + tt
                xi = nx % 2; nx += 1
                LD(xt[xi][:], xin[t * 128:(t + 1) * 128, :], [b_xt[xi]], R=[bxin[t]])
                V("scalar_tensor_tensor", [b_xt[xi], b_acc[tt]], [b_acc[tt]], out=accs[:, tt, :], in0=xt[xi][:], scalar=ALPHA, in1=accs[:, tt, :],
                  op0=ALU.mult, op1=ALU.add)
                self.ffn_finish_tile(o, accs[:, tt, :], b_acc[tt], xout[t * 128:(t + 1) * 128, :], bxout[t], is_out)
        S.barrier()


_PROG = {}
IN_KEYS = ["w_ada", "b_ada", "w_in", "nsa_cmp_pos", "nsa_cmp_w1", "nsa_cmp_w2", "dsa_kv_norm", "dsa_w_uk", "dsa_w_uv",
           "sgu_norm_g", "sgu_norm_b", "sgu_w", "sgu_b", "w_out", "ln1_g", "ln1_b", "ln2_g", "ln2_b",
           "ffn_w_gate", "ffn_w_up", "ffn_w_down", "moe_w_router", "moe_b_router", "moe_w_gate", "moe_w_up", "moe_w_down"]


def make_in_maps(inputs, ncores):
    shared = {}
    for k in IN_KEYS:
        a = np.ascontiguousarray(np.asarray(inputs[k], dtype=np.float32))
        if k in ("sgu_norm_g", "sgu_norm_b"):
            a = a.reshape(a.shape[0], -1)
        shared[k] = a
    x = np.asarray(inputs["x"], dtype=np.float32)
    c = np.asarray(inputs["c"], dtype=np.float32)
    pos = np.asarray(inputs["positions"], dtype=np.int32)
    maps = []
    for b in range(ncores):
        m = dict(shared)
        m["x"] = np.ascontiguousarray(x[b])
        m["c"] = np.ascontiguousarray(c[b:b + 1])
        m["pos"] = np.ascontiguousarray(pos[b:b + 1])
        maps.append(m)
    return maps


def kernel(**inputs):
    x = np.asarray(inputs["x"])
    Bn, SL, _ = x.shape
    key = (SL,)
    if key not in _PROG:
        _PROG[key] = Prog(SL)
    prog = _PROG[key]
    maps = make_in_maps(inputs, Bn)
    res = run_bass_kernel_spmd(prog.nc, maps, core_ids=list(range(Bn)))
    return np.stack([np.asarray(r["y"], dtype=np.float32) for r in res.results], axis=0)
```

```python
import math
import numpy as np
from contextlib import ExitStack
import concourse.bass as bass
import concourse.mybir as mybir
from concourse.bass_utils import run_bass_kernel_spmd

F32 = mybir.dt.float32
BF16 = mybir.dt.bfloat16
I32 = mybir.dt.int32
AF = mybir.ActivationFunctionType
ALU = mybir.AluOpType
AX = mybir.AxisListType

D = 1024
DEPTH = 2
ALPHA = (2 * DEPTH) ** 0.25
Q0, KV0, G0, DQ0, CKV0, IQ0, IK0, IW0, SGU0, INW = 0, 384, 1152, 1170, 1554, 1682, 1810, 1842, 1846, 2358
D_FF = 2816
E_FF = 3584
NEXP = 8
NEG = -30000.0
FILL = -1.0e30
NBIS = 16


class Buf:
    __slots__ = ("name", "w", "r", "excl")

    def __init__(self, name="", excl=False):
        self.name = name
        self.w = None
        self.r = {}
        self.excl = excl


def PBuf():
    return Buf("psum", True)


class Eng:
    def __init__(self, key, eng, is_pe=False):
        self.key, self.eng, self.is_pe = key, eng, is_pe
        self.sem = None
        self.count = 0
        self.seen = {}
        self.epoch = 0
        self.dma_sems, self.dma_vals, self.dma_rr = [], [], 0
        self.n_inst = 0
        self.n_wait = 0


class Sched:
    EPOCH = 30000

    def __init__(self, nc, stack, n_dma_sems=10):
        self.nc, self.stack = nc, stack
        self.pe = Eng("pe", nc.tensor, True)
        self.act = Eng("act", nc.scalar)
        self.dve = Eng("dve", nc.vector)
        self.pool = Eng("pool", nc.gpsimd)
        self.sp = Eng("sp", nc.sync)
        self.engs = [self.pe, self.act, self.dve, self.pool, self.sp]
        self.nsem = 0
        for e in self.engs:
            e.sem = self._newsem(e.key + "_p0")
        for e in (self.sp, self.pool):
            for i in range(n_dma_sems):
                e.dma_sems.append(self._newsem(f"{e.key}_d{i}"))
                e.dma_vals.append(0)
        self.out_tokens = []

    def _newsem(self, name):
        self.nsem += 1
        return self.stack.enter_context(self.nc.semaphore(name))

    def _wait(self, E, tok):
        sem, val, src = tok
        if src == E.key and E.is_pe:
            return
        k = id(sem)
        if E.seen.get(k, 0) >= val:
            return
        E.eng.wait_ge(sem, val)
        E.n_wait += 1
        E.seen[k] = val

    def _deps(self, E, reads, writes):
        for b in reads:
            if b.w is not None:
                self._wait(E, b.w)
            if b.excl:
                for k, t in b.r.items():
                    if k != E.key:
                        self._wait(E, t)
        for b in writes:
            if b.w is not None:
                self._wait(E, b.w)
            for t in b.r.values():
                self._wait(E, t)

    def _mark(self, tok, reads, writes):
        for b in reads:
            b.r[tok[2]] = tok
        for b in writes:
            b.w = tok
            b.r = {}

    def op(self, E, fn, reads=(), writes=()):
        self._deps(E, reads, writes)
        if E.count >= self.EPOCH:
            E.epoch += 1
            E.sem = self._newsem(f"{E.key}_p{E.epoch}")
            E.count = 0
        inst = fn(E.eng)
        E.count += 1
        E.n_inst += 1
        inst.then_inc(E.sem, 1)
        tok = (E.sem, E.count, E.key)
        self._mark(tok, reads, writes)
        return tok

    def dma(self, Q, out, in_, reads=(), writes=(), is_output=False, **kw):
        self._deps(Q, reads, writes)
        i = Q.dma_rr
        Q.dma_rr = (Q.dma_rr + 1) % len(Q.dma_sems)
        sem = Q.dma_sems[i]
        if Q.dma_vals[i] > 0:
            self._wait(Q, (sem, Q.dma_vals[i], "dma"))
        inst = Q.eng.dma_start(out=out, in_=in_, **kw)
        Q.dma_vals[i] += 16
        inst.then_inc(sem, 16)
        Q.n_inst += 1
        tok = (sem, Q.dma_vals[i], "dma%s%d" % (Q.key, i))
        self._mark(tok, reads, writes)
        if is_output:
            self.out_tokens.append(tok)
        return tok

    def all_tokens(self):
        toks = []
        for Q in (self.sp, self.pool):
            for i, sem in enumerate(Q.dma_sems):
                if Q.dma_vals[i] > 0:
                    toks.append((sem, Q.dma_vals[i], "dma"))
        for e in self.engs:
            if e.count > 0:
                toks.append((e.sem, e.count, e.key + "_bar"))
        return toks

    def barrier(self):
        toks = self.all_tokens()
        for E in self.engs:
            for t in toks:
                if t[2] == E.key + "_bar":
                    continue
                self._wait(E, t)

    def finish(self):
        for t in self.all_tokens():
            self._wait(self.sp, t)


class Prog:
    def __init__(self, S_len, n_layers=DEPTH, stop_after=None, debug=False):
        self.debug = debug
        self.KTOP = min(256, S_len // 4)
        self.SL = S_len
        self.NT = S_len // 128
        self.NG = S_len // 512
        self.n_layers = n_layers
        self.stop_after = stop_after
        self.nc = bass.Bass("TRN2", target_bir_lowering=False)
        self.build()

    def sb(self, st, name, shape, dt):
        self._uid = getattr(self, "_uid", 0) + 1
        return st.enter_context(self.nc.sbuf_tensor(f"{name}_{self._uid}", shape, dt))

    def ps(self, st, name, shape, dt):
        self._uid = getattr(self, "_uid", 0) + 1
        return st.enter_context(self.nc.psum_tensor(f"{name}_{self._uid}", shape, dt))

    def MM(self, out, lhsT, rhs, start, R, W, stop=True):
        self.S.op(self.S.pe, lambda e: e.matmul(out, lhsT=lhsT, rhs=rhs, start=start, stop=stop,
                                                skip_group_check=True), R, W)

    def TR(self, out, in_, ident, R, W):
        self.S.op(self.S.pe, lambda e: e.transpose(out=out, in_=in_, identity=ident), R, W)

    def A(self, out, in_, func, R, W, **kw):
        self.S.op(self.S.act, lambda e: e.activation(out=out, in_=in_, func=func, **kw), R, W)

    def V(self, name, R, W, **kw):
        self.S.op(self.S.dve, lambda e: getattr(e, name)(**kw), R, W)

    def G(self, name, R, W, **kw):
        if name == "affine_select" and isinstance(kw.get("fill"), (int, float)) and kw["fill"] != 0.0:
            regs = self.__dict__.setdefault("_fillregs", {})
            v = float(kw["fill"])
            if v not in regs:
                regs[v] = self.nc.gpsimd.to_reg(v)
            kw["fill"] = regs[v]
        self.S.op(self.S.pool, lambda e: getattr(e, name)(**kw), R, W)

    def LD(self, out, in_, W, R=(), q=None, **kw):
        self.S.dma(q or self.S.sp, out, in_, reads=R, writes=W, **kw)

    def dram_in(self, name, shape, dt=F32):
        return self.nc.dram_tensor(name, list(shape), dt, kind="ExternalInput").ap()

    def build(self):
        nc = self.nc
        SL, NT, NG = self.SL, self.NT, self.NG
        I = {}
        I["x"] = self.dram_in("x", [SL, D])
        I["c"] = self.dram_in("c", [1, D])
        I["pos"] = self.dram_in("pos", [1, SL], I32)
        I["w_ada"] = self.dram_in("w_ada", [DEPTH, D, 6 * D])
        I["b_ada"] = self.dram_in("b_ada", [DEPTH, 6 * D])
        I["w_in"] = self.dram_in("w_in", [DEPTH, D, INW])
        I["cmp_pos"] = self.dram_in("nsa_cmp_pos", [DEPTH, 2, 32, 64])
        I["cmp_w1"] = self.dram_in("nsa_cmp_w1", [DEPTH, 2, 32, 64, 64])
        I["cmp_w2"] = self.dram_in("nsa_cmp_w2", [DEPTH, 2, 64, 64])
        I["kvn"] = self.dram_in("dsa_kv_norm", [DEPTH, 128])
        I["w_uk"] = self.dram_in("dsa_w_uk", [DEPTH, 128, 64])
        I["w_uv"] = self.dram_in("dsa_w_uv", [DEPTH, 128, 64])
        I["sgu_g"] = self.dram_in("sgu_norm_g", [DEPTH, 256])
        I["sgu_b"] = self.dram_in("sgu_norm_b", [DEPTH, 256])
        I["sgu_w"] = self.dram_in("sgu_w", [DEPTH, 4, 128, 128])
        I["sgu_bs"] = self.dram_in("sgu_b", [DEPTH, 4, 128])
        I["w_out"] = self.dram_in("w_out", [DEPTH, D, D])
        for n in ("ln1_g", "ln1_b", "ln2_g", "ln2_b"):
            I[n] = self.dram_in(n, [DEPTH, D])
        I["ffn_wg"] = self.dram_in("ffn_w_gate", [1, D, D_FF])
        I["ffn_wu"] = self.dram_in("ffn_w_up", [1, D, D_FF])
        I["ffn_wd"] = self.dram_in("ffn_w_down", [1, D_FF, D])
        I["moe_wr"] = self.dram_in("moe_w_router", [1, D, NEXP])
        I["moe_br"] = self.dram_in("moe_b_router", [1, NEXP])
        I["moe_wg"] = self.dram_in("moe_w_gate", [1, NEXP, D, E_FF])
        I["moe_wu"] = self.dram_in("moe_w_up", [1, NEXP, D, E_FF])
        I["moe_wd"] = self.dram_in("moe_w_down", [1, NEXP, E_FF, D])
        self.I = I
        self.y = nc.dram_tensor("y", [SL, D], F32, kind="ExternalOutput").ap()
        if self.debug:
            self.dbg = nc.dram_tensor("dbg", [SL, D], F32, kind="ExternalOutput").ap()
        self.rope_d = nc.dram_tensor("rope_d", [4, 128, SL], F32, kind="Internal").ap()
        self.xa = nc.dram_tensor("xa", [SL, D], F32, kind="Internal").ap()
        self.xb = nc.dram_tensor("xb", [SL, D], F32, kind="Internal").ap()
        self.b_rope = Buf("rope_d")
        self.b_xa = [Buf() for _ in range(NT)]
        self.b_xb = [Buf() for _ in range(NT)]
        self.b_y = [Buf() for _ in range(NT)]

        with ExitStack() as st0:
            self.S = Sched(nc, st0)
            self.setup_consts(st0)
            self.setup_mod(st0)
            self.setup_rope()
            xin, bxin = I["x"], [Buf() for _ in range(NT)]
            for l in range(self.n_layers):
                last = (l == self.n_layers - 1)
                self.S.barrier()
                with ExitStack() as st:
                    self.mixer(st, l, xin, bxin, self.xa, self.b_xa)
                if self.stop_after == ("mix", l):
                    self.copy_out(self.xa, self.b_xa)
                    break
                self.S.barrier()
                dst, bdst = (self.y, self.b_y) if last else (self.xb, self.b_xb)
                with ExitStack() as st:
                    if l % 2 == 0:
                        self.ffn_dense(st, l, self.xa, self.b_xa, dst, bdst, is_out=last)
                    else:
                        self.ffn_moe(st, l, self.xa, self.b_xa, dst, bdst, is_out=last)
                if (not last) and self.stop_after == ("ffn", l):
                    self.copy_out(self.xb, self.b_xb)
                    break
                xin, bxin = self.xb, self.b_xb
            self.S.finish()
            self.stats = {e.key: (e.n_inst, e.n_wait) for e in self.S.engs}

    def copy_out(self, src, bsrc):
        self.S.barrier()
        for t in range(self.NT):
            self.S.dma(self.S.sp, self.y[t * 128:(t + 1) * 128, :], src[t * 128:(t + 1) * 128, :],
                       reads=[bsrc[t]], writes=[self.b_y[t]], is_output=True)

    def setup_consts(self, st):
        C = self.C = {}
        B = self.B = {}

        def mk(name, shape, dt):
            C[name] = self.sb(st, name, shape, dt)
            B[name] = Buf(name)
            return C[name], B[name]

        idf, b = mk("ident_f", [128, 128], F32)
        self.G("memset", [], [b], ap=idf[:], constant=0.0)
        self.G("affine_select", [b], [b], out=idf[:], in_=idf[:], pattern=[[-1, 128]],
               compare_op=ALU.not_equal, fill=1.0, base=0, channel_multiplier=1)
        idb, b2 = mk("ident_b", [128, 128], BF16)
        self.V("tensor_copy", [b], [b2], out=idb[:], in_=idf[:])
        ones, b3 = mk("ones", [128, 128], F32)
        self.G("memset", [], [b3], ap=ones[:], constant=1.0)
        onesb, b3b = mk("onesb", [128, 128], BF16)
        self.G("memset", [], [b3b], ap=onesb[:], constant=1.0)
        zf, bz = mk("ztmp", [128, 384], F32)
        ntc, b4 = mk("negtri_c", [128, 384], BF16)
        self.G("memset", [], [bz], ap=zf[:], constant=0.0)
        self.G("affine_select", [bz], [bz], out=zf[:], in_=zf[:], pattern=[[0, 3], [1, 128]],
               compare_op=ALU.is_ge, fill=NEG, base=0, channel_multiplier=-1)
        self.V("tensor_copy", [bz], [b4], out=ntc[:], in_=zf[:])
        ntw, b5 = mk("negtri_w", [128, 384], BF16)
        self.G("memset", [b4], [bz], ap=zf[:], constant=0.0)
        self.G("affine_select", [bz], [bz], out=zf[:], in_=zf[:], pattern=[[0, 3], [-1, 128]],
               compare_op=ALU.is_ge, fill=NEG, base=-1, channel_multiplier=1)
        self.V("tensor_copy", [bz], [b5], out=ntw[:], in_=zf[:])
        rel, b6 = mk("rel", [128, 128], F32)
        self.G("memset", [], [b6], ap=rel[:], constant=0.0)
        for half in range(2):
            sl = rel[half * 64:(half + 1) * 64, :]
            self.G("memset", [b6], [b6], ap=rel[half * 64:(half + 1) * 64, 62 + half:64 + half], constant=1.0e4)
            if 64 + half < 128:
                self.G("memset", [b6], [b6], ap=rel[half * 64:(half + 1) * 64, 64 + half:128], constant=FILL)
        col0, b7 = mk("col0", [128, 64], F32)
        self.G("memset", [], [b7], ap=col0[:], constant=0.0)
        self.G("memset", [b7], [b7], ap=col0[:, 0:1], constant=1.0e4)
        ovf, b8 = mk("ovf", [128, 2, 64], F32)
        ov, b9 = mk("overlap", [128, 2, 64], BF16)
        self.G("memset", [], [b8], ap=ovf[:], constant=1.0)
        for nt in range(2):
            self.G("affine_select", [b8], [b8], out=ovf[:, nt, :], in_=ovf[:, nt, :], pattern=[[-4, 64]],
                   compare_op=ALU.is_ge, fill=0.0, base=128 * nt + 1, channel_multiplier=1)
            self.G("affine_select", [b8], [b8], out=ovf[:, nt, :], in_=ovf[:, nt, :], pattern=[[4, 64]],
                   compare_op=ALU.is_ge, fill=0.0, base=3 - 128 * nt, channel_multiplier=-1)
        self.V("tensor_copy", [b8], [b9], out=ov[:], in_=ovf[:])
        for nm, dh in (("rot_h", 64), ("rot_i", 32)):
            rm, brm = mk(nm, [128, 128], F32)
            hh = dh // 2
            self.G("memset", [], [brm], ap=rm[:], constant=0.0)
            for c0 in range(0, 128, dh):
                self.G("affine_select", [brm], [brm], out=rm[:, c0:c0 + hh], in_=rm[:, c0:c0 + hh], pattern=[[-1, hh]],
                       compare_op=ALU.not_equal, fill=-1.0, base=-(c0 + hh), channel_multiplier=1)
                self.G("affine_select", [brm], [brm], out=rm[:, c0 + hh:c0 + dh], in_=rm[:, c0 + hh:c0 + dh], pattern=[[-1, hh]],
                       compare_op=ALU.not_equal, fill=1.0, base=-c0, channel_multiplier=1)
        p2, b10 = mk("pow2", [128, NBIS], F32)
        for k in range(NBIS):
            self.G("memset", [b10], [b10], ap=p2[:, k:k + 1], constant=2.0 ** -(k + 1))

    def setup_mod(self, st):
        nc, S, C, B, I = self.nc, self.S, self.C, self.B, self.I
        modcol = self.sb(st, "modcol", [128, DEPTH, 4, 8], F32)
        self.gate_d = nc.dram_tensor("gate_d", [DEPTH * 2, D], F32, kind="Internal").ap()
        self.b_gate = Buf("gate_d")
        self.modcol = modcol
        self.b_mod = Buf("mod")
        with ExitStack() as s2:
            crow = self.sb(s2, "crow", [1, D], F32); b_crow = Buf()
            ccol = self.sb(s2, "ccol", [128, 8], F32); b_ccol = Buf()
            modrow = self.sb(s2, "modrow", [1, 6 * D], F32); b_modrow = Buf()
            brow = self.sb(s2, "brow", [1, 6 * D], F32); b_brow = Buf()
            wa = [self.sb(s2, f"wa{i}", [128, 8, 512], F32) for i in range(2)]
            b_wa = [Buf(), Buf()]
            pc = self.ps(s2, "pc", [128, 512], F32); b_pc = PBuf()
            pr = [self.ps(s2, f"pr{i}", [128, 512], F32) for i in range(2)]
            b_pr = [PBuf(), PBuf()]
            self.LD(crow[:], I["c"], [b_crow])
            for k in range(8):
                self.MM(pc[:, k:k + 1], crow[0:1, k * 128:(k + 1) * 128], C["ones"][0:1, 0:1], True,
                        [b_crow, B["ones"]], [b_pc])
            self.A(ccol[:], pc[:, 0:8], AF.Silu, [b_pc], [b_ccol])
            cnt = 0
            for l in range(self.n_layers):
                self.LD(brow[:], I["b_ada"][l:l + 1, :], [b_brow])
                for j in range(12):
                    w = wa[cnt % 2]; bw = b_wa[cnt % 2]; p = pr[cnt % 2]; bp = b_pr[cnt % 2]
                    cnt += 1
                    self.LD(w[:], I["w_ada"][l, :, j * 512:(j + 1) * 512].rearrange("(k p) n -> p k n", p=128), [bw])
                    for k in range(8):
                        self.MM(p[0:1, :], ccol[:, k:k + 1], w[:, k, :], k == 0, [b_ccol, bw], [bp])
                    self.V("tensor_tensor", [bp, b_brow], [b_modrow], out=modrow[0:1, j * 512:(j + 1) * 512],
                           in0=p[0:1, :], in1=brow[0:1, j * 512:(j + 1) * 512], op=ALU.add)
                for vi, seg in enumerate((0, 1, 3, 4)):
                    for k in range(8):
                        self.MM(pc[:, 16 + vi * 8 + k:16 + vi * 8 + k + 1],
                                modrow[0:1, seg * D + k * 128:seg * D + (k + 1) * 128], C["ones"][0:1, 0:1], True,
                                [b_modrow, B["ones"]], [b_pc])
                for vi in range(4):
                    if vi % 2 == 0:
                        self.V("tensor_copy", [b_pc], [self.b_mod], out=modcol[:, l, vi, :], in_=pc[:, 16 + vi * 8:24 + vi * 8])
                    else:
                        self.V("tensor_scalar", [b_pc], [self.b_mod], out=modcol[:, l, vi, :], in0=pc[:, 16 + vi * 8:24 + vi * 8],
                               scalar1=1.0, scalar2=None, op0=ALU.add)
                self.LD(self.gate_d[2 * l:2 * l + 1, :], modrow[0:1, 2 * D:3 * D], [self.b_gate], R=[b_modrow])
                self.LD(self.gate_d[2 * l + 1:2 * l + 2, :], modrow[0:1, 5 * D:6 * D], [self.b_gate], R=[b_modrow])
            S.barrier()

    def setup_rope(self):
        S, I = self.S, self.I
        with ExitStack() as st:
            pidx = self.sb(st, "pidx", [128, 1], I32); b_p = Buf()
            pm = self.sb(st, "pm", [128, 2], I32); b_pm = Buf()
            pmf = self.sb(st, "pmf", [128, 2], F32); b_pmf = Buf()
            inv = self.sb(st, "inv", [128, 2], F32); b_inv = Buf()
            self.G("iota", [], [b_p], out=pidx[:], pattern=[[0, 1]], base=0, channel_multiplier=1)
            self.V("tensor_single_scalar", [b_p], [b_pm], out=pm[:, 0:1], in_=pidx[:], scalar=31, op=ALU.bitwise_and)
            self.V("tensor_single_scalar", [b_p], [b_pm], out=pm[:, 1:2], in_=pidx[:], scalar=15, op=ALU.bitwise_and)
            self.V("tensor_copy", [b_pm], [b_pmf], out=pmf[:], in_=pm[:])
            lnth = math.log(10000.0)
            self.A(inv[:, 0:1], pmf[:, 0:1], AF.Exp, [b_pmf], [b_inv], scale=-lnth / 32.0)
            self.A(inv[:, 1:2], pmf[:, 1:2], AF.Exp, [b_pmf], [b_inv], scale=-lnth / 16.0)
            posi = self.sb(st, "posi", [128, 512], I32); b_posi = Buf()
            posf = self.sb(st, "posf", [128, 512], F32); b_posf = Buf()
            u = self.sb(st, "ru", [128, 512], F32); b_u = Buf()
            ki = self.sb(st, "rki", [128, 512], I32); b_ki = Buf()
            kf = self.sb(st, "rkf", [128, 512], F32); b_kf = Buf()
            t1 = self.sb(st, "rt1", [128, 512], F32); b_t1 = Buf()
            tab = [self.sb(st, f"rtab{i}", [128, 512], F32) for i in range(2)]
            b_tab = [Buf(), Buf()]
            n = 0
            for c in range(self.NG):
                self.LD(posi[:], I["pos"][0:1, c * 512:(c + 1) * 512].to_broadcast([128, 512]), [b_posi])
                self.V("tensor_copy", [b_posi], [b_posf], out=posf[:], in_=posi[:])
                for ti in range(4):
                    fi = ti // 2
                    off = 0.25 if ti % 2 == 0 else 0.0
                    self.V("tensor_scalar", [b_posf, b_inv], [b_u], out=u[:], in0=posf[:], scalar1=inv[:, fi:fi + 1],
                           scalar2=1.0 / (2 * math.pi), op0=ALU.mult, op1=ALU.mult)
                    if off:
                        self.V("tensor_scalar", [b_u], [b_u], out=u[:], in0=u[:], scalar1=off, scalar2=None, op0=ALU.add)
                    self.V("tensor_copy", [b_u], [b_ki], out=ki[:], in_=u[:])
                    self.V("tensor_copy", [b_ki], [b_kf], out=kf[:], in_=ki[:])
                    self.V("tensor_tensor", [b_u, b_kf], [b_u], out=u[:], in0=u[:], in1=kf[:], op=ALU.subtract)
                    self.V("tensor_single_scalar", [b_u], [b_t1], out=t1[:], in_=u[:], scalar=0.5, op=ALU.is_gt)
                    self.V("tensor_tensor", [b_u, b_t1], [b_u], out=u[:], in0=u[:], in1=t1[:], op=ALU.subtract)
                    self.V("tensor_single_scalar", [b_u], [b_t1], out=t1[:], in_=u[:], scalar=-0.5, op=ALU.is_lt)
                    self.V("tensor_tensor", [b_u, b_t1], [b_u], out=u[:], in0=u[:], in1=t1[:], op=ALU.add)
                    tb = tab[n % 2]; bt = b_tab[n % 2]; n += 1
                    self.A(tb[:], u[:], AF.Sin, [b_u], [bt], scale=6.28318)
                    self.LD(self.rope_d[ti, :, c * 512:(c + 1) * 512], tb[:], [self.b_rope], R=[bt])
            S.barrier()

    def ln_stats(self, xt, b_x, mv, b_mv, rstd, stats, b_st, eps=1e-5):
        for c2 in range(2):
            self.V("bn_stats", [b_x], [b_st], out=stats[:, c2, :], in_=xt[:, c2 * 512:(c2 + 1) * 512])
        self.V("bn_aggr", [b_st], [b_mv], out=mv[:, 0:2], in_=stats[:].rearrange("p a b -> p (a b)"))
        self.V("tensor_scalar", [b_mv], [b_mv], out=rstd, in0=mv[:, 1:2], scalar1=eps, scalar2=None, op0=ALU.add)
        self.A(rstd, rstd, AF.Sqrt, [b_mv], [b_mv])
        self.V("reciprocal", [b_mv], [b_mv], out=rstd, in_=rstd)

    def mixer(self, st, l, xin, bxin, xout, bxout):
        nc, S, C, B, I = self.nc, self.S, self.C, self.B, self.I
        SL, NT, NG = self.SL, self.NT, self.NG
        sb = lambda n, s, d: self.sb(st, n, s, d)
        MM, TR, A, V, G, LD = self.MM, self.TR, self.A, self.V, self.G, self.LD
        PQ = S.pool
        SA = max(SL, 4096)

        pA = self.ps(st, "pA", [128, 512], F32); b_pA = PBuf()
        pB = self.ps(st, "pB", [128, 512], F32); b_pB = PBuf()
        pS = [self.ps(st, f"pS{i}", [128, 512], F32) for i in range(2)]; b_pS = [PBuf(), PBuf()]
        pM = self.ps(st, "pM", [128, 1024], BF16); _bpm = PBuf(); b_pM = [_bpm, _bpm]
        pO = [self.ps(st, f"pO{i}", [128, 512], F32) for i in range(3)]; b_pO = [PBuf(), PBuf(), PBuf()]

        acc = sb("acc", [128, SA], F32); b_acc = Buf()
        dmask = sb("dmask", [128, SA], BF16); b_dm = Buf()
        win = sb("win", [128, 8, INW], BF16); b_win = Buf()
        for (a, b) in ((384, 1179), (1179, INW)):
            LD(win[:, :, a:b], I["w_in"][l, :, a:b].rearrange("(k p) n -> p k n", p=128), [b_win], q=PQ)
        for k in range(8):
            for r in range(3):
                LD(win[:, k, r * 128:(r + 1) * 128].rearrange("p (g d) -> p g d", g=2),
                   I["w_in"][l, k * 128:(k + 1) * 128, 0:384].rearrange("p (g r d) -> p g r d", g=2, r=3)[:, :, r, :], [b_win], q=PQ)
        ik4 = sb("ik4", [128, 8, 128], BF16); b_ik4 = Buf()
        for k in range(8):
            for r4 in range(4):
                (V if r4 % 2 == 0 else G)("tensor_copy", [b_win], [b_ik4], out=ik4[:, k, r4 * 32:(r4 + 1) * 32], in_=win[:, k, IK0:IK0 + 32])
        wout = sb("wout", [128, 8, D], BF16); b_wout = Buf()
        LD(wout[:], I["w_out"][l].rearrange("(k p) n -> p k n", p=128), [b_wout], q=PQ)
        lng = sb("lng", [128, D], F32); lnb = sb("lnb", [128, D], F32); b_ln = Buf()
        LD(lng[:], I["ln1_g"][l:l + 1, :].to_broadcast([128, D]), [b_ln])
        LD(lnb[:], I["ln1_b"][l:l + 1, :].to_broadcast([128, D]), [b_ln])
        w1 = sb("cw1", [128, 2, 32, 64], BF16); w2 = sb("cw2", [128, 2, 64], BF16); b_cw = Buf()
        for g in range(2):
            LD(w1[64 * g:64 * g + 64], I["cmp_w1"][l].rearrange("j l d e -> d j l e"), [b_cw], q=PQ)
            LD(w2[64 * g:64 * g + 64], I["cmp_w2"][l].rearrange("j e f -> e j f"), [b_cw], q=PQ)
        cbias = sb("cbias", [128, 2], F32); b_cb = Buf()
        wuk = sb("wuk", [128, 128], BF16); wuv = sb("wuv", [128, 64], BF16); b_wu = Buf()
        for hf in range(2):
            LD(wuk[:, hf * 64:(hf + 1) * 64], I["w_uk"][l], [b_wu], q=PQ)
        LD(wuv[:], I["w_uv"][l], [b_wu], q=PQ)
        kvg = sb("kvg", [128, 128], F32); b_kvg = Buf()
        LD(kvg[:], I["kvn"][l:l + 1, :].to_broadcast([128, 128]), [b_kvg])
        sgg = sb("sgg", [128, 256], F32); sgb = sb("sgb", [128, 256], F32); b_sg = Buf()
        LD(sgg[:], I["sgu_g"][l:l + 1, :].to_broadcast([128, 256]), [b_sg])
        LD(sgb[:], I["sgu_b"][l:l + 1, :].to_broadcast([128, 256]), [b_sg])
        wsT = sb("wsT", [128, 4, 128], BF16); b_ws = Buf()
        bs = sb("sgbs", [128, 4], F32); b_bs = Buf()
        with ExitStack() as s2:
            gbc = self.sb(s2, "gbc", [128, D], F32); b_gbc = Buf()
            LD(gbc[:], self.gate_d[2 * l:2 * l + 1, :].to_broadcast([128, D]), [b_gbc], R=[self.b_gate])
            for k in range(8):
                V("tensor_tensor", [b_wout, b_gbc], [b_wout], out=wout[:, k, :], in0=wout[:, k, :], in1=gbc[:], op=ALU.mult)
            w1n = self.sb(s2, "w1n", [32, 2, 64, 64], F32); posn = self.sb(s2, "posn", [32, 2, 64], F32); b_n = Buf()
            LD(w1n[:], I["cmp_w1"][l].rearrange("j l d e -> l j d e"), [b_n])
            LD(posn[:], I["cmp_pos"][l].rearrange("j l d -> l j d"), [b_n])
            for j in range(2):
                for g in range(2):
                    for d in range(64):
                        MM(pA[64 * g:64 * g + 64, j:j + 1], w1n[:, j, d, :], posn[:, j, d:d + 1], d == 0, [b_n], [b_pA])
            V("tensor_copy", [b_pA], [b_cb], out=cbias[:], in_=pA[:, 0:2])
            wsn = self.sb(s2, "wsn", [128, 4, 128], F32); b_wsn = Buf()
            wsf = self.sb(s2, "wsf", [128, 4, 128], F32); b_wsf = Buf()
            bsn = self.sb(s2, "bsn", [4, 128], F32); b_bsn = Buf()
            LD(wsn[:], I["sgu_w"][l].rearrange("g t s -> t g s"), [b_wsn])
            LD(bsn[:], I["sgu_bs"][l], [b_bsn])
            for g in range(4):
                TR(pB[:, g * 128:(g + 1) * 128], wsn[:, g, :], C["ident_f"][:], [b_wsn, B["ident_f"]], [b_pB])
            V("tensor_copy", [b_pB], [b_wsf], out=wsf[:].rearrange("p g t -> p (g t)"), in_=pB[:, :])
            G("affine_select", [b_wsf], [b_wsf], out=wsf[:], in_=wsf[:], pattern=[[0, 4], [1, 128]],
              compare_op=ALU.is_ge, fill=0.0, base=0, channel_multiplier=-1)
            V("tensor_copy", [b_wsf], [b_ws], out=wsT[:], in_=wsf[:])
            TR(pS[0][:, 0:4], bsn[:, :], C["ident_f"][0:4, 0:4], [b_bsn, B["ident_f"]], [b_pS[0]])
            V("tensor_copy", [b_pS[0]], [b_bs], out=bs[:], in_=pS[0][:, 0:4])
            S.barrier()

        xcT = sb("xcT", [128, 2, 528], BF16); b_xcT = Buf()
        kselT = sb("kselT", [128, SL], BF16); kwinT = sb("kwinT", [128, 1024], BF16)
        kdsT = sb("kdsT", [128, SL], BF16); ikT = sb("ikT", [128, SL], BF16)
        vsel = sb("vsel", [128, NT, 2, 65], BF16); vwin = sb("vwin", [128, 8, 2, 65], BF16)
        vds = sb("vds", [128, NT, 65], BF16)
        b_kv = [Buf() for _ in range(NG)]
        b_kw = [Buf(), Buf()]
        b_kv0 = Buf()
        G("memset", [], [b_kv0], ap=vsel[:, :, :, 64:65], constant=1.0)
        G("memset", [], [b_kv0], ap=vwin[:, :, :, 64:65], constant=1.0)
        G("memset", [], [b_kv0], ap=vds[:, :, 64:65], constant=1.0)
        hidT = sb("hidT", [128, 2, 256], BF16); kcT = sb("kcT", [128, 256], BF16)
        vcmp = sb("vcmp", [128, 2, 2, 129], BF16); b_cmp = Buf()
        G("memset", [], [b_cmp], ap=hidT[:], constant=0.0)
        G("memset", [b_cmp], [b_cmp], ap=kcT[:], constant=0.0)
        G("memset", [b_cmp], [b_cmp], ap=vcmp[:], constant=0.0)
        G("memset", [b_cmp], [b_cmp], ap=vcmp[:, :, :, 64:65], constant=1.0)
        for g in range(2):
            G("tensor_copy", [b_cmp, B["overlap"]], [b_cmp], out=vcmp[:, :, g, 65:129], in_=C["overlap"][:])

        xs = [sb("xs0", [128, D], F32)] * 2; _bxs = Buf(); b_xs = [_bxs, _bxs]
        junk = xs[0][:].bitcast(mybir.dt.uint8); b_junk = _bxs
        xr = [sb("xr0", [128, D], F32)] * 2; _bxr = Buf(); b_xr = [_bxr, _bxr]
        stats2 = sb("stats2", [128, 2, 6], F32); b_st2 = Buf()
        mv2 = sb("mv2", [128, 4], F32); b_mv2 = Buf()
        stats = sb("stats", [128, 2, 6], F32); b_st = Buf()
        mv = sb("mv", [128, 4], F32); b_mv = Buf()
        mv3 = sb("mv3", [128, 2], F32); b_mv3 = Buf()
        rt = acc[:, 2048:4096].rearrange("p (a t) -> p a t", a=4); b_rt = b_acc
        hT = dmask[:, 0:4096].rearrange("p (k t) -> p k t", k=8); b_hT = b_dm
        qf = sb("qf", [128, 512], F32); b_qf = Buf()
        rl = [sb(f"rl{i}", [128, 512], F32) for i in range(2)]; b_rl = [Buf(), Buf()]
        t1, b_t1, t2, b_t2 = rl[0], b_rl[0], rl[1], b_rl[1]
        qrT = sb("qrT", [128, 3, 512], BF16); qropT = sb("qropT", [128, 3, 512], BF16)
        dqT = sb("dqT", [128, 3, 512], BF16); iqT = sb("iqT", [64, 2, 512], BF16); b_q = Buf()
        gl = sb("gl", [128, 4, 24], F32); b_gl = Buf()
        ckv = sb("ckv", [128, 128], F32); b_ckv = Buf()
        ckvT = sb("ckvT", [128, 512], BF16); b_ckvT = Buf()
        zt, b_zt = qf, b_qf
        ocs = sb("ocs", [128, 4, 256], BF16); b_ocs = Buf()
        cat = [sb(f"cat{i}", [128, D], BF16) for i in range(2)]; b_cat = [Buf(), Buf()]
        catT = sb("catT", [128, 8, 128], BF16); b_catT = Buf()
        E = [sb(f"E{i}", [128, 768], BF16) for i in range(2)]; b_E = [Buf(), Buf()]
        Pm = [sb(f"Pm{i}", [128, 768], BF16) for i in range(2)]; b_Pm = [Buf(), Buf()]
        sm = sb("sm", [128, 64], F32); b_sm = Buf()
        sm_n = sb("sm_n", [128, 64], F32); b_smn = Buf()
        sm_d = sb("sm_d", [128, 64], F32); b_smd = Buf()
        Ed = [sb(f"Ed{i}", [128, 768], BF16) for i in range(2)]; b_Ed = [Buf(), Buf()]
        b_Eh = [Buf() for _ in range(4)]; b_Ph = [Buf() for _ in range(4)]
        b_Edh = [Buf() for _ in range(4)]; b_Pdh = [Buf(), Buf()]
        Pd = [sb("Pd0", [128, 768], BF16)] * 2; _bpd = Buf(); b_Pd = [_bpd, _bpd]
        oc = sb("oc", [128, 6, 64], F32); b_oc = Buf()
        imp = sb("imp", [128, 6, 64], F32); b_imp = Buf()
        sc = sb("sc", [128, 2, 64], F32); sc2 = sb("sc2", [128, 2, 64], F32); b_sc = Buf()
        m8 = sb("m8", [128, 2, 8], F32); b_m8 = Buf()
        bm = sb("bm", [128, 2, 64], BF16); b_bm = Buf()
        bx = sb("bx", [128, 2, 128], BF16); b_bx = [Buf(), Buf()]
        bst = sb("bst", [128, 8 + NBIS], F32); b_bst = Buf()
        wsg = sb("wsg", [128, 12], F32); b_wsg = Buf()
        sg = sb("sgt", [128, 4, 256], F32); b_sgt = Buf()
        vln = sb("vln", [128, 256], BF16); b_vln = Buf()

        nE = [0]

        def nextE():
            i = nE[0] % 2
            nE[0] += 1
            return i

        nEd = [0]

        def nextEd():
            i_ = nEd[0] % 2
            nEd[0] += 1
            return i_

        nxs = 0
        import os
        MS = float(os.environ.get("MIXSTOP", "9"))
        for gi in range(NG):
            gs = slice(gi * 512, (gi + 1) * 512)
            wsl = slice((gi % 2) * 512, (gi % 2) * 512 + 512)
            KVW = [b_kv[gi]]
            KWW = [b_kw[gi % 2]]
            LD(rt, self.rope_d[:, :, gs].rearrange("a p t -> p a t"), [b_rt], R=[self.b_rope])
            for tl in range(4):
                t = gi * 4 + tl
                xi = nxs % 2; nxs += 1
                LD(xs[xi][:], xin[t * 128:(t + 1) * 128, :], [b_xs[xi]], R=[bxin[t]])
                self.ln_stats(xs[xi], b_xs[xi], mv, b_mv, mv[:, 2:3], stats, b_st)
                V("tensor_scalar", [b_xs[xi], b_mv], [b_xs[xi]], out=xs[xi][:], in0=xs[xi][:], scalar1=mv[:, 0:1], scalar2=mv[:, 2:3],
                  op0=ALU.subtract, op1=ALU.mult)
                for hf in range(2):
                    pp, bpp = (pA, b_pA) if hf == 0 else (pB, b_pB)
                    for k4 in range(4):
                        TR(pp[:, k4 * 128:(k4 + 1) * 128], xs[xi][:, (hf * 4 + k4) * 128:(hf * 4 + k4 + 1) * 128], C["ident_f"][:],
                           [b_xs[xi], B["ident_f"]], [bpp])
                    for k4 in range(4):
                        k = hf * 4 + k4
                        A(hT[:, k, tl * 128:(tl + 1) * 128], pp[:, k4 * 128:(k4 + 1) * 128], AF.Identity, [bpp, self.b_mod], [b_hT],
                          bias=self.modcol[:, l, 0, k:k + 1], scale=self.modcol[:, l, 1, k:k + 1])

            if MS <= 1:
                S.barrier(); return
            def proj_fm(wcols_fn, M, pp, bpp):
                for k in range(8):
                    MM(pp[0:M, :], wcols_fn(k), hT[:, k, :], k == 0, [b_win, b_ik4, b_hT], [bpp])

            def rope_from_pA(dst, M, tab, Rm, RB):
                RO = int(os.environ.get("ROPE_OFF", "0"))
                if RO == 1:
                    A(dst[0], pA[0:M, :], AF.Copy, [b_pA], dst[1]); return
                A(qf[0:M, :], pA[0:M, :], AF.Copy, [b_pA], [b_qf])
                if RO == 2:
                    A(dst[0], pA[0:M, :], AF.Copy, [b_pA], dst[1]); return
                if RO == 3:
                    MM(pB[0:M, :], Rm[0:M, 0:M], qf[0:M, :], True, [RB, b_qf], [b_pB])
                    A(dst[0], pB[0:M, :], AF.Copy, [b_pB], dst[1]); return
                MM(pB[0:M, :], Rm[0:M, 0:M], qf[0:M, :], True, [RB, b_qf], [b_pB])
                V("tensor_tensor", [b_pA, b_rt], [b_t1], out=t1[0:M, :], in0=pA[0:M, :], in1=rt[0:M, tab, :], op=ALU.mult)
                if RO == 4:
                    A(dst[0], t1[0:M, :], AF.Copy, [b_t1], dst[1]); return
                V("tensor_tensor", [b_pB, b_rt], [b_t2], out=t2[0:M, :], in0=pB[0:M, :], in1=rt[0:M, tab + 1, :], op=ALU.mult)
                if RO == 5:
                    A(dst[0], t2[0:M, :], AF.Copy, [b_t2], dst[1]); return
                G("tensor_tensor", [b_t1, b_t2], dst[1], out=dst[0], in0=t1[0:M, :], in1=t2[0:M, :], op=ALU.add)

            def rope_fm(dst, wp, M, tab):
                proj_fm(wp, M, pA, b_pA)
                if tab == 0:
                    rope_from_pA(dst, M, 0, C["rot_h"], B["rot_h"])
                else:
                    rope_from_pA(dst, M, 2, C["rot_i"], B["rot_i"])

            def run_il(gens):
                gens = [g_ for g_ in gens if g_ is not None]
                while gens:
                    for g_ in list(gens):
                        try:
                            next(g_)
                        except StopIteration:
                            gens.remove(g_)

            def g_proj():
                for r in range(3):
                    proj_fm(lambda k: win[:, k, Q0 + r * 128:Q0 + (r + 1) * 128], 128, pA, b_pA)
                    A(qrT[:, r, :], pA[:, :], AF.Copy, [b_pA], [b_q])
                    rope_from_pA((qropT[:, r, :], [b_q]), 128, 0, C["rot_h"], B["rot_h"])
                    yield
                    rope_fm((dqT[:, r, :], [b_q]), lambda k: win[:, k, DQ0 + r * 128:DQ0 + (r + 1) * 128], 128, 0)
                    yield
                rope_fm((kselT[:, gs], KVW), lambda k: win[:, k, KV0 + 256:KV0 + 384], 128, 0)
                yield
                rope_fm((kwinT[:, wsl], KWW), lambda k: win[:, k, KV0 + 512:KV0 + 640], 128, 0)
                yield
                for hh2 in range(2):
                    rope_fm((iqT[:, hh2, :], [b_q]), lambda k: win[:, k, IQ0 + hh2 * 64:IQ0 + hh2 * 64 + 64], 64, 2)
                    yield
                rope_fm((ikT[:, gs], KVW), lambda k: ik4[:, k, :], 128, 2)
                yield
                if gi > 0:
                    G("tensor_copy", [b_xcT], [b_xcT], out=xcT[:, :, 0:16], in_=xcT[:, :, 512:528])
                for j in range(2):
                    proj_fm(lambda k: win[:, k, KV0 + j * 128:KV0 + (j + 1) * 128], 128, pA, b_pA)
                    A(xcT[:, j, 16:528], pA[:, :], AF.Copy, [b_pA], [b_xcT])
                    yield

            def g_tok():
                pT_, bT_ = pO[0], b_pO[0]
                for tl in range(4):
                    t = gi * 4 + tl
                    ts = slice(tl * 128, (tl + 1) * 128)
                    for k in range(8):
                        MM(pT_[:, 0:128], hT[:, k, ts], win[:, k, KV0 + 384:KV0 + 512], k == 0, [b_hT, b_win], [bT_])
                    for k in range(8):
                        MM(pT_[:, 128:256], hT[:, k, ts], win[:, k, KV0 + 640:KV0 + 768], False, [b_hT, b_win], [bT_])
                    for k in range(8):
                        MM(pT_[:, 256:384], hT[:, k, ts], win[:, k, CKV0:CKV0 + 128], False, [b_hT, b_win], [bT_])
                    for k in range(8):
                        MM(pT_[:, 384:402], hT[:, k, ts], win[:, k, G0:G0 + 18], False, [b_hT, b_win], [bT_])
                    for k in range(8):
                        MM(pT_[:, 402:406], hT[:, k, ts], win[:, k, IW0:IW0 + 4], False, [b_hT, b_win], [bT_])
                    yield
                    A(vsel[:, t, :, 0:64], pT_[:, 0:128].rearrange("p (g d) -> p g d", g=2), AF.Copy, [bT_], KVW)
                    A(vwin[:, t % 8, :, 0:64], pT_[:, 128:256].rearrange("p (g d) -> p g d", g=2), AF.Copy, [bT_], KWW)
                    A(gl[:, tl, 0:18], pT_[:, 384:402], AF.Sigmoid, [bT_], [b_gl])
                    V("tensor_scalar", [bT_], [b_gl], out=gl[:, tl, 18:22], in0=pT_[:, 402:406], scalar1=0.5, scalar2=None, op0=ALU.mult)
                    A(ckv[:], pT_[:, 256:384], AF.Square, [bT_], [b_ckv, b_mv3], accum_out=mv3[:, 0:1])
                    V("tensor_scalar", [b_ckv, b_mv3], [b_mv3], out=mv3[:, 0:1], in0=mv3[:, 0:1], scalar1=1.0 / 128, scalar2=1e-6, op0=ALU.mult, op1=ALU.add)
                    A(mv3[:, 0:1], mv3[:, 0:1], AF.Sqrt, [b_mv3], [b_mv3])
                    V("reciprocal", [b_mv3], [b_mv3], out=mv3[:, 0:1], in_=mv3[:, 0:1])
                    V("scalar_tensor_tensor", [bT_, b_mv3, b_kvg], [b_ckv], out=ckv[:], in0=pT_[:, 256:384], scalar=mv3[:, 0:1], in1=kvg[:],
                      op0=ALU.mult, op1=ALU.mult)
                    yield
                    TR(pO[1][:, 0:128], ckv[:], C["ident_f"][:], [b_ckv, B["ident_f"]], [b_pO[1]])
                    A(ckvT[:, ts], pO[1][:, 0:128], AF.Copy, [b_pO[1]], [b_ckvT])
                    MM(pS[0][:, 0:64], ckvT[:, ts], wuv[:], True, [b_ckvT, b_wu], [b_pS[0]])
                    A(vds[:, t, 0:64], pS[0][:, 0:64], AF.Copy, [b_pS[0]], KVW)
                    yield

            def g_sgu():
                pZ, bZ = pO[2], b_pO[2]
                for tl in range(4):
                    ts = slice(tl * 128, (tl + 1) * 128)
                    for k in range(8):
                        MM(pZ[:, :], hT[:, k, ts], win[:, k, SGU0:SGU0 + 512], k == 0, [b_hT, b_win], [bZ])
                    yield
                    z = pZ[:, :]
                    sg01 = sg[:, 0:2, :].rearrange("p a b -> p (a b)")
                    A(sg01, z, AF.Square, [bZ], [b_sgt])
                    V("tensor_scalar", [b_sgt], [b_sgt], out=sg01, in0=sg01, scalar1=0.044715, scalar2=1.0, op0=ALU.mult, op1=ALU.add)
                    V("tensor_tensor", [b_sgt, bZ], [b_sgt], out=sg01, in0=sg01, in1=z, op=ALU.mult)
                    A(sg01, sg01, AF.Sigmoid, [b_sgt], [b_sgt], scale=1.5957691216)
                    V("tensor_tensor", [b_sgt, bZ], [b_sgt], out=sg01, in0=sg01, in1=z, op=ALU.mult)
                    yield
                    u_ = sg[:, 0, :]; v_ = sg[:, 1, :]; v3 = v_.rearrange("p (g d) -> p g d", g=4)
                    s2v = sg[:, 2, :].rearrange("p (g d) -> p g d", g=4)
                    s3v = sg[:, 3, :].rearrange("p (g d) -> p g d", g=4)
                    V("tensor_reduce", [b_sgt], [b_sm], out=sm[:, 32:36], in_=v3, axis=AX.X, op=ALU.add)
                    V("tensor_scalar", [b_sm], [b_sm], out=sm[:, 32:36], in0=sm[:, 32:36], scalar1=1.0 / 64, scalar2=None, op0=ALU.mult)
                    V("tensor_tensor", [b_sgt, b_sm], [b_sgt], out=s2v, in0=v3, in1=sm[:, 32:36].unsqueeze(2).to_broadcast([128, 4, 64]), op=ALU.subtract)
                    V("tensor_tensor", [b_sgt], [b_sgt], out=sg[:, 3, :], in0=sg[:, 2, :], in1=sg[:, 2, :], op=ALU.mult)
                    V("tensor_reduce", [b_sgt], [b_sm], out=sm[:, 36:40], in_=s3v, axis=AX.X, op=ALU.add)
                    V("tensor_scalar", [b_sm], [b_sm], out=sm[:, 36:40], in0=sm[:, 36:40], scalar1=1.0 / 64, scalar2=1e-5, op0=ALU.mult, op1=ALU.add)
                    A(sm[:, 36:40], sm[:, 36:40], AF.Sqrt, [b_sm], [b_sm])
                    V("reciprocal", [b_sm], [b_sm], out=sm[:, 36:40], in_=sm[:, 36:40])
                    yield
                    V("tensor_tensor", [b_sgt, b_sm], [b_sgt], out=s2v, in0=s2v, in1=sm[:, 36:40].unsqueeze(2).to_broadcast([128, 4, 64]), op=ALU.mult)
                    V("tensor_tensor", [b_sgt, b_sg], [b_sgt], out=sg[:, 2, :], in0=sg[:, 2, :], in1=sgg[:], op=ALU.mult)
                    V("tensor_tensor", [b_sgt, b_sg], [b_vln], out=vln[:], in0=sg[:, 2, :], in1=sgb[:], op=ALU.add)
                    for g4 in range(4):
                        MM(pS[1][:, g4 * 64:(g4 + 1) * 64], wsT[:, g4, :], vln[:, g4 * 64:(g4 + 1) * 64], g4 == 0, [b_ws, b_vln], [b_pS[1]])
                    V("tensor_tensor", [b_pS[1], b_bs], [b_sgt], out=s3v, in0=pS[1][:, 0:256].rearrange("p (g d) -> p g d", g=4),
                      in1=bs[:, 0:4].unsqueeze(2).to_broadcast([128, 4, 64]), op=ALU.add)
                    V("tensor_tensor", [b_sgt], [b_ocs], out=ocs[:, tl, :], in0=sg[:, 3, :], in1=u_, op=ALU.mult)
                    yield

            run_il([g_proj(), g_tok(), g_sgu()])
            if MS <= 1.6:
                S.barrier(); return
            MM(pA[:, :], wuk[:, :], ckvT[:, :], True, [b_wu, b_ckvT], [b_pA])
            rope_from_pA((kdsT[:, gs], KVW), 128, 0, C["rot_h"], B["rot_h"])

            if MS <= 2:
                S.barrier(); return
            n0 = max(0, 32 * gi - 1)
            n1 = 32 * gi + 31
            nn = n1 - n0
            lo0 = 16 * (n0 - 32 * gi + 1)
            for j in range(2):
                for g in range(2):
                    pq, bpq = (pA, b_pA) if g == 0 else (pB, b_pB)
                    for ll in range(32):
                        rhs = xcT[64 * g:64 * g + 64, j, lo0 + ll:lo0 + ll + 16 * (nn - 1) + 1:16]
                        MM(pq[64 * g:64 * g + 64, j * 32:j * 32 + nn], w1[64 * g:64 * g + 64, j, ll, :], rhs, ll == 0,
                           [b_cw, b_xcT], [bpq])
            for j in range(2):
                xx = sg[:, 0, 0:nn]
                V("tensor_scalar", [b_pA, b_cb], [b_sgt], out=sg[0:64, 0, 0:nn], in0=pA[0:64, j * 32:j * 32 + nn], scalar1=cbias[0:64, j:j + 1], scalar2=None, op0=ALU.add)
                V("tensor_scalar", [b_pB, b_cb], [b_sgt], out=sg[64:128, 0, 0:nn], in0=pB[64:128, j * 32:j * 32 + nn], scalar1=cbias[64:128, j:j + 1], scalar2=None, op0=ALU.add)
                V("tensor_tensor", [b_sgt], [b_sgt], out=sg[:, 1, 0:nn], in0=xx, in1=xx, op=ALU.mult)
                V("tensor_scalar", [b_sgt], [b_sgt], out=sg[:, 1, 0:nn], in0=sg[:, 1, 0:nn], scalar1=0.044715, scalar2=1.0, op0=ALU.mult, op1=ALU.add)
                V("tensor_tensor", [b_sgt], [b_sgt], out=sg[:, 1, 0:nn], in0=sg[:, 1, 0:nn], in1=xx, op=ALU.mult)
                A(sg[:, 1, 0:nn], sg[:, 1, 0:nn], AF.Sigmoid, [b_sgt], [b_sgt], scale=1.5957691216)
                V("tensor_tensor", [b_sgt], [b_cmp], out=hidT[:, j, n0:n1], in0=sg[:, 1, 0:nn], in1=xx, op=ALU.mult)
            for g in range(2):
                MM(pS[g][64 * g:64 * g + 64, 0:nn], w2[64 * g:64 * g + 64, 0, :], hidT[64 * g:64 * g + 64, 0, n0:n1], True, [b_cw, b_cmp], [b_pS[g]])
                V("tensor_copy", [b_pS[g]], [b_cmp], out=kcT[64 * g:64 * g + 64, n0:n1], in_=pS[g][64 * g:64 * g + 64, 0:nn])
            for a in ((32 * gi - 32, 32 * gi), (32 * gi, 32 * gi + 32)):
                if a[0] < 0:
                    continue
                nt_, po = a[0] // 128, a[0] % 128
                for g in range(2):
                    if po < 96:
                        MM(pS[g][po:po + 32, 64:128], hidT[64 * g:64 * g + 64, 1, a[0]:a[1]], w2[64 * g:64 * g + 64, 1, :], True,
                           [b_cw, b_cmp], [b_pS[g]])
                        V("tensor_copy", [b_pS[g]], [b_cmp], out=vcmp[po:po + 32, nt_, g, 0:64], in_=pS[g][po:po + 32, 64:128])
                    else:
                        MM(pS[g][64:128, 64:128], hidT[64 * g:64 * g + 64, 1, a[0] - 32:a[1]], w2[64 * g:64 * g + 64, 1, :], True,
                           [b_cw, b_cmp], [b_pS[g]])
                        V("tensor_copy", [b_pS[g]], [b_cmp], out=vcmp[64:128, nt_, g, 0:64], in_=pS[g][64:128, 64:128])

            if MS <= 2.5:
                S.barrier(); return
            def nsa_chain(i, tl, ci):
                ts = slice(tl * 128, (tl + 1) * 128)
                KVR = [b_kv[x] for x in range(gi + 1)] + [b_kv0]
                sm = sm_n; b_sm = b_smn
                pOc = [pO[0], pO[2]]; b_pOc = [b_pO[0], b_pO[2]]
                catc, b_catc = cat[ci], b_cat[ci]
                n_nt = 1 if (8 * i + 7) <= 128 else 2
                first = [True, True]
                for nt_ in range(n_nt):
                    ei = nextE()
                    bE2 = [b_Eh[2 * ei], b_Eh[2 * ei + 1]]
                    for g in range(2):
                        MM(pS[g][:, 0:384].rearrange("p (r t) -> p r t", r=3), kcT[64 * g:64 * g + 64, nt_ * 128:(nt_ + 1) * 128],
                           qrT[64 * g:64 * g + 64, :, ts], True, [b_cmp, b_q], [b_pS[g]])
                        A(E[ei][:, g * 384:(g + 1) * 384], pS[g][:, 0:384], AF.Exp, [b_pS[g]], [bE2[g]], scale=0.125)
                    G("affine_select", bE2, bE2, out=E[ei][:, :], in_=E[ei][:, :], pattern=[[0, 6], [1, 128]],
                      compare_op=ALU.is_ge, fill=0.0, base=128 * i - 2048 * nt_ - 31, channel_multiplier=-16)
                    for g in range(2):
                        for r in range(3):
                            MM(pOc[g][:, r * 129:(r + 1) * 129], E[ei][:, (g * 3 + r) * 128:(g * 3 + r + 1) * 128], vcmp[:, nt_, g, :],
                               first[g], [bE2[g], b_cmp], [b_pOc[g]])
                            first[g] = False
                    yield
                for g in range(2):
                    po3 = pOc[g][:, 0:387].rearrange("p (r c) -> p r c", r=3)
                    V("tensor_scalar", [b_pOc[g]], [b_sm], out=sm[:, 40 + g * 3:43 + g * 3], in0=po3[:, :, 64], scalar1=0.0, scalar2=None, op0=ALU.is_equal)
                    V("tensor_tensor", [b_pOc[g], b_sm], [b_sm], out=sm[:, g * 3:g * 3 + 3], in0=po3[:, :, 64], in1=sm[:, 40 + g * 3:43 + g * 3], op=ALU.add)
                V("reciprocal", [b_sm], [b_sm], out=sm[:, 0:6], in_=sm[:, 0:6])
                glv = gl[:, tl, 0:18].rearrange("p (h b) -> p h b", b=3)
                V("tensor_tensor", [b_sm, b_gl], [b_sm], out=sm[:, 6:12], in0=sm[:, 0:6], in1=glv[:, :, 0], op=ALU.mult)
                for g in range(2):
                    po3 = pOc[g][:, 0:387].rearrange("p (r c) -> p r c", r=3)
                    V("tensor_tensor", [b_pOc[g], b_sm], [b_imp], out=imp[:, g * 3:g * 3 + 3, :], in0=po3[:, :, 65:129],
                      in1=sm[:, g * 3:g * 3 + 3].unsqueeze(2).to_broadcast([128, 3, 64]), op=ALU.mult)
                    V("tensor_tensor", [b_pOc[g], b_sm], [b_oc], out=oc[:, g * 3:g * 3 + 3, :], in0=po3[:, :, 0:64],
                      in1=sm[:, 6 + g * 3:9 + g * 3].unsqueeze(2).to_broadcast([128, 3, 64]), op=ALU.mult)
                yield
                for g in range(2):
                    V("tensor_tensor", [b_imp], [b_sc], out=sc[:, g, :], in0=imp[:, g * 3, :], in1=imp[:, g * 3 + 1, :], op=ALU.add)
                    V("tensor_tensor", [b_imp, b_sc], [b_sc], out=sc[:, g, :], in0=sc[:, g, :], in1=imp[:, g * 3 + 2, :], op=ALU.add)
                    V("tensor_tensor", [b_sc, B["rel"]], [b_sc], out=sc[:, g, :], in0=sc[:, g, :], in1=C["rel"][:, 63 - 2 * i:127 - 2 * i], op=ALU.add)
                    V("tensor_tensor", [b_sc, B["col0"]], [b_sc], out=sc[:, g, :], in0=sc[:, g, :], in1=C["col0"][:], op=ALU.add)
                    V("max", [b_sc], [b_m8], out=m8[:, g, :], in_=sc[:, g, :])
                    V("match_replace", [b_sc, b_m8], [b_sc], out=sc2[:, g, :], in_to_replace=m8[:, g, :], in_values=sc[:, g, :], imm_value=-3.0e38)
                    V("max", [b_sc], [b_m8], out=m8[:, g, :], in_=sc2[:, g, :])
                    V("tensor_scalar", [b_sc, b_m8], [b_bm], out=bm[:, g, :], in0=sc[:, g, :], scalar1=m8[:, g, 7:8], scalar2=None, op0=ALU.is_ge)
                yield
                stO = [True]

                def sel_front(it):
                    j2, g, k = it
                    eh = k % 4
                    Eh = E[eh // 2][:, (eh % 2) * 384:(eh % 2 + 1) * 384]
                    MM(pS[g][:, 0:384].rearrange("p (r t) -> p r t", r=3), kselT[64 * g:64 * g + 64, j2 * 128:(j2 + 1) * 128],
                       qropT[64 * g:64 * g + 64, :, ts], True, KVR + [b_q], [b_pS[g]])
                    if j2 == i:
                        MM(pS[g][:, 0:384], C["ident_b"][:], C["negtri_c"][:], False, [B["ident_b"], B["negtri_c"]], [b_pS[g]])
                    A(Eh, pS[g][:, 0:384], AF.Exp, [b_pS[g]], [b_Eh[eh]], scale=0.125)
                    V("tensor_copy", [b_bm], [b_bx[g]], out=bx[:, g, :].rearrange("p (a b) -> p a b", a=2),
                      in_=bm[:, g, 2 * j2:2 * j2 + 2].unsqueeze(2).to_broadcast([128, 2, 64]))
                    TR(pM[:, g * 128:(g + 1) * 128], bx[:, g, :], C["ident_b"][:], [b_bx[g], B["ident_b"]], [b_pM[0]])

                def sel_back(it):
                    j2, g, k = it
                    eh = k % 4
                    Eh = E[eh // 2][:, (eh % 2) * 384:(eh % 2 + 1) * 384]
                    Ph = Pm[eh // 2][:, (eh % 2) * 384:(eh % 2 + 1) * 384]
                    V("tensor_tensor", [b_Eh[eh], b_pM[0]], [b_Ph[eh]], out=Ph.rearrange("p (r t) -> p r t", r=3),
                      in0=Eh.rearrange("p (r t) -> p r t", r=3),
                      in1=pM[:, g * 128:(g + 1) * 128].unsqueeze(1).to_broadcast([128, 3, 128]), op=ALU.mult)
                    for r in range(3):
                        h = g * 3 + r
                        MM(pO[2][:, h * 65:(h + 1) * 65], Ph[:, r * 128:(r + 1) * 128], vsel[:, j2, g, :], stO[0],
                           [b_Ph[eh]] + KVR, [b_pO[2]])
                        stO[0] = False

                items = [(j2, g, 2 * j2 + g) for j2 in range(i + 1) for g in range(2)]
                prev = None
                for it in items:
                    sel_front(it)
                    if prev is not None:
                        sel_back(prev)
                        yield
                    prev = it
                sel_back(prev)
                yield
                po6 = pO[2][:, 0:390].rearrange("p (h c) -> p h c", h=6)
                V("tensor_scalar", [b_pO[2]], [b_sm], out=sm[:, 12:18], in0=po6[:, :, 64], scalar1=1e-30, scalar2=None, op0=ALU.max)
                V("reciprocal", [b_sm], [b_sm], out=sm[:, 12:18], in_=sm[:, 12:18])
                V("tensor_tensor", [b_sm, b_gl], [b_sm], out=sm[:, 12:18], in0=sm[:, 12:18], in1=glv[:, :, 1], op=ALU.mult)
                V("tensor_tensor", [b_pO[2], b_sm], [b_imp], out=imp[:], in0=po6[:, :, 0:64],
                  in1=sm[:, 12:18].unsqueeze(2).to_broadcast([128, 6, 64]), op=ALU.mult)
                V("tensor_tensor", [b_imp, b_oc], [b_oc], out=oc[:], in0=oc[:], in1=imp[:], op=ALU.add)
                yield
                stW = [True]

                def win_front(it):
                    j2, g, k = it
                    eh = k % 4
                    Eh = E[eh // 2][:, (eh % 2) * 384:(eh % 2 + 1) * 384]
                    KWR = [b_kw[(j2 // 4) % 2], b_kv0]
                    wc = slice((j2 % 8) * 128, (j2 % 8 + 1) * 128)
                    MM(pS[g][:, 0:384].rearrange("p (r t) -> p r t", r=3), kwinT[64 * g:64 * g + 64, wc],
                       qropT[64 * g:64 * g + 64, :, ts], True, KWR + [b_q], [b_pS[g]])
                    if j2 == i:
                        MM(pS[g][:, 0:384], C["ident_b"][:], C["negtri_c"][:], False, [B["ident_b"], B["negtri_c"]], [b_pS[g]])
                    if j2 == i - 4:
                        MM(pS[g][:, 0:384], C["ident_b"][:], C["negtri_w"][:], False, [B["ident_b"], B["negtri_w"]], [b_pS[g]])
                    A(Eh, pS[g][:, 0:384], AF.Exp, [b_pS[g]], [b_Eh[eh]], scale=0.125)

                def win_back(it):
                    j2, g, k = it
                    eh = k % 4
                    Eh = E[eh // 2][:, (eh % 2) * 384:(eh % 2 + 1) * 384]
                    KWR = [b_kw[(j2 // 4) % 2], b_kv0]
                    for r in range(3):
                        h = g * 3 + r
                        MM(pO[0][:, h * 65:(h + 1) * 65], Eh[:, r * 128:(r + 1) * 128], vwin[:, j2 % 8, g, :], stW[0],
                           [b_Eh[eh]] + KWR, [b_pO[0]])
                        stW[0] = False

                items = [(j2, g, 2 * jj + g) for jj, j2 in enumerate(range(max(0, i - 4), i + 1)) for g in range(2)]
                prev = None
                for it in items:
                    win_front(it)
                    if prev is not None:
                        win_back(prev)
                        yield
                    prev = it
                win_back(prev)
                yield
                po6 = pO[0][:, 0:390].rearrange("p (h c) -> p h c", h=6)
                V("tensor_scalar", [b_pO[0]], [b_sm], out=sm[:, 18:24], in0=po6[:, :, 64], scalar1=1e-30, scalar2=None, op0=ALU.max)
                V("reciprocal", [b_sm], [b_sm], out=sm[:, 18:24], in_=sm[:, 18:24])
                V("tensor_tensor", [b_sm, b_gl], [b_sm], out=sm[:, 18:24], in0=sm[:, 18:24], in1=glv[:, :, 2], op=ALU.mult)
                V("tensor_tensor", [b_pO[0], b_sm], [b_imp], out=imp[:], in0=po6[:, :, 0:64],
                  in1=sm[:, 18:24].unsqueeze(2).to_broadcast([128, 6, 64]), op=ALU.mult)
                V("tensor_tensor", [b_imp, b_oc], [b_catc], out=catc[:, 0:384].rearrange("p (h d) -> p h d", h=6), in0=oc[:], in1=imp[:], op=ALU.add)
                G("tensor_copy", [b_ocs], [b_catc], out=catc[:, 768:1024], in_=ocs[:, tl, :])
                yield

            def dsa_a(i, tl):
                ts = slice(tl * 128, (tl + 1) * 128)
                KVR = [b_kv[x] for x in range(gi + 1)] + [b_kv0]
                nk = 128 * (i + 1)
                V("tensor_single_scalar", [b_gl], [b_wsg], out=wsg[:, 8:12], in_=gl[:, tl, 18:22], scalar=-1.0, op=ALU.mult)
                V("tensor_tensor", [b_gl, b_wsg], [b_wsg], out=wsg[:, 0:4], in0=gl[:, tl, 18:22], in1=wsg[:, 8:12], op=ALU.max)
                V("tensor_single_scalar", [b_gl], [b_wsg], out=wsg[:, 4:8], in_=gl[:, tl, 18:22], scalar=0.0, op=ALU.is_gt)
                V("tensor_single_scalar", [b_gl], [b_wsg], out=wsg[:, 8:12], in_=gl[:, tl, 18:22], scalar=0.0, op=ALU.is_lt)
                V("tensor_tensor", [b_wsg], [b_wsg], out=wsg[:, 4:8], in0=wsg[:, 4:8], in1=wsg[:, 8:12], op=ALU.subtract)
                for c0 in range(0, nk, 512):
                    cw = min(512, nk - c0)
                    for h in range(4):
                        pp, bpp = (pA, b_pA) if h % 2 == 0 else (pB, b_pB)
                        ri = h % 2
                        MM(pp[:, 0:cw], iqT[32 * (h % 2):32 * (h % 2) + 32, h // 2, ts], ikT[32 * (h % 2):32 * (h % 2) + 32, c0:c0 + cw], True, [b_q] + KVR, [bpp])
                        A(rl[ri][:, 0:cw], pp[:, 0:cw], AF.Relu, [bpp, b_wsg], [b_rl[ri]], scale=wsg[:, h:h + 1])
                        if h == 0:
                            V("tensor_scalar", [b_rl[ri], b_wsg], [b_acc], out=acc[:, c0:c0 + cw], in0=rl[ri][:, 0:cw], scalar1=wsg[:, 4:5], scalar2=None, op0=ALU.mult)
                        else:
                            V("scalar_tensor_tensor", [b_rl[ri], b_wsg, b_acc], [b_acc], out=acc[:, c0:c0 + cw], in0=rl[ri][:, 0:cw],
                              scalar=wsg[:, 4 + h:5 + h], in1=acc[:, c0:c0 + cw], op0=ALU.mult, op1=ALU.add)
                    yield
                if nk > self.KTOP:
                    V("tensor_reduce", [b_acc], [b_bst], out=bst[:, 1:2], in_=acc[:, 0:nk], axis=AX.X, op=ALU.max)
                    V("tensor_reduce", [b_acc], [b_bst], out=bst[:, 0:1], in_=acc[:, 0:nk], axis=AX.X, op=ALU.min)
                G("affine_select", [b_acc], [b_acc], out=acc[:, 128 * i:128 * i + 128], in_=acc[:, 128 * i:128 * i + 128], pattern=[[-1, 128]],
                  compare_op=ALU.is_ge, fill=FILL, base=0, channel_multiplier=1)
                yield
                if nk > self.KTOP:
                    V("tensor_scalar", [b_bst], [b_bst], out=bst[:, 0:1], in0=bst[:, 0:1], scalar1=-1.0, scalar2=None, op0=ALU.add)
                    V("tensor_tensor", [b_bst], [b_bst], out=bst[:, 2:3], in0=bst[:, 1:2], in1=bst[:, 0:1], op=ALU.subtract)
                    V("tensor_scalar", [b_bst, B["pow2"]], [b_bst], out=bst[:, 8:8 + NBIS], in0=C["pow2"][:], scalar1=bst[:, 2:3], scalar2=None, op0=ALU.mult)
                    for kb in range(NBIS):
                        V("tensor_tensor", [b_bst], [b_bst], out=bst[:, 3:4], in0=bst[:, 0:1], in1=bst[:, 8 + kb:9 + kb], op=ALU.add)
                        V("tensor_scalar", [b_acc, b_bst], [b_junk, b_bst], out=junk[:, 0:nk], in0=acc[:, 0:nk], scalar1=bst[:, 3:4], scalar2=None,
                          op0=ALU.is_gt, op1=ALU.add, accum_out=bst[:, 4:5])
                        V("tensor_scalar", [b_bst], [b_bst], out=bst[:, 5:6], in0=bst[:, 4:5], scalar1=self.KTOP - 0.5, scalar2=bst[:, 8 + kb:9 + kb],
                          op0=ALU.is_gt, op1=ALU.mult)
                        V("tensor_tensor", [b_bst], [b_bst], out=bst[:, 0:1], in0=bst[:, 0:1], in1=bst[:, 5:6], op=ALU.add)
                        yield

            def dsa_b(i, tl, ci):
                ts = slice(tl * 128, (tl + 1) * 128)
                KVR = [b_kv[x] for x in range(gi + 1)] + [b_kv0]
                nk = 128 * (i + 1)
                sm = sm_d; b_sm = b_smd
                catc, b_catc = cat[ci], b_cat[ci]
                if nk > self.KTOP:
                    V("tensor_scalar", [b_acc, b_bst], [b_dm], out=dmask[:, 0:nk], in0=acc[:, 0:nk], scalar1=bst[:, 0:1], scalar2=None, op0=ALU.is_gt)
                else:
                    V("tensor_single_scalar", [b_acc], [b_dm], out=dmask[:, 0:nk], in_=acc[:, 0:nk], scalar=-1.0e29, op=ALU.is_gt)
                yield
                stD = [True]
                pSd = [pA, pB]; b_pSd = [b_pA, b_pB]

                def dsa_front(it):
                    j2, hb, k = it
                    eh = k % 4
                    Eh = Ed[eh // 2][:, (eh % 2) * 384:(eh % 2 + 1) * 384]
                    po_ = 64 * hb
                    MM(pSd[hb][:, 0:384].rearrange("p (r t) -> p r t", r=3), kdsT[po_:po_ + 64, j2 * 128:(j2 + 1) * 128],
                       dqT[po_:po_ + 64, :, ts], True, KVR + [b_q], [b_pSd[hb]])
                    A(Eh, pSd[hb][:, 0:384], AF.Exp, [b_pSd[hb]], [b_Edh[eh]], scale=0.125)
                    if hb == 0:
                        mo = 512 + 128 * (j2 % 2)
                        TR(pM[:, mo:mo + 128], dmask[:, j2 * 128:(j2 + 1) * 128], C["ident_b"][:], [b_dm, B["ident_b"]], [b_pM[1]])

                def dsa_back(it):
                    j2, hb, k = it
                    eh = k % 4
                    Eh = Ed[eh // 2][:, (eh % 2) * 384:(eh % 2 + 1) * 384]
                    Ph = Pd[0][:, (k % 2) * 384:(k % 2 + 1) * 384]
                    mo = 512 + 128 * (j2 % 2)
                    V("tensor_tensor", [b_Edh[eh], b_pM[1]], [b_Pdh[k % 2]], out=Ph.rearrange("p (r t) -> p r t", r=3),
                      in0=Eh.rearrange("p (r t) -> p r t", r=3),
                      in1=pM[:, mo:mo + 128].unsqueeze(1).to_broadcast([128, 3, 128]), op=ALU.mult)
                    for r in range(3):
                        hh = 2 * r + hb
                        MM(pO[1][:, hh * 65:(hh + 1) * 65], Ph[:, r * 128:(r + 1) * 128], vds[:, j2, :], stD[0], [b_Pdh[k % 2]] + KVR, [b_pO[1]])
                        stD[0] = False

                items = [(j2, hb, 2 * j2 + hb) for j2 in range(i + 1) for hb in range(2)]
                prev = None
                for it in items:
                    dsa_front(it)
                    if prev is not None:
                        dsa_back(prev)
                        yield
                    prev = it
                dsa_back(prev)
                yield
                po6 = pO[1][:, 0:390].rearrange("p (h c) -> p h c", h=6)
                V("tensor_scalar", [b_pO[1]], [b_sm], out=sm[:, 24:30], in0=po6[:, :, 64], scalar1=1e-30, scalar2=None, op0=ALU.max)
                V("reciprocal", [b_sm], [b_sm], out=sm[:, 24:30], in_=sm[:, 24:30])
                V("tensor_tensor", [b_pO[1], b_sm], [b_catc], out=catc[:, 384:768].rearrange("p (h d) -> p h d", h=6), in0=po6[:, :, 0:64],
                  in1=sm[:, 24:30].unsqueeze(2).to_broadcast([128, 6, 64]), op=ALU.mult)
                yield

            def out_chain(i, ci):
                catc, b_catc = cat[ci], b_cat[ci]
                xrc, b_xrc = xr[ci], b_xr[ci]
                if self.debug and l == 0:
                    self.S.dma(self.S.pool, self.dbg[i * 128:(i + 1) * 128, :], catc[:], reads=[b_catc], writes=[Buf()], is_output=True)
                LD(xrc[:], xin[i * 128:(i + 1) * 128, :], [b_xrc], R=[bxin[i]])
                yield
                pCT = pS[0][:, :].bitcast(BF16)
                for k in range(8):
                    TR(pCT[:, k * 128:(k + 1) * 128], catc[:, k * 128:(k + 1) * 128], C["ident_b"][:], [b_catc, B["ident_b"]], [b_pS[0]])
                A(catT[:].rearrange("p k t -> p (k t)"), pCT, AF.Copy, [b_pS[0]], [b_catT])
                yield
                for hf in range(2):
                    pp, bpp = (pA, b_pA) if hf == 0 else (pB, b_pB)
                    for k in range(8):
                        MM(pp[:, :], catT[:, k, :], wout[:, k, hf * 512:(hf + 1) * 512], k == 0, [b_catT, b_wout], [bpp])
                    V("scalar_tensor_tensor", [bpp, b_xrc], [b_xrc], out=xrc[:, hf * 512:(hf + 1) * 512], in0=xrc[:, hf * 512:(hf + 1) * 512],
                      scalar=ALPHA, in1=pp[:, :], op0=ALU.mult, op1=ALU.add)
                    yield
                self.ln_stats(xrc, b_xrc, mv2, b_mv2, mv2[:, 2:3], stats2, b_st2)
                V("tensor_scalar", [b_xrc, b_mv2], [b_xrc], out=xrc[:], in0=xrc[:], scalar1=mv2[:, 0:1], scalar2=mv2[:, 2:3], op0=ALU.subtract, op1=ALU.mult)
                yield
                G("tensor_tensor", [b_xrc, b_ln], [b_xrc], out=xrc[:], in0=xrc[:], in1=lng[:], op=ALU.mult)
                G("tensor_tensor", [b_xrc, b_ln], [b_xrc], out=xrc[:], in0=xrc[:], in1=lnb[:], op=ALU.add)
                LD(xout[i * 128:(i + 1) * 128, :], xrc[:], [bxout[i]], R=[b_xrc])
                yield

            def run_interleaved(gens):
                gens = [g_ for g_ in gens if g_ is not None]
                while gens:
                    for g_ in list(gens):
                        try:
                            next(g_)
                        except StopIteration:
                            gens.remove(g_)

            i0 = gi * 4
            run_interleaved([dsa_a(i0, 0), nsa_chain(i0, 0, i0 % 2)])
            pend = None
            for tl in range(4):
                i = gi * 4 + tl
                ci = i % 2
                gens = [dsa_b(i, tl, ci)]
                if tl < 3:
                    gens += [dsa_a(i + 1, tl + 1), nsa_chain(i + 1, tl + 1, (i + 1) % 2)]
                gens.append(pend)
                run_interleaved(gens)
                pend = out_chain(i, ci)
            run_interleaved([pend])
        S.barrier()

    def ffn_common(self, st, l):
        C, B, I = self.C, self.B, self.I
        sb = lambda n, s, d: self.sb(st, n, s, d)
        o = {}
        o["pA"] = self.ps(st, "fA", [128, 512], F32); o["bpA"] = PBuf()
        o["pB"] = self.ps(st, "fB", [128, 512], F32); o["bpB"] = PBuf()
        o["pT"] = [self.ps(st, f"fT{i}", [128, 512], F32) for i in range(2)]; o["bpT"] = [PBuf(), PBuf()]
        o["pD"] = [self.ps(st, f"fD{i}", [128, 512], F32) for i in range(2)]; o["bpD"] = [PBuf(), PBuf()]
        o["pR"] = self.ps(st, "fR", [128, 512], F32); o["bpR"] = PBuf()
        gbc = sb("gbc2", [128, D], F32); b_gbc = Buf()
        self.LD(gbc[:], self.gate_d[2 * l + 1:2 * l + 2, :].to_broadcast([128, D]), [b_gbc], R=[self.b_gate])
        o["gbc"], o["b_gbc"] = gbc, b_gbc
        lng = sb("lng2", [128, D], F32); lnb = sb("lnb2", [128, D], F32); b_ln = Buf()
        self.LD(lng[:], I["ln2_g"][l:l + 1, :].to_broadcast([128, D]), [b_ln])
        self.LD(lnb[:], I["ln2_b"][l:l + 1, :].to_broadcast([128, D]), [b_ln])
        o["lng"], o["lnb"], o["b_ln"] = lng, lnb, b_ln
        o["stats"] = sb("fstats", [128, 2, 6], F32); o["b_st"] = Buf()
        o["mv"] = sb("fmv", [128, 4], F32); o["b_mv"] = Buf()
        o["xn"] = sb("fxn", [128, D], F32); o["b_xn"] = Buf()
        o["ybuf"] = [sb("fy0", [128, D], F32)] * 2; _by = Buf(); o["b_y"] = [_by, _by]
        return o

    def ffn_ln_mod(self, o, l, xt, b_x, hT, b_hT, tl, hT32=None, b_hT32=None):
        C, B = self.C, self.B
        self.ln_stats(xt, b_x, o["mv"], o["b_mv"], o["mv"][:, 2:3], o["stats"], o["b_st"])
        self.V("tensor_scalar", [b_x, o["b_mv"]], [o["b_xn"]], out=o["xn"][:], in0=xt[:], scalar1=o["mv"][:, 0:1], scalar2=o["mv"][:, 2:3],
               op0=ALU.subtract, op1=ALU.mult)
        for hf in range(2):
            pp, bpp = o["pT"][hf], o["bpT"][hf]
            for k4 in range(4):
                self.TR(pp[:, k4 * 128:(k4 + 1) * 128], o["xn"][:, (hf * 4 + k4) * 128:(hf * 4 + k4 + 1) * 128], C["ident_f"][:],
                        [o["b_xn"], B["ident_f"]], [bpp])
            for k4 in range(4):
                k = hf * 4 + k4
                self.A(hT[:, k, tl * 128:(tl + 1) * 128], pp[:, k4 * 128:(k4 + 1) * 128], AF.Identity, [bpp, self.b_mod], [b_hT],
                       bias=self.modcol[:, l, 2, k:k + 1], scale=self.modcol[:, l, 3, k:k + 1])
                if hT32 is not None:
                    self.V("tensor_scalar", [bpp, self.b_mod], [b_hT32], out=hT32[:, k, :], in0=pp[:, k4 * 128:(k4 + 1) * 128],
                           scalar1=self.modcol[:, l, 3, k:k + 1], scalar2=self.modcol[:, l, 2, k:k + 1], op0=ALU.mult, op1=ALU.add)

    def ffn_finish_tile(self, o, yb, b_yb, dst_ap, b_dst, is_out):
        self.ln_stats(yb, b_yb, o["mv"], o["b_mv"], o["mv"][:, 2:3], o["stats"], o["b_st"])
        self.V("tensor_scalar", [b_yb, o["b_mv"]], [b_yb], out=yb[:], in0=yb[:], scalar1=o["mv"][:, 0:1], scalar2=o["mv"][:, 2:3],
               op0=ALU.subtract, op1=ALU.mult)
        self.G("tensor_tensor", [b_yb, o["b_ln"]], [b_yb], out=yb[:], in0=yb[:], in1=o["lng"][:], op=ALU.mult)
        self.G("tensor_tensor", [b_yb, o["b_ln"]], [b_yb], out=yb[:], in0=yb[:], in1=o["lnb"][:], op=ALU.add)
        self.S.dma(self.S.sp, dst_ap, yb[:], reads=[b_yb], writes=[b_dst], is_output=is_out)

    def ffn_dense(self, st, l, xin, bxin, xout, bxout, is_out):
        S, C, B, I = self.S, self.C, self.B, self.I
        sb = lambda n, s, d: self.sb(st, n, s, d)
        MM, A, V, G, LD = self.MM, self.A, self.V, self.G, self.LD
        j = l // 2
        NF = D_FF // 128
        o = self.ffn_common(st, l)
        wg = sb("wg", [128, 8, D_FF], BF16); wu = sb("wu", [128, 8, D_FF], BF16); wd = sb("wd", [128, NF, D], BF16)
        b_wg, b_wu, b_wd = Buf(), Buf(), Buf()
        for (a, b) in ((0, 1408), (1408, D_FF)):
            LD(wg[:, :, a:b], I["ffn_wg"][j, :, a:b].rearrange("(k p) n -> p k n", p=128), [b_wg], q=S.pool)
            LD(wu[:, :, a:b], I["ffn_wu"][j, :, a:b].rearrange("(k p) n -> p k n", p=128), [b_wu], q=S.pool)
        for (a, b) in ((0, 11), (11, NF)):
            LD(wd[:, a:b, :], I["ffn_wd"][j, a * 128:b * 128, :].rearrange("(k p) n -> p k n", p=128), [b_wd], q=S.pool)
        for k in range(NF):
            V("tensor_tensor", [b_wd, o["b_gbc"]], [b_wd], out=wd[:, k, :], in0=wd[:, k, :], in1=o["gbc"][:], op=ALU.mult)
        xt = [sb(f"fx{i}", [128, D], F32) for i in range(4)]; b_xt = [Buf() for _ in range(4)]
        hT = sb("fhT", [128, 8, 512], BF16); b_hT = Buf()
        hid = sb("hid", [128, NF, 512], BF16); b_hid = Buf()
        sg = [sb("fsg0", [128, 512], F32)] * 2; _bs = Buf(); b_sg = [_bs, _bs]
        ny = 0
        for gi in range(self.NG):
            for tl in range(4):
                t = gi * 4 + tl
                LD(xt[tl][:], xin[t * 128:(t + 1) * 128, :], [b_xt[tl]], R=[bxin[t]])
                self.ffn_ln_mod(o, l, xt[tl], b_xt[tl], hT, b_hT, tl)
            for fc in range(NF):
                for k in range(8):
                    MM(o["pA"][:, :], wg[:, k, fc * 128:(fc + 1) * 128], hT[:, k, :], k == 0, [b_wg, b_hT], [o["bpA"]])
                for k in range(8):
                    MM(o["pB"][:, :], wu[:, k, fc * 128:(fc + 1) * 128], hT[:, k, :], k == 0, [b_wu, b_hT], [o["bpB"]])
                si = fc % 2
                A(sg[si][:], o["pA"][:, :], AF.Silu, [o["bpA"]], [b_sg[si]])
                V("tensor_tensor", [b_sg[si], o["bpB"]], [b_hid], out=hid[:, fc, :], in0=sg[si][:], in1=o["pB"][:, :], op=ALU.mult)
            for tl in range(4):
                t = gi * 4 + tl
                yb, byb = o["ybuf"][ny % 2], o["b_y"][ny % 2]; ny += 1
                for hf in range(2):
                    pd, bpd = o["pD"][hf], o["bpD"][hf]
                    for fc in range(NF):
                        MM(pd[:, :], hid[:, fc, tl * 128:(tl + 1) * 128], wd[:, fc, hf * 512:(hf + 1) * 512], fc == 0, [b_hid, b_wd], [bpd])
                    V("scalar_tensor_tensor", [bpd, b_xt[tl]], [byb], out=yb[:, hf * 512:(hf + 1) * 512], in0=xt[tl][:, hf * 512:(hf + 1) * 512],
                      scalar=ALPHA, in1=pd[:, :], op0=ALU.mult, op1=ALU.add)
                self.ffn_finish_tile(o, yb, byb, xout[t * 128:(t + 1) * 128, :], bxout[t], is_out)
        S.barrier()

    def ffn_moe(self, st, l, xin, bxin, xout, bxout, is_out):
        S, C, B, I = self.S, self.C, self.B, self.I
        sb = lambda n, s, d: self.sb(st, n, s, d)
        MM, A, V, G, LD = self.MM, self.A, self.V, self.G, self.LD
        j = l // 2
        o = self.ffn_common(st, l)
        SGT = min(self.NT, 16)
        NSG = self.NT // SGT
        wr = sb("wr", [128, 8, NEXP], F32); b_wr = Buf()
        LD(wr[:], I["moe_wr"][j].rearrange("(k p) n -> p k n", p=128), [b_wr])
        brb = sb("brb", [128, NEXP], F32)
        LD(brb[:], I["moe_br"][j:j + 1, :].to_broadcast([128, NEXP]), [b_wr])
        hTa = sb("hTa", [128, 8, SGT * 128], BF16); b_hTa = [Buf() for _ in range(SGT // 4)]
        hT32 = sb("hT32", [128, 8, 128], F32); b_hT32 = Buf()
        accs = sb("accs", [128, SGT, D], F32); b_acc = [Buf() for _ in range(SGT)]
        gates = sb("gates", [128, SGT, NEXP], F32); b_gt = Buf()
        lg = sb("lg", [128, 40], F32); b_lg = Buf()
        xt = [sb(f"mx{i}", [128, D], F32) for i in range(2)]; b_xt = [Buf(), Buf()]
        wgc = [sb(f"wgc{i}", [128, 8, 512], BF16) for i in range(2)]
        wuc = [sb(f"wuc{i}", [128, 8, 512], BF16) for i in range(2)]
        wdc = [sb(f"wdc{i}", [128, 4, D], BF16) for i in range(2)]
        b_wc = [Buf(), Buf()]
        hid = sb("mhid", [128, 4, 512], BF16); b_hid = Buf()
        sg = [sb(f"msg{i}", [128, 512], F32) for i in range(2)]; b_sg = [Buf(), Buf()]
        nx = 0
        nw = 0
        for sgi in range(NSG):
            for tt in range(SGT):
                t = sgi * SGT + tt
                xi = nx % 2; nx += 1
                LD(xt[xi][:], xin[t * 128:(t + 1) * 128, :], [b_xt[xi]], R=[bxin[t]])
                self.ffn_ln_mod(o, l, xt[xi], b_xt[xi], hTa[:, :, (tt // 4) * 512:(tt // 4 + 1) * 512], b_hTa[tt // 4], tt % 4, hT32, b_hT32)
                for k in range(8):
                    MM(o["pR"][:, 0:NEXP], hT32[:, k, :], wr[:, k, :], k == 0, [b_hT32, b_wr], [o["bpR"]])
                V("tensor_tensor", [o["bpR"], b_wr], [b_lg], out=lg[:, 0:8], in0=o["pR"][:, 0:NEXP], in1=brb[:], op=ALU.add)
                V("max", [b_lg], [b_lg], out=lg[:, 8:16], in_=lg[:, 0:8])
                V("tensor_tensor", [b_lg], [b_lg], out=lg[:, 16:17], in0=lg[:, 9:10], in1=lg[:, 8:9], op=ALU.subtract)
                A(lg[:, 17:18], lg[:, 16:17], AF.Sigmoid, [b_lg], [b_lg])
                V("tensor_scalar", [b_lg], [b_lg], out=lg[:, 18:19], in0=lg[:, 17:18], scalar1=-1.0, scalar2=1.0, op0=ALU.mult, op1=ALU.add)
                V("tensor_scalar", [b_lg], [b_lg], out=lg[:, 24:32], in0=lg[:, 0:8], scalar1=lg[:, 8:9], scalar2=lg[:, 18:19], op0=ALU.is_equal, op1=ALU.mult)
                V("tensor_scalar", [b_lg], [b_lg], out=lg[:, 32:40], in0=lg[:, 0:8], scalar1=lg[:, 9:10], scalar2=lg[:, 17:18], op0=ALU.is_equal, op1=ALU.mult)
                V("tensor_tensor", [b_lg], [b_gt], out=gates[:, tt, :], in0=lg[:, 24:32], in1=lg[:, 32:40], op=ALU.add)
            import os
            MOES = int(os.environ.get("MOESTOP", "99"))
            for e in range(min(NEXP, MOES)):
                for fc in range(E_FF // 512 if MOES > 1 else 1):
                    wi = nw % 2; nw += 1
                    f0 = fc * 512
                    LD(wgc[wi][:], I["moe_wg"][j, e, :, f0:f0 + 512].rearrange("(k p) n -> p k n", p=128), [b_wc[wi]], q=S.pool)
                    LD(wuc[wi][:], I["moe_wu"][j, e, :, f0:f0 + 512].rearrange("(k p) n -> p k n", p=128), [b_wc[wi]], q=S.pool)
                    LD(wdc[wi][:], I["moe_wd"][j, e, f0:f0 + 512, :].rearrange("(k p) n -> p k n", p=128), [b_wc[wi]], q=S.pool)
                    for k in range(4):
                        G("tensor_tensor", [b_wc[wi], o["b_gbc"]], [b_wc[wi]], out=wdc[wi][:, k, :], in0=wdc[wi][:, k, :], in1=o["gbc"][:], op=ALU.mult)
                    first = (e == 0 and fc == 0)
                    for tg in range(SGT // 4):
                        hTg = hTa[:, :, tg * 512:(tg + 1) * 512]
                        for sc_ in range(4):
                            for k in range(8):
                                MM(o["pA"][:, :], wgc[wi][:, k, sc_ * 128:(sc_ + 1) * 128], hTg[:, k, :], k == 0, [b_wc[wi], b_hTa[tg]], [o["bpA"]])
                            for k in range(8):
                                MM(o["pB"][:, :], wuc[wi][:, k, sc_ * 128:(sc_ + 1) * 128], hTg[:, k, :], k == 0, [b_wc[wi], b_hTa[tg]], [o["bpB"]])
                            si = sc_ % 2
                            A(sg[si][:], o["pA"][:, :], AF.Silu, [o["bpA"]], [b_sg[si]])
                            V("tensor_tensor", [b_sg[si], o["bpB"]], [b_hid], out=hid[:, sc_, :], in0=sg[si][:], in1=o["pB"][:, :], op=ALU.mult)
                        for tl in range(4):
                            tt = tg * 4 + tl
                            for hf in range(2):
                                pd, bpd = o["pD"][hf], o["bpD"][hf]
                                for sc_ in range(4):
                                    MM(pd[:, :], hid[:, sc_, tl * 128:(tl + 1) * 128], wdc[wi][:, sc_, hf * 512:(hf + 1) * 512], sc_ == 0, [b_hid, b_wc[wi]], [bpd])
                                if first:
                                    V("tensor_scalar", [bpd, b_gt], [b_acc[tt]], out=accs[:, tt, hf * 512:(hf + 1) * 512], in0=pd[:, :],
                                      scalar1=gates[:, tt, e:e + 1], scalar2=None, op0=ALU.mult)
                                else:
                                    V("scalar_tensor_tensor", [bpd, b_gt, b_acc[tt]], [b_acc[tt]], out=accs[:, tt, hf * 512:(hf + 1) * 512], in0=pd[:, :],
                                      scalar=gates[:, tt, e:e + 1], in1=accs[:, tt, hf * 512:(hf + 1) * 512], op0=ALU.mult, op1=ALU.add)
            for tt in range(SGT):
                t = sgi * SGT + tt
                xi = nx % 2; nx += 1
                LD(xt[xi][:], xin[t * 128:(t + 1) * 128, :], [b_xt[xi]], R=[bxin[t]])
                V("scalar_tensor_tensor", [b_xt[xi], b_acc[tt]], [b_acc[tt]], out=accs[:, tt, :], in0=xt[xi][:], scalar=ALPHA, in1=accs[:, tt, :],
                  op0=ALU.mult, op1=ALU.add)
                self.ffn_finish_tile(o, accs[:, tt, :], b_acc[tt], xout[t * 128:(t + 1) * 128, :], bxout[t], is_out)
        S.barrier()


_PROG = {}
IN_KEYS = ["w_ada", "b_ada", "w_in", "nsa_cmp_pos", "nsa_cmp_w1", "nsa_cmp_w2", "dsa_kv_norm", "dsa_w_uk", "dsa_w_uv",
           "sgu_norm_g", "sgu_norm_b", "sgu_w", "sgu_b", "w_out", "ln1_g", "ln1_b", "ln2_g", "ln2_b",
           "ffn_w_gate", "ffn_w_up", "ffn_w_down", "moe_w_router", "moe_b_router", "moe_w_gate", "moe_w_up", "moe_w_down"]


def make_in_maps(inputs, ncores):
    shared = {}
    for k in IN_KEYS:
        a = np.ascontiguousarray(np.asarray(inputs[k], dtype=np.float32))
        if k in ("sgu_norm_g", "sgu_norm_b"):
            a = a.reshape(a.shape[0], -1)
        shared[k] = a
    x = np.asarray(inputs["x"], dtype=np.float32)
    c = np.asarray(inputs["c"], dtype=np.float32)
    pos = np.asarray(inputs["positions"], dtype=np.int32)
    maps = []
    for b in range(ncores):
        m = dict(shared)
        m["x"] = np.ascontiguousarray(x[b])
        m["c"] = np.ascontiguousarray(c[b:b + 1])
        m["pos"] = np.ascontiguousarray(pos[b:b + 1])
        maps.append(m)
    return maps


def kernel(**inputs):
    x = np.asarray(inputs["x"])
    Bn, SL, _ = x.shape
    key = (SL,)
    if key not in _PROG:
        _PROG[key] = Prog(SL)
    prog = _PROG[key]
    maps = make_in_maps(inputs, Bn)
    res = run_bass_kernel_spmd(prog.nc, maps, core_ids=list(range(Bn)))
    return np.stack([np.asarray(r["y"], dtype=np.float32) for r in res.results], axis=0)
```

```python
import math
import numpy as np
from contextlib import ExitStack
import concourse.bass as bass
import concourse.mybir as mybir
from concourse.bass_utils import run_bass_kernel_spmd

F32 = mybir.dt.float32
BF16 = mybir.dt.bfloat16
I32 = mybir.dt.int32
AF = mybir.ActivationFunctionType
ALU = mybir.AluOpType
AX = mybir.AxisListType

D = 1024
DEPTH = 2
ALPHA = (2 * DEPTH) ** 0.25
Q0, KV0, G0, DQ0, CKV0, IQ0, IK0, IW0, SGU0, INW = 0, 384, 1152, 1170, 1554, 1682, 1810, 1842, 1846, 2358
D_FF = 2816
E_FF = 3584
NEXP = 8
NEG = -30000.0
FILL = -1.0e30
NBIS = 16


class Buf:
    __slots__ = ("name", "w", "r", "excl")

    def __init__(self, name="", excl=False):
        self.name = name
        self.w = None
        self.r = {}
        self.excl = excl


def PBuf():
    return Buf("psum", True)


class Eng:
    def __init__(self, key, eng, is_pe=False):
        self.key, self.eng, self.is_pe = key, eng, is_pe
        self.sem = None
        self.count = 0
        self.seen = {}
        self.epoch = 0
        self.dma_sems, self.dma_vals, self.dma_rr = [], [], 0
        self.n_inst = 0
        self.n_wait = 0


class Sched:
    EPOCH = 30000

    def __init__(self, nc, stack, n_dma_sems=10):
        self.nc, self.stack = nc, stack
        self.pe = Eng("pe", nc.tensor, True)
        self.act = Eng("act", nc.scalar)
        self.dve = Eng("dve", nc.vector)
        self.pool = Eng("pool", nc.gpsimd)
        self.sp = Eng("sp", nc.sync)
        self.engs = [self.pe, self.act, self.dve, self.pool, self.sp]
        self.nsem = 0
        for e in self.engs:
            e.sem = self._newsem(e.key + "_p0")
        for e in (self.sp, self.pool):
            for i in range(n_dma_sems):
                e.dma_sems.append(self._newsem(f"{e.key}_d{i}"))
                e.dma_vals.append(0)
        self.out_tokens = []

    def _newsem(self, name):
        self.nsem += 1
        return self.stack.enter_context(self.nc.semaphore(name))

    def _wait(self, E, tok):
        sem, val, src = tok
        if src == E.key and E.is_pe:
            return
        k = id(sem)
        if E.seen.get(k, 0) >= val:
            return
        E.eng.wait_ge(sem, val)
        E.n_wait += 1
        E.seen[k] = val

    def _deps(self, E, reads, writes):
        for b in reads:
            if b.w is not None:
                self._wait(E, b.w)
            if b.excl:
                for k, t in b.r.items():
                    if k != E.key:
                        self._wait(E, t)
        for b in writes:
            if b.w is not None:
                self._wait(E, b.w)
            for t in b.r.values():
                self._wait(E, t)

    def _mark(self, tok, reads, writes):
        for b in reads:
            b.r[tok[2]] = tok
        for b in writes:
            b.w = tok
            b.r = {}

    def op(self, E, fn, reads=(), writes=()):
        self._deps(E, reads, writes)
        if E.count >= self.EPOCH:
            E.epoch += 1
            E.sem = self._newsem(f"{E.key}_p{E.epoch}")
            E.count = 0
        inst = fn(E.eng)
        E.count += 1
        E.n_inst += 1
        inst.then_inc(E.sem, 1)
        tok = (E.sem, E.count, E.key)
        self._mark(tok, reads, writes)
        return tok

    def dma(self, Q, out, in_, reads=(), writes=(), is_output=False, **kw):
        self._deps(Q, reads, writes)
        i = Q.dma_rr
        Q.dma_rr = (Q.dma_rr + 1) % len(Q.dma_sems)
        sem = Q.dma_sems[i]
        if Q.dma_vals[i] > 0:
            self._wait(Q, (sem, Q.dma_vals[i], "dma"))
        inst = Q.eng.dma_start(out=out, in_=in_, **kw)
        Q.dma_vals[i] += 16
        inst.then_inc(sem, 16)
        Q.n_inst += 1
        tok = (sem, Q.dma_vals[i], "dma%s%d" % (Q.key, i))
        self._mark(tok, reads, writes)
        if is_output:
            self.out_tokens.append(tok)
        return tok

    def all_tokens(self):
        toks = []
        for Q in (self.sp, self.pool):
            for i, sem in enumerate(Q.dma_sems):
                if Q.dma_vals[i] > 0:
                    toks.append((sem, Q.dma_vals[i], "dma"))
        for e in self.engs:
            if e.count > 0:
                toks.append((e.sem, e.count, e.key + "_bar"))
        return toks

    def barrier(self):
        toks = self.all_tokens()
        for E in self.engs:
            for t in toks:
                if t[2] == E.key + "_bar":
                    continue
                self._wait(E, t)

    def finish(self):
        for t in self.all_tokens():
            self._wait(self.sp, t)


class Prog:
    def __init__(self, S_len, n_layers=DEPTH, stop_after=None, debug=False):
        self.debug = debug
        self.KTOP = min(256, S_len // 4)
        self.SL = S_len
        self.NT = S_len // 128
        self.NG = S_len // 512
        self.n_layers = n_layers
        self.stop_after = stop_after
        self.nc = bass.Bass("TRN2", target_bir_lowering=False)
        self.build()

    def sb(self, st, name, shape, dt):
        self._uid = getattr(self, "_uid", 0) + 1
        return st.enter_context(self.nc.sbuf_tensor(f"{name}_{self._uid}", shape, dt))

    def ps(self, st, name, shape, dt):
        self._uid = getattr(self, "_uid", 0) + 1
        return st.enter_context(self.nc.psum_tensor(f"{name}_{self._uid}", shape, dt))

    def MM(self, out, lhsT, rhs, start, R, W, stop=True):
        self.S.op(self.S.pe, lambda e: e.matmul(out, lhsT=lhsT, rhs=rhs, start=start, stop=stop,
                                                skip_group_check=True), R, W)

    def TR(self, out, in_, ident, R, W):
        self.S.op(self.S.pe, lambda e: e.transpose(out=out, in_=in_, identity=ident), R, W)

    def A(self, out, in_, func, R, W, **kw):
        self.S.op(self.S.act, lambda e: e.activation(out=out, in_=in_, func=func, **kw), R, W)

    def V(self, name, R, W, **kw):
        self.S.op(self.S.dve, lambda e: getattr(e, name)(**kw), R, W)

    def G(self, name, R, W, **kw):
        if name == "affine_select" and isinstance(kw.get("fill"), (int, float)) and kw["fill"] != 0.0:
            regs = self.__dict__.setdefault("_fillregs", {})
            v = float(kw["fill"])
            if v not in regs:
                regs[v] = self.nc.gpsimd.to_reg(v)
            kw["fill"] = regs[v]
        self.S.op(self.S.pool, lambda e: getattr(e, name)(**kw), R, W)

    def LD(self, out, in_, W, R=(), q=None, **kw):
        self.S.dma(q or self.S.sp, out, in_, reads=R, writes=W, **kw)

    def dram_in(self, name, shape, dt=F32):
        return self.nc.dram_tensor(name, list(shape), dt, kind="ExternalInput").ap()

    def build(self):
        nc = self.nc
        SL, NT, NG = self.SL, self.NT, self.NG
        I = {}
        I["x"] = self.dram_in("x", [SL, D])
        I["c"] = self.dram_in("c", [1, D])
        I["pos"] = self.dram_in("pos", [1, SL], I32)
        I["w_ada"] = self.dram_in("w_ada", [DEPTH, D, 6 * D])
        I["b_ada"] = self.dram_in("b_ada", [DEPTH, 6 * D])
        I["w_in"] = self.dram_in("w_in", [DEPTH, D, INW])
        I["cmp_pos"] = self.dram_in("nsa_cmp_pos", [DEPTH, 2, 32, 64])
        I["cmp_w1"] = self.dram_in("nsa_cmp_w1", [DEPTH, 2, 32, 64, 64])
        I["cmp_w2"] = self.dram_in("nsa_cmp_w2", [DEPTH, 2, 64, 64])
        I["kvn"] = self.dram_in("dsa_kv_norm", [DEPTH, 128])
        I["w_uk"] = self.dram_in("dsa_w_uk", [DEPTH, 128, 64])
        I["w_uv"] = self.dram_in("dsa_w_uv", [DEPTH, 128, 64])
        I["sgu_g"] = self.dram_in("sgu_norm_g", [DEPTH, 256])
        I["sgu_b"] = self.dram_in("sgu_norm_b", [DEPTH, 256])
        I["sgu_w"] = self.dram_in("sgu_w", [DEPTH, 4, 128, 128])
        I["sgu_bs"] = self.dram_in("sgu_b", [DEPTH, 4, 128])
        I["w_out"] = self.dram_in("w_out", [DEPTH, D, D])
        for n in ("ln1_g", "ln1_b", "ln2_g", "ln2_b"):
            I[n] = self.dram_in(n, [DEPTH, D])
        I["ffn_wg"] = self.dram_in("ffn_w_gate", [1, D, D_FF])
        I["ffn_wu"] = self.dram_in("ffn_w_up", [1, D, D_FF])
        I["ffn_wd"] = self.dram_in("ffn_w_down", [1, D_FF, D])
        I["moe_wr"] = self.dram_in("moe_w_router", [1, D, NEXP])
        I["moe_br"] = self.dram_in("moe_b_router", [1, NEXP])
        I["moe_wg"] = self.dram_in("moe_w_gate", [1, NEXP, D, E_FF])
        I["moe_wu"] = self.dram_in("moe_w_up", [1, NEXP, D, E_FF])
        I["moe_wd"] = self.dram_in("moe_w_down", [1, NEXP, E_FF, D])
        self.I = I
        self.y = nc.dram_tensor("y", [SL, D], F32, kind="ExternalOutput").ap()
        if self.debug:
            self.dbg = nc.dram_tensor("dbg", [SL, D], F32, kind="ExternalOutput").ap()
        self.rope_d = nc.dram_tensor("rope_d", [4, 128, SL], F32, kind="Internal").ap()
        self.xa = nc.dram_tensor("xa", [SL, D], F32, kind="Internal").ap()
        self.xb = nc.dram_tensor("xb", [SL, D], F32, kind="Internal").ap()
        self.b_rope = Buf("rope_d")
        self.b_xa = [Buf() for _ in range(NT)]
        self.b_xb = [Buf() for _ in range(NT)]
        self.b_y = [Buf() for _ in range(NT)]

        with ExitStack() as st0:
            self.S = Sched(nc, st0)
            self.setup_consts(st0)
            self.setup_mod(st0)
            self.setup_rope()
            xin, bxin = I["x"], [Buf() for _ in range(NT)]
            for l in range(self.n_layers):
                last = (l == self.n_layers - 1)
                self.S.barrier()
                with ExitStack() as st:
                    self.mixer(st, l, xin, bxin, self.xa, self.b_xa)
                if self.stop_after == ("mix", l):
                    self.copy_out(self.xa, self.b_xa)
                    break
                self.S.barrier()
                dst, bdst = (self.y, self.b_y) if last else (self.xb, self.b_xb)
                with ExitStack() as st:
                    if l % 2 == 0:
                        self.ffn_dense(st, l, self.xa, self.b_xa, dst, bdst, is_out=last)
                    else:
                        self.ffn_moe(st, l, self.xa, self.b_xa, dst, bdst, is_out=last)
                if (not last) and self.stop_after == ("ffn", l):
                    self.copy_out(self.xb, self.b_xb)
                    break
                xin, bxin = self.xb, self.b_xb
            self.S.finish()
            self.stats = {e.key: (e.n_inst, e.n_wait) for e in self.S.engs}

    def copy_out(self, src, bsrc):
        self.S.barrier()
        for t in range(self.NT):
            self.S.dma(self.S.sp, self.y[t * 128:(t + 1) * 128, :], src[t * 128:(t + 1) * 128, :],
                       reads=[bsrc[t]], writes=[self.b_y[t]], is_output=True)

    def setup_consts(self, st):
        C = self.C = {}
        B = self.B = {}

        def mk(name, shape, dt):
            C[name] = self.sb(st, name, shape, dt)
            B[name] = Buf(name)
            return C[name], B[name]

        idf, b = mk("ident_f", [128, 128], F32)
        self.G("memset", [], [b], ap=idf[:], constant=0.0)
        self.G("affine_select", [b], [b], out=idf[:], in_=idf[:], pattern=[[-1, 128]],
               compare_op=ALU.not_equal, fill=1.0, base=0, channel_multiplier=1)
        idb, b2 = mk("ident_b", [128, 128], BF16)
        self.V("tensor_copy", [b], [b2], out=idb[:], in_=idf[:])
        ones, b3 = mk("ones", [128, 128], F32)
        self.G("memset", [], [b3], ap=ones[:], constant=1.0)
        onesb, b3b = mk("onesb", [128, 128], BF16)
        self.G("memset", [], [b3b], ap=onesb[:], constant=1.0)
        zf, bz = mk("ztmp", [128, 384], F32)
        ntc, b4 = mk("negtri_c", [128, 384], BF16)
        self.G("memset", [], [bz], ap=zf[:], constant=0.0)
        self.G("affine_select", [bz], [bz], out=zf[:], in_=zf[:], pattern=[[0, 3], [1, 128]],
               compare_op=ALU.is_ge, fill=NEG, base=0, channel_multiplier=-1)
        self.V("tensor_copy", [bz], [b4], out=ntc[:], in_=zf[:])
        ntw, b5 = mk("negtri_w", [128, 384], BF16)
        self.G("memset", [b4], [bz], ap=zf[:], constant=0.0)
        self.G("affine_select", [bz], [bz], out=zf[:], in_=zf[:], pattern=[[0, 3], [-1, 128]],
               compare_op=ALU.is_ge, fill=NEG, base=-1, channel_multiplier=1)
        self.V("tensor_copy", [bz], [b5], out=ntw[:], in_=zf[:])
        rel, b6 = mk("rel", [128, 128], F32)
        self.G("memset", [], [b6], ap=rel[:], constant=0.0)
        for half in range(2):
            sl = rel[half * 64:(half + 1) * 64, :]
            self.G("memset", [b6], [b6], ap=rel[half * 64:(half + 1) * 64, 62 + half:64 + half], constant=1.0e4)
            if 64 + half < 128:
                self.G("memset", [b6], [b6], ap=rel[half * 64:(half + 1) * 64, 64 + half:128], constant=FILL)
        col0, b7 = mk("col0", [128, 64], F32)
        self.G("memset", [], [b7], ap=col0[:], constant=0.0)
        self.G("memset", [b7], [b7], ap=col0[:, 0:1], constant=1.0e4)
        ovf, b8 = mk("ovf", [128, 2, 64], F32)
        ov, b9 = mk("overlap", [128, 2, 64], BF16)
        self.G("memset", [], [b8], ap=ovf[:], constant=1.0)
        for nt in range(2):
            self.G("affine_select", [b8], [b8], out=ovf[:, nt, :], in_=ovf[:, nt, :], pattern=[[-4, 64]],
                   compare_op=ALU.is_ge, fill=0.0, base=128 * nt + 1, channel_multiplier=1)
            self.G("affine_select", [b8], [b8], out=ovf[:, nt, :], in_=ovf[:, nt, :], pattern=[[4, 64]],
                   compare_op=ALU.is_ge, fill=0.0, base=3 - 128 * nt, channel_multiplier=-1)
        self.V("tensor_copy", [b8], [b9], out=ov[:], in_=ovf[:])
        for nm, dh in (("rot_h", 64), ("rot_i", 32)):
            rm, brm = mk(nm, [128, 128], F32)
            hh = dh // 2
            self.G("memset", [], [brm], ap=rm[:], constant=0.0)
            for c0 in range(0, 128, dh):
                self.G("affine_select", [brm], [brm], out=rm[:, c0:c0 + hh], in_=rm[:, c0:c0 + hh], pattern=[[-1, hh]],
                       compare_op=ALU.not_equal, fill=-1.0, base=-(c0 + hh), channel_multiplier=1)
                self.G("affine_select", [brm], [brm], out=rm[:, c0 + hh:c0 + dh], in_=rm[:, c0 + hh:c0 + dh], pattern=[[-1, hh]],
                       compare_op=ALU.not_equal, fill=1.0, base=-c0, channel_multiplier=1)
        p2, b10 = mk("pow2", [128, NBIS], F32)
        for k in range(NBIS):
            self.G("memset", [b10], [b10], ap=p2[:, k:k + 1], constant=2.0 ** -(k + 1))

    def setup_mod(self, st):
        nc, S, C, B, I = self.nc, self.S, self.C, self.B, self.I
        modcol = self.sb(st, "modcol", [128, DEPTH, 4, 8], F32)
        self.gate_d = nc.dram_tensor("gate_d", [DEPTH * 2, D], F32, kind="Internal").ap()
        self.b_gate = Buf("gate_d")
        self.modcol = modcol
        self.b_mod = Buf("mod")
        with ExitStack() as s2:
            crow = self.sb(s2, "crow", [1, D], F32); b_crow = Buf()
            ccol = self.sb(s2, "ccol", [128, 8], F32); b_ccol = Buf()
            modrow = self.sb(s2, "modrow", [1, 6 * D], F32); b_modrow = Buf()
            brow = self.sb(s2, "brow", [1, 6 * D], F32); b_brow = Buf()
            wa = [self.sb(s2, f"wa{i}", [128, 8, 512], F32) for i in range(2)]
            b_wa = [Buf(), Buf()]
            pc = self.ps(s2, "pc", [128, 512], F32); b_pc = PBuf()
            pr = [self.ps(s2, f"pr{i}", [128, 512], F32) for i in range(2)]
            b_pr = [PBuf(), PBuf()]
            self.LD(crow[:], I["c"], [b_crow])
            for k in range(8):
                self.MM(pc[:, k:k + 1], crow[0:1, k * 128:(k + 1) * 128], C["ones"][0:1, 0:1], True,
                        [b_crow, B["ones"]], [b_pc])
            self.A(ccol[:], pc[:, 0:8], AF.Silu, [b_pc], [b_ccol])
            cnt = 0
            for l in range(self.n_layers):
                self.LD(brow[:], I["b_ada"][l:l + 1, :], [b_brow])
                for j in range(12):
                    w = wa[cnt % 2]; bw = b_wa[cnt % 2]; p = pr[cnt % 2]; bp = b_pr[cnt % 2]
                    cnt += 1
                    self.LD(w[:], I["w_ada"][l, :, j * 512:(j + 1) * 512].rearrange("(k p) n -> p k n", p=128), [bw])
                    for k in range(8):
                        self.MM(p[0:1, :], ccol[:, k:k + 1], w[:, k, :], k == 0, [b_ccol, bw], [bp])
                    self.V("tensor_tensor", [bp, b_brow], [b_modrow], out=modrow[0:1, j * 512:(j + 1) * 512],
                           in0=p[0:1, :], in1=brow[0:1, j * 512:(j + 1) * 512], op=ALU.add)
                for vi, seg in enumerate((0, 1, 3, 4)):
                    for k in range(8):
                        self.MM(pc[:, 16 + vi * 8 + k:16 + vi * 8 + k + 1],
                                modrow[0:1, seg * D + k * 128:seg * D + (k + 1) * 128], C["ones"][0:1, 0:1], True,
                                [b_modrow, B["ones"]], [b_pc])
                for vi in range(4):
                    if vi % 2 == 0:
                        self.V("tensor_copy", [b_pc], [self.b_mod], out=modcol[:, l, vi, :], in_=pc[:, 16 + vi * 8:24 + vi * 8])
                    else:
                        self.V("tensor_scalar", [b_pc], [self.b_mod], out=modcol[:, l, vi, :], in0=pc[:, 16 + vi * 8:24 + vi * 8],
                               scalar1=1.0, scalar2=None, op0=ALU.add)
                self.LD(self.gate_d[2 * l:2 * l + 1, :], modrow[0:1, 2 * D:3 * D], [self.b_gate], R=[b_modrow])
                self.LD(self.gate_d[2 * l + 1:2 * l + 2, :], modrow[0:1, 5 * D:6 * D], [self.b_gate], R=[b_modrow])
            S.barrier()

    def setup_rope(self):
        S, I = self.S, self.I
        with ExitStack() as st:
            pidx = self.sb(st, "pidx", [128, 1], I32); b_p = Buf()
            pm = self.sb(st, "pm", [128, 2], I32); b_pm = Buf()
            pmf = self.sb(st, "pmf", [128, 2], F32); b_pmf = Buf()
            inv = self.sb(st, "inv", [128, 2], F32); b_inv = Buf()
            self.G("iota", [], [b_p], out=pidx[:], pattern=[[0, 1]], base=0, channel_multiplier=1)
            self.V("tensor_single_scalar", [b_p], [b_pm], out=pm[:, 0:1], in_=pidx[:], scalar=31, op=ALU.bitwise_and)
            self.V("tensor_single_scalar", [b_p], [b_pm], out=pm[:, 1:2], in_=pidx[:], scalar=15, op=ALU.bitwise_and)
            self.V("tensor_copy", [b_pm], [b_pmf], out=pmf[:], in_=pm[:])
            lnth = math.log(10000.0)
            self.A(inv[:, 0:1], pmf[:, 0:1], AF.Exp, [b_pmf], [b_inv], scale=-lnth / 32.0)
            self.A(inv[:, 1:2], pmf[:, 1:2], AF.Exp, [b_pmf], [b_inv], scale=-lnth / 16.0)
            posi = self.sb(st, "posi", [128, 512], I32); b_posi = Buf()
            posf = self.sb(st, "posf", [128, 512], F32); b_posf = Buf()
            u = self.sb(st, "ru", [128, 512], F32); b_u = Buf()
            ki = self.sb(st, "rki", [128, 512], I32); b_ki = Buf()
            kf = self.sb(st, "rkf", [128, 512], F32); b_kf = Buf()
            t1 = self.sb(st, "rt1", [128, 512], F32); b_t1 = Buf()
            tab = [self.sb(st, f"rtab{i}", [128, 512], F32) for i in range(2)]
            b_tab = [Buf(), Buf()]
            n = 0
            for c in range(self.NG):
                self.LD(posi[:], I["pos"][0:1, c * 512:(c + 1) * 512].to_broadcast([128, 512]), [b_posi])
                self.V("tensor_copy", [b_posi], [b_posf], out=posf[:], in_=posi[:])
                for ti in range(4):
                    fi = ti // 2
                    off = 0.25 if ti % 2 == 0 else 0.0
                    self.V("tensor_scalar", [b_posf, b_inv], [b_u], out=u[:], in0=posf[:], scalar1=inv[:, fi:fi + 1],
                           scalar2=1.0 / (2 * math.pi), op0=ALU.mult, op1=ALU.mult)
                    if off:
                        self.V("tensor_scalar", [b_u], [b_u], out=u[:], in0=u[:], scalar1=off, scalar2=None, op0=ALU.add)
                    self.V("tensor_copy", [b_u], [b_ki], out=ki[:], in_=u[:])
                    self.V("tensor_copy", [b_ki], [b_kf], out=kf[:], in_=ki[:])
                    self.V("tensor_tensor", [b_u, b_kf], [b_u], out=u[:], in0=u[:], in1=kf[:], op=ALU.subtract)
                    self.V("tensor_single_scalar", [b_u], [b_t1], out=t1[:], in_=u[:], scalar=0.5, op=ALU.is_gt)
                    self.V("tensor_tensor", [b_u, b_t1], [b_u], out=u[:], in0=u[:], in1=t1[:], op=ALU.subtract)
                    self.V("tensor_single_scalar", [b_u], [b_t1], out=t1[:], in_=u[:], scalar=-0.5, op=ALU.is_lt)
                    self.V("tensor_tensor", [b_u, b_t1], [b_u], out=u[:], in0=u[:], in1=t1[:], op=ALU.add)
                    tb = tab[n % 2]; bt = b_tab[n % 2]; n += 1
                    self.A(tb[:], u[:], AF.Sin, [b_u], [bt], scale=6.28318)
                    self.LD(self.rope_d[ti, :, c * 512:(c + 1) * 512], tb[:], [self.b_rope], R=[bt])
            S.barrier()

    def ln_stats(self, xt, b_x, mv, b_mv, rstd, stats, b_st, eps=1e-5):
        for c2 in range(2):
            self.V("bn_stats", [b_x], [b_st], out=stats[:, c2, :], in_=xt[:, c2 * 512:(c2 + 1) * 512])
        self.V("bn_aggr", [b_st], [b_mv], out=mv[:, 0:2], in_=stats[:].rearrange("p a b -> p (a b)"))
        self.V("tensor_scalar", [b_mv], [b_mv], out=rstd, in0=mv[:, 1:2], scalar1=eps, scalar2=None, op0=ALU.add)
        self.A(rstd, rstd, AF.Sqrt, [b_mv], [b_mv])
        self.V("reciprocal", [b_mv], [b_mv], out=rstd, in_=rstd)

    def mixer(self, st, l, xin, bxin, xout, bxout):
        nc, S, C, B, I = self.nc, self.S, self.C, self.B, self.I
        SL, NT, NG = self.SL, self.NT, self.NG
        sb = lambda n, s, d: self.sb(st, n, s, d)
        MM, TR, A, V, G, LD = self.MM, self.TR, self.A, self.V, self.G, self.LD
        PQ = S.pool
        SA = max(SL, 4096)

        pA = self.ps(st, "pA", [128, 512], F32); b_pA = PBuf()
        pB = self.ps(st, "pB", [128, 512], F32); b_pB = PBuf()
        pS = [self.ps(st, f"pS{i}", [128, 512], F32) for i in range(2)]; b_pS = [PBuf(), PBuf()]
        pM = self.ps(st, "pM", [128, 1024], BF16); _bpm = PBuf(); b_pM = [_bpm, _bpm]
        pO = [self.ps(st, f"pO{i}", [128, 512], F32) for i in range(3)]; b_pO = [PBuf(), PBuf(), PBuf()]

        acc = sb("acc", [128, SA], F32); b_acc = Buf()
        dmask = sb("dmask", [128, SA], BF16); b_dm = Buf()
        win = sb("win", [128, 8, INW], BF16); b_win = Buf()
        for (a, b) in ((384, 1179), (1179, INW)):
            LD(win[:, :, a:b], I["w_in"][l, :, a:b].rearrange("(k p) n -> p k n", p=128), [b_win], q=PQ)
        for k in range(8):
            for r in range(3):
                LD(win[:, k, r * 128:(r + 1) * 128].rearrange("p (g d) -> p g d", g=2),
                   I["w_in"][l, k * 128:(k + 1) * 128, 0:384].rearrange("p (g r d) -> p g r d", g=2, r=3)[:, :, r, :], [b_win], q=PQ)
        ik4 = sb("ik4", [128, 8, 128], BF16); b_ik4 = Buf()
        for k in range(8):
            for r4 in range(4):
                (V if r4 % 2 == 0 else G)("tensor_copy", [b_win], [b_ik4], out=ik4[:, k, r4 * 32:(r4 + 1) * 32], in_=win[:, k, IK0:IK0 + 32])
        wout = sb("wout", [128, 8, D], BF16); b_wout = Buf()
        LD(wout[:], I["w_out"][l].rearrange("(k p) n -> p k n", p=128), [b_wout], q=PQ)
        lng = sb("lng", [128, D], F32); lnb = sb("lnb", [128, D], F32); b_ln = Buf()
        LD(lng[:], I["ln1_g"][l:l + 1, :].to_broadcast([128, D]), [b_ln])
        LD(lnb[:], I["ln1_b"][l:l + 1, :].to_broadcast([128, D]), [b_ln])
        w1 = sb("cw1", [128, 2, 32, 64], BF16); w2 = sb("cw2", [128, 2, 64], BF16); b_cw = Buf()
        for g in range(2):
            LD(w1[64 * g:64 * g + 64], I["cmp_w1"][l].rearrange("j l d e -> d j l e"), [b_cw], q=PQ)
            LD(w2[64 * g:64 * g + 64], I["cmp_w2"][l].rearrange("j e f -> e j f"), [b_cw], q=PQ)
        cbias = sb("cbias", [128, 2], F32); b_cb = Buf()
        wuk = sb("wuk", [128, 128], BF16); wuv = sb("wuv", [128, 64], BF16); b_wu = Buf()
        for hf in range(2):
            LD(wuk[:, hf * 64:(hf + 1) * 64], I["w_uk"][l], [b_wu], q=PQ)
        LD(wuv[:], I["w_uv"][l], [b_wu], q=PQ)
        kvg = sb("kvg", [128, 128], F32); b_kvg = Buf()
        LD(kvg[:], I["kvn"][l:l + 1, :].to_broadcast([128, 128]), [b_kvg])
        sgg = sb("sgg", [128, 256], F32); sgb = sb("sgb", [128, 256], F32); b_sg = Buf()
        LD(sgg[:], I["sgu_g"][l:l + 1, :].to_broadcast([128, 256]), [b_sg])
        LD(sgb[:], I["sgu_b"][l:l + 1, :].to_broadcast([128, 256]), [b_sg])
        wsT = sb("wsT", [128, 4, 128], BF16); b_ws = Buf()
        bs = sb("sgbs", [128, 4], F32); b_bs = Buf()
        with ExitStack() as s2:
            gbc = self.sb(s2, "gbc", [128, D], F32); b_gbc = Buf()
            LD(gbc[:], self.gate_d[2 * l:2 * l + 1, :].to_broadcast([128, D]), [b_gbc], R=[self.b_gate])
            for k in range(8):
                V("tensor_tensor", [b_wout, b_gbc], [b_wout], out=wout[:, k, :], in0=wout[:, k, :], in1=gbc[:], op=ALU.mult)
            w1n = self.sb(s2, "w1n", [32, 2, 64, 64], F32); posn = self.sb(s2, "posn", [32, 2, 64], F32); b_n = Buf()
            LD(w1n[:], I["cmp_w1"][l].rearrange("j l d e -> l j d e"), [b_n])
            LD(posn[:], I["cmp_pos"][l].rearrange("j l d -> l j d"), [b_n])
            for j in range(2):
                for g in range(2):
                    for d in range(64):
                        MM(pA[64 * g:64 * g + 64, j:j + 1], w1n[:, j, d, :], posn[:, j, d:d + 1], d == 0, [b_n], [b_pA])
            V("tensor_copy", [b_pA], [b_cb], out=cbias[:], in_=pA[:, 0:2])
            wsn = self.sb(s2, "wsn", [128, 4, 128], F32); b_wsn = Buf()
            wsf = self.sb(s2, "wsf", [128, 4, 128], F32); b_wsf = Buf()
            bsn = self.sb(s2, "bsn", [4, 128], F32); b_bsn = Buf()
            LD(wsn[:], I["sgu_w"][l].rearrange("g t s -> t g s"), [b_wsn])
            LD(bsn[:], I["sgu_bs"][l], [b_bsn])
            for g in range(4):
                TR(pB[:, g * 128:(g + 1) * 128], wsn[:, g, :], C["ident_f"][:], [b_wsn, B["ident_f"]], [b_pB])
            V("tensor_copy", [b_pB], [b_wsf], out=wsf[:].rearrange("p g t -> p (g t)"), in_=pB[:, :])
            G("affine_select", [b_wsf], [b_wsf], out=wsf[:], in_=wsf[:], pattern=[[0, 4], [1, 128]],
              compare_op=ALU.is_ge, fill=0.0, base=0, channel_multiplier=-1)
            V("tensor_copy", [b_wsf], [b_ws], out=wsT[:], in_=wsf[:])
            TR(pS[0][:, 0:4], bsn[:, :], C["ident_f"][0:4, 0:4], [b_bsn, B["ident_f"]], [b_pS[0]])
            V("tensor_copy", [b_pS[0]], [b_bs], out=bs[:], in_=pS[0][:, 0:4])
            S.barrier()

        xcT = sb("xcT", [128, 2, 528], BF16); b_xcT = Buf()
        kselT = sb("kselT", [128, SL], BF16); kwinT = sb("kwinT", [128, 1024], BF16)
        kdsT = sb("kdsT", [128, SL], BF16); ikT = sb("ikT", [128, SL], BF16)
        vsel = sb("vsel", [128, NT, 2, 65], BF16); vwin = sb("vwin", [128, 8, 2, 65], BF16)
        vds = sb("vds", [128, NT, 65], BF16)
        b_kv = [Buf() for _ in range(NG)]
        b_kw = [Buf(), Buf()]
        b_kv0 = Buf()
        G("memset", [], [b_kv0], ap=vsel[:, :, :, 64:65], constant=1.0)
        G("memset", [], [b_kv0], ap=vwin[:, :, :, 64:65], constant=1.0)
        G("memset", [], [b_kv0], ap=vds[:, :, 64:65], constant=1.0)
        hidT = sb("hidT", [128, 2, 256], BF16); kcT = sb("kcT", [128, 256], BF16)
        vcmp = sb("vcmp", [128, 2, 2, 129], BF16); b_cmp = Buf()
        G("memset", [], [b_cmp], ap=hidT[:], constant=0.0)
        G("memset", [b_cmp], [b_cmp], ap=kcT[:], constant=0.0)
        G("memset", [b_cmp], [b_cmp], ap=vcmp[:], constant=0.0)
        G("memset", [b_cmp], [b_cmp], ap=vcmp[:, :, :, 64:65], constant=1.0)
        for g in range(2):
            G("tensor_copy", [b_cmp, B["overlap"]], [b_cmp], out=vcmp[:, :, g, 65:129], in_=C["overlap"][:])

        xs = [sb("xs0", [128, D], F32)] * 2; _bxs = Buf(); b_xs = [_bxs, _bxs]
        junk = xs[0][:].bitcast(mybir.dt.uint8); b_junk = _bxs
        xr = [sb("xr0", [128, D], F32)] * 2; _bxr = Buf(); b_xr = [_bxr, _bxr]
        stats2 = sb("stats2", [128, 2, 6], F32); b_st2 = Buf()
        mv2 = sb("mv2", [128, 4], F32); b_mv2 = Buf()
        stats = sb("stats", [128, 2, 6], F32); b_st = Buf()
        mv = sb("mv", [128, 4], F32); b_mv = Buf()
        mv3 = sb("mv3", [128, 2], F32); b_mv3 = Buf()
        rt = acc[:, 2048:4096].rearrange("p (a t) -> p a t", a=4); b_rt = b_acc
        hT = dmask[:, 0:4096].rearrange("p (k t) -> p k t", k=8); b_hT = b_dm
        qf = sb("qf", [128, 512], F32); b_qf = Buf()
        rl = [sb(f"rl{i}", [128, 512], F32) for i in range(2)]; b_rl = [Buf(), Buf()]
        t1, b_t1, t2, b_t2 = rl[0], b_rl[0], rl[1], b_rl[1]
        qrT = sb("qrT", [128, 3, 512], BF16); qropT = sb("qropT", [128, 3, 512], BF16)
        dqT = sb("dqT", [128, 3, 512], BF16); iqT = sb("iqT", [64, 2, 512], BF16); b_q = Buf()
        gl = sb("gl", [128, 4, 24], F32); b_gl = Buf()
        ckv = sb("ckv", [128, 128], F32); b_ckv = Buf()
        ckvT = sb("ckvT", [128, 512], BF16); b_ckvT = Buf()
        zt, b_zt = qf, b_qf
        ocs = sb("ocs", [128, 4, 256], BF16); b_ocs = Buf()
        cat = [sb(f"cat{i}", [128, D], BF16) for i in range(2)]; b_cat = [Buf(), Buf()]
        catT = sb("catT", [128, 8, 128], BF16); b_catT = Buf()
        E = [sb(f"E{i}", [128, 768], BF16) for i in range(2)]; b_E = [Buf(), Buf()]
        Pm = [sb(f"Pm{i}", [128, 768], BF16) for i in range(2)]; b_Pm = [Buf(), Buf()]
        sm = sb("sm", [128, 64], F32); b_sm = Buf()
        sm_n = sb("sm_n", [128, 64], F32); b_smn = Buf()
        sm_d = sb("sm_d", [128, 64], F32); b_smd = Buf()
        Ed = [sb(f"Ed{i}", [128, 768], BF16) for i in range(2)]; b_Ed = [Buf(), Buf()]
        b_Eh = [Buf() for _ in range(4)]; b_Ph = [Buf() for _ in range(4)]
        b_Edh = [Buf() for _ in range(4)]; b_Pdh = [Buf(), Buf()]
        Pd = [sb("Pd0", [128, 768], BF16)] * 2; _bpd = Buf(); b_Pd = [_bpd, _bpd]
        oc = sb("oc", [128, 6, 64], F32); b_oc = Buf()
        imp = sb("imp", [128, 6, 64], F32); b_imp = Buf()
        sc = sb("sc", [128, 2, 64], F32); sc2 = sb("sc2", [128, 2, 64], F32); b_sc = Buf()
        m8 = sb("m8", [128, 2, 8], F32); b_m8 = Buf()
        bm = sb("bm", [128, 2, 64], BF16); b_bm = Buf()
        bx = sb("bx", [128, 2, 128], BF16); b_bx = [Buf(), Buf()]
        bst = sb("bst", [128, 8 + NBIS], F32); b_bst = Buf()
        bst2 = sb("bst2", [128, 16], F32); b_bs2 = Buf()
        wsg = sb("wsg", [128, 12], F32); b_wsg = Buf()
        sg = sb("sgt", [128, 4, 256], F32); b_sgt = Buf()
        vln = sb("vln", [128, 256], BF16); b_vln = Buf()

        nE = [0]

        def nextE():
            i = nE[0] % 2
            nE[0] += 1
            return i

        nEd = [0]

        def nextEd():
            i_ = nEd[0] % 2
            nEd[0] += 1
            return i_

        nxs = 0
        import os
        MS = float(os.environ.get("MIXSTOP", "9"))
        for gi in range(NG):
            gs = slice(gi * 512, (gi + 1) * 512)
            wsl = slice((gi % 2) * 512, (gi % 2) * 512 + 512)
            KVW = [b_kv[gi]]
            KWW = [b_kw[gi % 2]]
            LD(rt, self.rope_d[:, :, gs].rearrange("a p t -> p a t"), [b_rt], R=[self.b_rope])
            for tl in range(4):
                t = gi * 4 + tl
                xi = nxs % 2; nxs += 1
                LD(xs[xi][:], xin[t * 128:(t + 1) * 128, :], [b_xs[xi]], R=[bxin[t]])
                self.ln_stats(xs[xi], b_xs[xi], mv, b_mv, mv[:, 2:3], stats, b_st)
                V("tensor_scalar", [b_xs[xi], b_mv], [b_xs[xi]], out=xs[xi][:], in0=xs[xi][:], scalar1=mv[:, 0:1], scalar2=mv[:, 2:3],
                  op0=ALU.subtract, op1=ALU.mult)
                for hf in range(2):
                    pp, bpp = (pA, b_pA) if hf == 0 else (pB, b_pB)
                    for k4 in range(4):
                        TR(pp[:, k4 * 128:(k4 + 1) * 128], xs[xi][:, (hf * 4 + k4) * 128:(hf * 4 + k4 + 1) * 128], C["ident_f"][:],
                           [b_xs[xi], B["ident_f"]], [bpp])
                    for k4 in range(4):
                        k = hf * 4 + k4
                        A(hT[:, k, tl * 128:(tl + 1) * 128], pp[:, k4 * 128:(k4 + 1) * 128], AF.Identity, [bpp, self.b_mod], [b_hT],
                          bias=self.modcol[:, l, 0, k:k + 1], scale=self.modcol[:, l, 1, k:k + 1])

            if MS <= 1:
                S.barrier(); return
            def proj_fm(wcols_fn, M, pp, bpp):
                for k in range(8):
                    MM(pp[0:M, :], wcols_fn(k), hT[:, k, :], k == 0, [b_win, b_ik4, b_hT], [bpp])

            def rope_from_pA(dst, M, tab, Rm, RB):
                RO = int(os.environ.get("ROPE_OFF", "0"))
                if RO == 1:
                    A(dst[0], pA[0:M, :], AF.Copy, [b_pA], dst[1]); return
                A(qf[0:M, :], pA[0:M, :], AF.Copy, [b_pA], [b_qf])
                if RO == 2:
                    A(dst[0], pA[0:M, :], AF.Copy, [b_pA], dst[1]); return
                if RO == 3:
                    MM(pB[0:M, :], Rm[0:M, 0:M], qf[0:M, :], True, [RB, b_qf], [b_pB])
                    A(dst[0], pB[0:M, :], AF.Copy, [b_pB], dst[1]); return
                MM(pB[0:M, :], Rm[0:M, 0:M], qf[0:M, :], True, [RB, b_qf], [b_pB])
                V("tensor_tensor", [b_pA, b_rt], [b_t1], out=t1[0:M, :], in0=pA[0:M, :], in1=rt[0:M, tab, :], op=ALU.mult)
                if RO == 4:
                    A(dst[0], t1[0:M, :], AF.Copy, [b_t1], dst[1]); return
                V("tensor_tensor", [b_pB, b_rt], [b_t2], out=t2[0:M, :], in0=pB[0:M, :], in1=rt[0:M, tab + 1, :], op=ALU.mult)
                if RO == 5:
                    A(dst[0], t2[0:M, :], AF.Copy, [b_t2], dst[1]); return
                G("tensor_tensor", [b_t1, b_t2], dst[1], out=dst[0], in0=t1[0:M, :], in1=t2[0:M, :], op=ALU.add)

            def rope_fm(dst, wp, M, tab):
                proj_fm(wp, M, pA, b_pA)
                if tab == 0:
                    rope_from_pA(dst, M, 0, C["rot_h"], B["rot_h"])
                else:
                    rope_from_pA(dst, M, 2, C["rot_i"], B["rot_i"])

            def run_il(gens):
                gens = [g_ for g_ in gens if g_ is not None]
                while gens:
                    for g_ in list(gens):
                        try:
                            next(g_)
                        except StopIteration:
                            gens.remove(g_)

            def g_proj():
                for r in range(3):
                    proj_fm(lambda k: win[:, k, Q0 + r * 128:Q0 + (r + 1) * 128], 128, pA, b_pA)
                    A(qrT[:, r, :], pA[:, :], AF.Copy, [b_pA], [b_q])
                    rope_from_pA((qropT[:, r, :], [b_q]), 128, 0, C["rot_h"], B["rot_h"])
                    yield
                    rope_fm((dqT[:, r, :], [b_q]), lambda k: win[:, k, DQ0 + r * 128:DQ0 + (r + 1) * 128], 128, 0)
                    yield
                rope_fm((kselT[:, gs], KVW), lambda k: win[:, k, KV0 + 256:KV0 + 384], 128, 0)
                yield
                rope_fm((kwinT[:, wsl], KWW), lambda k: win[:, k, KV0 + 512:KV0 + 640], 128, 0)
                yield
                for hh2 in range(2):
                    rope_fm((iqT[:, hh2, :], [b_q]), lambda k: win[:, k, IQ0 + hh2 * 64:IQ0 + hh2 * 64 + 64], 64, 2)
                    yield
                rope_fm((ikT[:, gs], KVW), lambda k: ik4[:, k, :], 128, 2)
                yield
                if gi > 0:
                    G("tensor_copy", [b_xcT], [b_xcT], out=xcT[:, :, 0:16], in_=xcT[:, :, 512:528])
                for j in range(2):
                    proj_fm(lambda k: win[:, k, KV0 + j * 128:KV0 + (j + 1) * 128], 128, pA, b_pA)
                    A(xcT[:, j, 16:528], pA[:, :], AF.Copy, [b_pA], [b_xcT])
                    yield

            def g_tok():
                pT_, bT_ = pO[0], b_pO[0]
                for tl in range(4):
                    t = gi * 4 + tl
                    ts = slice(tl * 128, (tl + 1) * 128)
                    for k in range(8):
                        MM(pT_[:, 0:128], hT[:, k, ts], win[:, k, KV0 + 384:KV0 + 512], k == 0, [b_hT, b_win], [bT_])
                    for k in range(8):
                        MM(pT_[:, 128:256], hT[:, k, ts], win[:, k, KV0 + 640:KV0 + 768], False, [b_hT, b_win], [bT_])
                    for k in range(8):
                        MM(pT_[:, 256:384], hT[:, k, ts], win[:, k, CKV0:CKV0 + 128], False, [b_hT, b_win], [bT_])
                    for k in range(8):
                        MM(pT_[:, 384:402], hT[:, k, ts], win[:, k, G0:G0 + 18], False, [b_hT, b_win], [bT_])
                    for k in range(8):
                        MM(pT_[:, 402:406], hT[:, k, ts], win[:, k, IW0:IW0 + 4], False, [b_hT, b_win], [bT_])
                    yield
                    A(vsel[:, t, :, 0:64], pT_[:, 0:128].rearrange("p (g d) -> p g d", g=2), AF.Copy, [bT_], KVW)
                    A(vwin[:, t % 8, :, 0:64], pT_[:, 128:256].rearrange("p (g d) -> p g d", g=2), AF.Copy, [bT_], KWW)
                    A(gl[:, tl, 0:18], pT_[:, 384:402], AF.Sigmoid, [bT_], [b_gl])
                    V("tensor_scalar", [bT_], [b_gl], out=gl[:, tl, 18:22], in0=pT_[:, 402:406], scalar1=0.5, scalar2=None, op0=ALU.mult)
                    A(ckv[:], pT_[:, 256:384], AF.Square, [bT_], [b_ckv, b_mv3], accum_out=mv3[:, 0:1])
                    V("tensor_scalar", [b_ckv, b_mv3], [b_mv3], out=mv3[:, 0:1], in0=mv3[:, 0:1], scalar1=1.0 / 128, scalar2=1e-6, op0=ALU.mult, op1=ALU.add)
                    A(mv3[:, 0:1], mv3[:, 0:1], AF.Sqrt, [b_mv3], [b_mv3])
                    V("reciprocal", [b_mv3], [b_mv3], out=mv3[:, 0:1], in_=mv3[:, 0:1])
                    V("scalar_tensor_tensor", [bT_, b_mv3, b_kvg], [b_ckv], out=ckv[:], in0=pT_[:, 256:384], scalar=mv3[:, 0:1], in1=kvg[:],
                      op0=ALU.mult, op1=ALU.mult)
                    yield
                    TR(pO[1][:, 0:128], ckv[:], C["ident_f"][:], [b_ckv, B["ident_f"]], [b_pO[1]])
                    A(ckvT[:, ts], pO[1][:, 0:128], AF.Copy, [b_pO[1]], [b_ckvT])
                    MM(pS[0][:, 0:64], ckvT[:, ts], wuv[:], True, [b_ckvT, b_wu], [b_pS[0]])
                    A(vds[:, t, 0:64], pS[0][:, 0:64], AF.Copy, [b_pS[0]], KVW)
                    yield

            def g_sgu():
                pZ, bZ = pO[2], b_pO[2]
                for tl in range(4):
                    ts = slice(tl * 128, (tl + 1) * 128)
                    for k in range(8):
                        MM(pZ[:, :], hT[:, k, ts], win[:, k, SGU0:SGU0 + 512], k == 0, [b_hT, b_win], [bZ])
                    yield
                    z = pZ[:, :]
                    sg01 = sg[:, 0:2, :].rearrange("p a b -> p (a b)")
                    A(sg01, z, AF.Square, [bZ], [b_sgt])
                    V("tensor_scalar", [b_sgt], [b_sgt], out=sg01, in0=sg01, scalar1=0.044715, scalar2=1.0, op0=ALU.mult, op1=ALU.add)
                    V("tensor_tensor", [b_sgt, bZ], [b_sgt], out=sg01, in0=sg01, in1=z, op=ALU.mult)
                    A(sg01, sg01, AF.Sigmoid, [b_sgt], [b_sgt], scale=1.5957691216)
                    V("tensor_tensor", [b_sgt, bZ], [b_sgt], out=sg01, in0=sg01, in1=z, op=ALU.mult)
                    yield
                    u_ = sg[:, 0, :]; v_ = sg[:, 1, :]; v3 = v_.rearrange("p (g d) -> p g d", g=4)
                    s2v = sg[:, 2, :].rearrange("p (g d) -> p g d", g=4)
                    s3v = sg[:, 3, :].rearrange("p (g d) -> p g d", g=4)
                    V("tensor_reduce", [b_sgt], [b_sm], out=sm[:, 32:36], in_=v3, axis=AX.X, op=ALU.add)
                    V("tensor_scalar", [b_sm], [b_sm], out=sm[:, 32:36], in0=sm[:, 32:36], scalar1=1.0 / 64, scalar2=None, op0=ALU.mult)
                    V("tensor_tensor", [b_sgt, b_sm], [b_sgt], out=s2v, in0=v3, in1=sm[:, 32:36].unsqueeze(2).to_broadcast([128, 4, 64]), op=ALU.subtract)
                    V("tensor_tensor", [b_sgt], [b_sgt], out=sg[:, 3, :], in0=sg[:, 2, :], in1=sg[:, 2, :], op=ALU.mult)
                    V("tensor_reduce", [b_sgt], [b_sm], out=sm[:, 36:40], in_=s3v, axis=AX.X, op=ALU.add)
                    V("tensor_scalar", [b_sm], [b_sm], out=sm[:, 36:40], in0=sm[:, 36:40], scalar1=1.0 / 64, scalar2=1e-5, op0=ALU.mult, op1=ALU.add)
                    A(sm[:, 36:40], sm[:, 36:40], AF.Sqrt, [b_sm], [b_sm])
                    V("reciprocal", [b_sm], [b_sm], out=sm[:, 36:40], in_=sm[:, 36:40])
                    yield
                    V("tensor_tensor", [b_sgt, b_sm], [b_sgt], out=s2v, in0=s2v, in1=sm[:, 36:40].unsqueeze(2).to_broadcast([128, 4, 64]), op=ALU.mult)
                    V("tensor_tensor", [b_sgt, b_sg], [b_sgt], out=sg[:, 2, :], in0=sg[:, 2, :], in1=sgg[:], op=ALU.mult)
                    V("tensor_tensor", [b_sgt, b_sg], [b_vln], out=vln[:], in0=sg[:, 2, :], in1=sgb[:], op=ALU.add)
                    for g4 in range(4):
                        MM(pS[1][:, g4 * 64:(g4 + 1) * 64], wsT[:, g4, :], vln[:, g4 * 64:(g4 + 1) * 64], g4 == 0, [b_ws, b_vln], [b_pS[1]])
                    V("tensor_tensor", [b_pS[1], b_bs], [b_sgt], out=s3v, in0=pS[1][:, 0:256].rearrange("p (g d) -> p g d", g=4),
                      in1=bs[:, 0:4].unsqueeze(2).to_broadcast([128, 4, 64]), op=ALU.add)
                    V("tensor_tensor", [b_sgt], [b_ocs], out=ocs[:, tl, :], in0=sg[:, 3, :], in1=u_, op=ALU.mult)
                    yield

            run_il([g_proj(), g_tok(), g_sgu()])
            if MS <= 1.6:
                S.barrier(); return
            MM(pA[:, :], wuk[:, :], ckvT[:, :], True, [b_wu, b_ckvT], [b_pA])
            rope_from_pA((kdsT[:, gs], KVW), 128, 0, C["rot_h"], B["rot_h"])

            if MS <= 2:
                S.barrier(); return
            n0 = max(0, 32 * gi - 1)
            n1 = 32 * gi + 31
            nn = n1 - n0
            lo0 = 16 * (n0 - 32 * gi + 1)
            for j in range(2):
                for g in range(2):
                    pq, bpq = (pA, b_pA) if g == 0 else (pB, b_pB)
                    for ll in range(32):
                        rhs = xcT[64 * g:64 * g + 64, j, lo0 + ll:lo0 + ll + 16 * (nn - 1) + 1:16]
                        MM(pq[64 * g:64 * g + 64, j * 32:j * 32 + nn], w1[64 * g:64 * g + 64, j, ll, :], rhs, ll == 0,
                           [b_cw, b_xcT], [bpq])
            for j in range(2):
                xx = sg[:, 0, 0:nn]
                V("tensor_scalar", [b_pA, b_cb], [b_sgt], out=sg[0:64, 0, 0:nn], in0=pA[0:64, j * 32:j * 32 + nn], scalar1=cbias[0:64, j:j + 1], scalar2=None, op0=ALU.add)
                V("tensor_scalar", [b_pB, b_cb], [b_sgt], out=sg[64:128, 0, 0:nn], in0=pB[64:128, j * 32:j * 32 + nn], scalar1=cbias[64:128, j:j + 1], scalar2=None, op0=ALU.add)
                V("tensor_tensor", [b_sgt], [b_sgt], out=sg[:, 1, 0:nn], in0=xx, in1=xx, op=ALU.mult)
                V("tensor_scalar", [b_sgt], [b_sgt], out=sg[:, 1, 0:nn], in0=sg[:, 1, 0:nn], scalar1=0.044715, scalar2=1.0, op0=ALU.mult, op1=ALU.add)
                V("tensor_tensor", [b_sgt], [b_sgt], out=sg[:, 1, 0:nn], in0=sg[:, 1, 0:nn], in1=xx, op=ALU.mult)
                A(sg[:, 1, 0:nn], sg[:, 1, 0:nn], AF.Sigmoid, [b_sgt], [b_sgt], scale=1.5957691216)
                V("tensor_tensor", [b_sgt], [b_cmp], out=hidT[:, j, n0:n1], in0=sg[:, 1, 0:nn], in1=xx, op=ALU.mult)
            for g in range(2):
                MM(pS[g][64 * g:64 * g + 64, 0:nn], w2[64 * g:64 * g + 64, 0, :], hidT[64 * g:64 * g + 64, 0, n0:n1], True, [b_cw, b_cmp], [b_pS[g]])
                V("tensor_copy", [b_pS[g]], [b_cmp], out=kcT[64 * g:64 * g + 64, n0:n1], in_=pS[g][64 * g:64 * g + 64, 0:nn])
            for a in ((32 * gi - 32, 32 * gi), (32 * gi, 32 * gi + 32)):
                if a[0] < 0:
                    continue
                nt_, po = a[0] // 128, a[0] % 128
                for g in range(2):
                    if po < 96:
                        MM(pS[g][po:po + 32, 64:128], hidT[64 * g:64 * g + 64, 1, a[0]:a[1]], w2[64 * g:64 * g + 64, 1, :], True,
                           [b_cw, b_cmp], [b_pS[g]])
                        V("tensor_copy", [b_pS[g]], [b_cmp], out=vcmp[po:po + 32, nt_, g, 0:64], in_=pS[g][po:po + 32, 64:128])
                    else:
                        MM(pS[g][64:128, 64:128], hidT[64 * g:64 * g + 64, 1, a[0] - 32:a[1]], w2[64 * g:64 * g + 64, 1, :], True,
                           [b_cw, b_cmp], [b_pS[g]])
                        V("tensor_copy", [b_pS[g]], [b_cmp], out=vcmp[64:128, nt_, g, 0:64], in_=pS[g][64:128, 64:128])

            if MS <= 2.5:
                S.barrier(); return
            def nsa_chain(i, tl, ci):
                ts = slice(tl * 128, (tl + 1) * 128)
                KVR = [b_kv[x] for x in range(gi + 1)] + [b_kv0]
                sm = sm_n; b_sm = b_smn
                pOc = [pO[0], pO[2]]; b_pOc = [b_pO[0], b_pO[2]]
                catc, b_catc = cat[ci], b_cat[ci]
                n_nt = 1 if (8 * i + 7) <= 128 else 2
                first = [True, True]
                for nt_ in range(n_nt):
                    ei = nextE()
                    bE2 = [b_Eh[2 * ei], b_Eh[2 * ei + 1]]
                    for g in range(2):
                        MM(pS[g][:, 0:384].rearrange("p (r t) -> p r t", r=3), kcT[64 * g:64 * g + 64, nt_ * 128:(nt_ + 1) * 128],
                           qrT[64 * g:64 * g + 64, :, ts], True, [b_cmp, b_q], [b_pS[g]])
                        A(E[ei][:, g * 384:(g + 1) * 384], pS[g][:, 0:384], AF.Exp, [b_pS[g]], [bE2[g]], scale=0.125)
                    G("affine_select", bE2, bE2, out=E[ei][:, :], in_=E[ei][:, :], pattern=[[0, 6], [1, 128]],
                      compare_op=ALU.is_ge, fill=0.0, base=128 * i - 2048 * nt_ - 31, channel_multiplier=-16)
                    for g in range(2):
                        for r in range(3):
                            MM(pOc[g][:, r * 129:(r + 1) * 129], E[ei][:, (g * 3 + r) * 128:(g * 3 + r + 1) * 128], vcmp[:, nt_, g, :],
                               first[g], [bE2[g], b_cmp], [b_pOc[g]])
                            first[g] = False
                    yield
                for g in range(2):
                    po3 = pOc[g][:, 0:387].rearrange("p (r c) -> p r c", r=3)
                    V("tensor_scalar", [b_pOc[g]], [b_sm], out=sm[:, 40 + g * 3:43 + g * 3], in0=po3[:, :, 64], scalar1=0.0, scalar2=None, op0=ALU.is_equal)
                    V("tensor_tensor", [b_pOc[g], b_sm], [b_sm], out=sm[:, g * 3:g * 3 + 3], in0=po3[:, :, 64], in1=sm[:, 40 + g * 3:43 + g * 3], op=ALU.add)
                V("reciprocal", [b_sm], [b_sm], out=sm[:, 0:6], in_=sm[:, 0:6])
                glv = gl[:, tl, 0:18].rearrange("p (h b) -> p h b", b=3)
                V("tensor_tensor", [b_sm, b_gl], [b_sm], out=sm[:, 6:12], in0=sm[:, 0:6], in1=glv[:, :, 0], op=ALU.mult)
                for g in range(2):
                    po3 = pOc[g][:, 0:387].rearrange("p (r c) -> p r c", r=3)
                    V("tensor_tensor", [b_pOc[g], b_sm], [b_imp], out=imp[:, g * 3:g * 3 + 3, :], in0=po3[:, :, 65:129],
                      in1=sm[:, g * 3:g * 3 + 3].unsqueeze(2).to_broadcast([128, 3, 64]), op=ALU.mult)
                    V("tensor_tensor", [b_pOc[g], b_sm], [b_oc], out=oc[:, g * 3:g * 3 + 3, :], in0=po3[:, :, 0:64],
                      in1=sm[:, 6 + g * 3:9 + g * 3].unsqueeze(2).to_broadcast([128, 3, 64]), op=ALU.mult)
                yield
                for g in range(2):
                    V("tensor_tensor", [b_imp], [b_sc], out=sc[:, g, :], in0=imp[:, g * 3, :], in1=imp[:, g * 3 + 1, :], op=ALU.add)
                    V("tensor_tensor", [b_imp, b_sc], [b_sc], out=sc[:, g, :], in0=sc[:, g, :], in1=imp[:, g * 3 + 2, :], op=ALU.add)
                    V("tensor_tensor", [b_sc, B["rel"]], [b_sc], out=sc[:, g, :], in0=sc[:, g, :], in1=C["rel"][:, 63 - 2 * i:127 - 2 * i], op=ALU.add)
                    V("tensor_tensor", [b_sc, B["col0"]], [b_sc], out=sc[:, g, :], in0=sc[:, g, :], in1=C["col0"][:], op=ALU.add)
                    V("max", [b_sc], [b_m8], out=m8[:, g, :], in_=sc[:, g, :])
                    V("match_replace", [b_sc, b_m8], [b_sc], out=sc2[:, g, :], in_to_replace=m8[:, g, :], in_values=sc[:, g, :], imm_value=-3.0e38)
                    V("max", [b_sc], [b_m8], out=m8[:, g, :], in_=sc2[:, g, :])
                    V("tensor_scalar", [b_sc, b_m8], [b_bm], out=bm[:, g, :], in0=sc[:, g, :], scalar1=m8[:, g, 7:8], scalar2=None, op0=ALU.is_ge)
                yield
                stO = [True]

                def sel_front(it):
                    j2, g, k = it
                    eh = k % 4
                    Eh = E[eh // 2][:, (eh % 2) * 384:(eh % 2 + 1) * 384]
                    MM(pS[g][:, 0:384].rearrange("p (r t) -> p r t", r=3), kselT[64 * g:64 * g + 64, j2 * 128:(j2 + 1) * 128],
                       qropT[64 * g:64 * g + 64, :, ts], True, KVR + [b_q], [b_pS[g]])
                    if j2 == i:
                        MM(pS[g][:, 0:384], C["ident_b"][:], C["negtri_c"][:], False, [B["ident_b"], B["negtri_c"]], [b_pS[g]])
                    A(Eh, pS[g][:, 0:384], AF.Exp, [b_pS[g]], [b_Eh[eh]], scale=0.125)
                    V("tensor_copy", [b_bm], [b_bx[g]], out=bx[:, g, :].rearrange("p (a b) -> p a b", a=2),
                      in_=bm[:, g, 2 * j2:2 * j2 + 2].unsqueeze(2).to_broadcast([128, 2, 64]))
                    TR(pM[:, g * 128:(g + 1) * 128], bx[:, g, :], C["ident_b"][:], [b_bx[g], B["ident_b"]], [b_pM[0]])

                def sel_back(it):
                    j2, g, k = it
                    eh = k % 4
                    Eh = E[eh // 2][:, (eh % 2) * 384:(eh % 2 + 1) * 384]
                    Ph = Pm[eh // 2][:, (eh % 2) * 384:(eh % 2 + 1) * 384]
                    V("tensor_tensor", [b_Eh[eh], b_pM[0]], [b_Ph[eh]], out=Ph.rearrange("p (r t) -> p r t", r=3),
                      in0=Eh.rearrange("p (r t) -> p r t", r=3),
                      in1=pM[:, g * 128:(g + 1) * 128].unsqueeze(1).to_broadcast([128, 3, 128]), op=ALU.mult)
                    for r in range(3):
                        h = g * 3 + r
                        MM(pO[2][:, h * 65:(h + 1) * 65], Ph[:, r * 128:(r + 1) * 128], vsel[:, j2, g, :], stO[0],
                           [b_Ph[eh]] + KVR, [b_pO[2]])
                        stO[0] = False

                items = [(j2, g, 2 * j2 + g) for j2 in range(i + 1) for g in range(2)]
                prev = None
                for it in items:
                    sel_front(it)
                    if prev is not None:
                        sel_back(prev)
                        yield
                    prev = it
                sel_back(prev)
                yield
                po6 = pO[2][:, 0:390].rearrange("p (h c) -> p h c", h=6)
                V("tensor_scalar", [b_pO[2]], [b_sm], out=sm[:, 12:18], in0=po6[:, :, 64], scalar1=1e-30, scalar2=None, op0=ALU.max)
                V("reciprocal", [b_sm], [b_sm], out=sm[:, 12:18], in_=sm[:, 12:18])
                V("tensor_tensor", [b_sm, b_gl], [b_sm], out=sm[:, 12:18], in0=sm[:, 12:18], in1=glv[:, :, 1], op=ALU.mult)
                V("tensor_tensor", [b_pO[2], b_sm], [b_imp], out=imp[:], in0=po6[:, :, 0:64],
                  in1=sm[:, 12:18].unsqueeze(2).to_broadcast([128, 6, 64]), op=ALU.mult)
                V("tensor_tensor", [b_imp, b_oc], [b_oc], out=oc[:], in0=oc[:], in1=imp[:], op=ALU.add)
                yield
                stW = [True]

                def win_front(it):
                    j2, g, k = it
                    eh = k % 4
                    Eh = E[eh // 2][:, (eh % 2) * 384:(eh % 2 + 1) * 384]
                    KWR = [b_kw[(j2 // 4) % 2], b_kv0]
                    wc = slice((j2 % 8) * 128, (j2 % 8 + 1) * 128)
                    MM(pS[g][:, 0:384].rearrange("p (r t) -> p r t", r=3), kwinT[64 * g:64 * g + 64, wc],
                       qropT[64 * g:64 * g + 64, :, ts], True, KWR + [b_q], [b_pS[g]])
                    if j2 == i:
                        MM(pS[g][:, 0:384], C["ident_b"][:], C["negtri_c"][:], False, [B["ident_b"], B["negtri_c"]], [b_pS[g]])
                    if j2 == i - 4:
                        MM(pS[g][:, 0:384], C["ident_b"][:], C["negtri_w"][:], False, [B["ident_b"], B["negtri_w"]], [b_pS[g]])
                    A(Eh, pS[g][:, 0:384], AF.Exp, [b_pS[g]], [b_Eh[eh]], scale=0.125)

                def win_back(it):
                    j2, g, k = it
                    eh = k % 4
                    Eh = E[eh // 2][:, (eh % 2) * 384:(eh % 2 + 1) * 384]
                    KWR = [b_kw[(j2 // 4) % 2], b_kv0]
                    for r in range(3):
                        h = g * 3 + r
                        MM(pO[0][:, h * 65:(h + 1) * 65], Eh[:, r * 128:(r + 1) * 128], vwin[:, j2 % 8, g, :], stW[0],
                           [b_Eh[eh]] + KWR, [b_pO[0]])
                        stW[0] = False

                items = [(j2, g, 2 * jj + g) for jj, j2 in enumerate(range(max(0, i - 4), i + 1)) for g in range(2)]
                prev = None
                for it in items:
                    win_front(it)
                    if prev is not None:
                        win_back(prev)
                        yield
                    prev = it
                win_back(prev)
                yield
                po6 = pO[0][:, 0:390].rearrange("p (h c) -> p h c", h=6)
                V("tensor_scalar", [b_pO[0]], [b_sm], out=sm[:, 18:24], in0=po6[:, :, 64], scalar1=1e-30, scalar2=None, op0=ALU.max)
                V("reciprocal", [b_sm], [b_sm], out=sm[:, 18:24], in_=sm[:, 18:24])
                V("tensor_tensor", [b_sm, b_gl], [b_sm], out=sm[:, 18:24], in0=sm[:, 18:24], in1=glv[:, :, 2], op=ALU.mult)
                V("tensor_tensor", [b_pO[0], b_sm], [b_imp], out=imp[:], in0=po6[:, :, 0:64],
                  in1=sm[:, 18:24].unsqueeze(2).to_broadcast([128, 6, 64]), op=ALU.mult)
                V("tensor_tensor", [b_imp, b_oc], [b_catc], out=catc[:, 0:384].rearrange("p (h d) -> p h d", h=6), in0=oc[:], in1=imp[:], op=ALU.add)
                G("tensor_copy", [b_ocs], [b_catc], out=catc[:, 768:1024], in_=ocs[:, tl, :])
                yield

            def dsa_a(i, tl):
                ts = slice(tl * 128, (tl + 1) * 128)
                KVR = [b_kv[x] for x in range(gi + 1)] + [b_kv0]
                nk = 128 * (i + 1)
                V("tensor_single_scalar", [b_gl], [b_wsg], out=wsg[:, 8:12], in_=gl[:, tl, 18:22], scalar=-1.0, op=ALU.mult)
                V("tensor_tensor", [b_gl, b_wsg], [b_wsg], out=wsg[:, 0:4], in0=gl[:, tl, 18:22], in1=wsg[:, 8:12], op=ALU.max)
                V("tensor_single_scalar", [b_gl], [b_wsg], out=wsg[:, 4:8], in_=gl[:, tl, 18:22], scalar=0.0, op=ALU.is_gt)
                V("tensor_single_scalar", [b_gl], [b_wsg], out=wsg[:, 8:12], in_=gl[:, tl, 18:22], scalar=0.0, op=ALU.is_lt)
                V("tensor_tensor", [b_wsg], [b_wsg], out=wsg[:, 4:8], in0=wsg[:, 4:8], in1=wsg[:, 8:12], op=ALU.subtract)
                for c0 in range(0, nk, 512):
                    cw = min(512, nk - c0)
                    for h in range(4):
                        pp, bpp = (pA, b_pA) if h % 2 == 0 else (pB, b_pB)
                        ri = h % 2
                        MM(pp[:, 0:cw], iqT[32 * (h % 2):32 * (h % 2) + 32, h // 2, ts], ikT[32 * (h % 2):32 * (h % 2) + 32, c0:c0 + cw], True, [b_q] + KVR, [bpp])
                        A(rl[ri][:, 0:cw], pp[:, 0:cw], AF.Relu, [bpp, b_wsg], [b_rl[ri]], scale=wsg[:, h:h + 1])
                        if h == 0:
                            V("tensor_scalar", [b_rl[ri], b_wsg], [b_acc], out=acc[:, c0:c0 + cw], in0=rl[ri][:, 0:cw], scalar1=wsg[:, 4:5], scalar2=None, op0=ALU.mult)
                        else:
                            V("scalar_tensor_tensor", [b_rl[ri], b_wsg, b_acc], [b_acc], out=acc[:, c0:c0 + cw], in0=rl[ri][:, 0:cw],
                              scalar=wsg[:, 4 + h:5 + h], in1=acc[:, c0:c0 + cw], op0=ALU.mult, op1=ALU.add)
                    yield
                CH = 1024
                cs = list(range(0, nk, CH))
                if nk > self.KTOP:
                    for ci_, c0 in enumerate(cs):
                        cw = min(CH, nk - c0)
                        V("tensor_reduce", [b_acc], [b_bs2], out=bst2[:, ci_:ci_ + 1], in_=acc[:, c0:c0 + cw], axis=AX.X, op=ALU.max)
                        V("tensor_reduce", [b_acc], [b_bs2], out=bst2[:, 8 + ci_:9 + ci_], in_=acc[:, c0:c0 + cw], axis=AX.X, op=ALU.min)
                        yield
                    V("tensor_reduce", [b_bs2], [b_bst], out=bst[:, 1:2], in_=bst2[:, 0:len(cs)], axis=AX.X, op=ALU.max)
                    V("tensor_reduce", [b_bs2], [b_bst], out=bst[:, 0:1], in_=bst2[:, 8:8 + len(cs)], axis=AX.X, op=ALU.min)
                G("affine_select", [b_acc], [b_acc], out=acc[:, 128 * i:128 * i + 128], in_=acc[:, 128 * i:128 * i + 128], pattern=[[-1, 128]],
                  compare_op=ALU.is_ge, fill=FILL, base=0, channel_multiplier=1)
                yield
                if nk > self.KTOP:
                    V("tensor_scalar", [b_bst], [b_bst], out=bst[:, 0:1], in0=bst[:, 0:1], scalar1=-1.0, scalar2=None, op0=ALU.add)
                    V("tensor_tensor", [b_bst], [b_bst], out=bst[:, 2:3], in0=bst[:, 1:2], in1=bst[:, 0:1], op=ALU.subtract)
                    V("tensor_scalar", [b_bst, B["pow2"]], [b_bst], out=bst[:, 8:8 + NBIS], in0=C["pow2"][:], scalar1=bst[:, 2:3], scalar2=None, op0=ALU.mult)
                    for kb in range(NBIS):
                        V("tensor_tensor", [b_bst], [b_bst], out=bst[:, 3:4], in0=bst[:, 0:1], in1=bst[:, 8 + kb:9 + kb], op=ALU.add)
                        for ci_, c0 in enumerate(cs):
                            cw = min(CH, nk - c0)
                            V("tensor_scalar", [b_acc, b_bst], [b_junk, b_bst], out=junk[:, c0:c0 + cw], in0=acc[:, c0:c0 + cw], scalar1=bst[:, 3:4],
                              scalar2=(None if ci_ == 0 else bst[:, 4:5]), op0=ALU.is_gt, op1=ALU.add, accum_out=bst[:, 4:5])
                            if ci_ < len(cs) - 1:
                                yield
                        V("tensor_scalar", [b_bst], [b_bst], out=bst[:, 5:6], in0=bst[:, 4:5], scalar1=self.KTOP - 0.5, scalar2=bst[:, 8 + kb:9 + kb],
                          op0=ALU.is_gt, op1=ALU.mult)
                        V("tensor_tensor", [b_bst], [b_bst], out=bst[:, 0:1], in0=bst[:, 0:1], in1=bst[:, 5:6], op=ALU.add)
                        yield

            def dsa_b(i, tl, ci):
                ts = slice(tl * 128, (tl + 1) * 128)
                KVR = [b_kv[x] for x in range(gi + 1)] + [b_kv0]
                nk = 128 * (i + 1)
                sm = sm_d; b_sm = b_smd
                catc, b_catc = cat[ci], b_cat[ci]
                if nk > self.KTOP:
                    for c0 in range(0, nk, 1024):
                        cw = min(1024, nk - c0)
                        V("tensor_scalar", [b_acc, b_bst], [b_dm], out=dmask[:, c0:c0 + cw], in0=acc[:, c0:c0 + cw], scalar1=bst[:, 0:1], scalar2=None, op0=ALU.is_gt)
                        if c0 + 1024 < nk:
                            yield
                else:
                    V("tensor_single_scalar", [b_acc], [b_dm], out=dmask[:, 0:nk], in_=acc[:, 0:nk], scalar=-1.0e29, op=ALU.is_gt)
                yield
                stD = [True]
                pSd = [pA, pB]; b_pSd = [b_pA, b_pB]

                def dsa_front(it):
                    j2, hb, k = it
                    eh = k % 4
                    Eh = Ed[eh // 2][:, (eh % 2) * 384:(eh % 2 + 1) * 384]
                    po_ = 64 * hb
                    MM(pSd[hb][:, 0:384].rearrange("p (r t) -> p r t", r=3), kdsT[po_:po_ + 64, j2 * 128:(j2 + 1) * 128],
                       dqT[po_:po_ + 64, :, ts], True, KVR + [b_q], [b_pSd[hb]])
                    A(Eh, pSd[hb][:, 0:384], AF.Exp, [b_pSd[hb]], [b_Edh[eh]], scale=0.125)
                    if hb == 0:
                        mo = 512 + 128 * (j2 % 2)
                        TR(pM[:, mo:mo + 128], dmask[:, j2 * 128:(j2 + 1) * 128], C["ident_b"][:], [b_dm, B["ident_b"]], [b_pM[1]])

                def dsa_back(it):
                    j2, hb, k = it
                    eh = k % 4
                    Eh = Ed[eh // 2][:, (eh % 2) * 384:(eh % 2 + 1) * 384]
                    Ph = Pd[0][:, (k % 2) * 384:(k % 2 + 1) * 384]
                    mo = 512 + 128 * (j2 % 2)
                    V("tensor_tensor", [b_Edh[eh], b_pM[1]], [b_Pdh[k % 2]], out=Ph.rearrange("p (r t) -> p r t", r=3),
                      in0=Eh.rearrange("p (r t) -> p r t", r=3),
                      in1=pM[:, mo:mo + 128].unsqueeze(1).to_broadcast([128, 3, 128]), op=ALU.mult)
                    for r in range(3):
                        hh = 2 * r + hb
                        MM(pO[1][:, hh * 65:(hh + 1) * 65], Ph[:, r * 128:(r + 1) * 128], vds[:, j2, :], stD[0], [b_Pdh[k % 2]] + KVR, [b_pO[1]])
                        stD[0] = False

                items = [(j2, hb, 2 * j2 + hb) for j2 in range(i + 1) for hb in range(2)]
                prev = None
                for it in items:
                    dsa_front(it)
                    if prev is not None:
                        dsa_back(prev)
                        yield
                    prev = it
                dsa_back(prev)
                yield
                po6 = pO[1][:, 0:390].rearrange("p (h c) -> p h c", h=6)
                V("tensor_scalar", [b_pO[1]], [b_sm], out=sm[:, 24:30], in0=po6[:, :, 64], scalar1=1e-30, scalar2=None, op0=ALU.max)
                V("reciprocal", [b_sm], [b_sm], out=sm[:, 24:30], in_=sm[:, 24:30])
                V("tensor_tensor", [b_pO[1], b_sm], [b_catc], out=catc[:, 384:768].rearrange("p (h d) -> p h d", h=6), in0=po6[:, :, 0:64],
                  in1=sm[:, 24:30].unsqueeze(2).to_broadcast([128, 6, 64]), op=ALU.mult)
                yield

            def out_chain(i, ci):
                catc, b_catc = cat[ci], b_cat[ci]
                xrc, b_xrc = xr[ci], b_xr[ci]
                if self.debug and l == 0:
                    self.S.dma(self.S.pool, self.dbg[i * 128:(i + 1) * 128, :], catc[:], reads=[b_catc], writes=[Buf()], is_output=True)
                LD(xrc[:], xin[i * 128:(i + 1) * 128, :], [b_xrc], R=[bxin[i]])
                yield
                pCT = pS[0][:, :].bitcast(BF16)
                for k in range(8):
                    TR(pCT[:, k * 128:(k + 1) * 128], catc[:, k * 128:(k + 1) * 128], C["ident_b"][:], [b_catc, B["ident_b"]], [b_pS[0]])
                A(catT[:].rearrange("p k t -> p (k t)"), pCT, AF.Copy, [b_pS[0]], [b_catT])
                yield
                for hf in range(2):
                    pp, bpp = (pA, b_pA) if hf == 0 else (pB, b_pB)
                    for k in range(8):
                        MM(pp[:, :], catT[:, k, :], wout[:, k, hf * 512:(hf + 1) * 512], k == 0, [b_catT, b_wout], [bpp])
                    V("scalar_tensor_tensor", [bpp, b_xrc], [b_xrc], out=xrc[:, hf * 512:(hf + 1) * 512], in0=xrc[:, hf * 512:(hf + 1) * 512],
                      scalar=ALPHA, in1=pp[:, :], op0=ALU.mult, op1=ALU.add)
                    yield
                self.ln_stats(xrc, b_xrc, mv2, b_mv2, mv2[:, 2:3], stats2, b_st2)
                V("tensor_scalar", [b_xrc, b_mv2], [b_xrc], out=xrc[:], in0=xrc[:], scalar1=mv2[:, 0:1], scalar2=mv2[:, 2:3], op0=ALU.subtract, op1=ALU.mult)
                yield
                G("tensor_tensor", [b_xrc, b_ln], [b_xrc], out=xrc[:], in0=xrc[:], in1=lng[:], op=ALU.mult)
                G("tensor_tensor", [b_xrc, b_ln], [b_xrc], out=xrc[:], in0=xrc[:], in1=lnb[:], op=ALU.add)
                LD(xout[i * 128:(i + 1) * 128, :], xrc[:], [bxout[i]], R=[b_xrc])
                yield

            def run_interleaved(gens):
                gens = [g_ for g_ in gens if g_ is not None]
                while gens:
                    for g_ in list(gens):
                        try:
                            next(g_)
                        except StopIteration:
                            gens.remove(g_)

            i0 = gi * 4
            run_interleaved([dsa_a(i0, 0), nsa_chain(i0, 0, i0 % 2)])
            pend = None
            for tl in range(4):
                i = gi * 4 + tl
                ci = i % 2
                gens = [dsa_b(i, tl, ci)]
                if tl < 3:
                    gens += [dsa_a(i + 1, tl + 1), nsa_chain(i + 1, tl + 1, (i + 1) % 2)]
                gens.append(pend)
                run_interleaved(gens)
                pend = out_chain(i, ci)
            run_interleaved([pend])
        S.barrier()

    def ffn_common(self, st, l):
        C, B, I = self.C, self.B, self.I
        sb = lambda n, s, d: self.sb(st, n, s, d)
        o = {}
        o["pA"] = self.ps(st, "fA", [128, 512], F32); o["bpA"] = PBuf()
        o["pB"] = self.ps(st, "fB", [128, 512], F32); o["bpB"] = PBuf()
        o["pT"] = [self.ps(st, f"fT{i}", [128, 512], F32) for i in range(2)]; o["bpT"] = [PBuf(), PBuf()]
        o["pD"] = [self.ps(st, f"fD{i}", [128, 512], F32) for i in range(2)]; o["bpD"] = [PBuf(), PBuf()]
        o["pR"] = self.ps(st, "fR", [128, 512], F32); o["bpR"] = PBuf()
        gbc = sb("gbc2", [128, D], F32); b_gbc = Buf()
        self.LD(gbc[:], self.gate_d[2 * l + 1:2 * l + 2, :].to_broadcast([128, D]), [b_gbc], R=[self.b_gate])
        o["gbc"], o["b_gbc"] = gbc, b_gbc
        lng = sb("lng2", [128, D], F32); lnb = sb("lnb2", [128, D], F32); b_ln = Buf()
        self.LD(lng[:], I["ln2_g"][l:l + 1, :].to_broadcast([128, D]), [b_ln])
        self.LD(lnb[:], I["ln2_b"][l:l + 1, :].to_broadcast([128, D]), [b_ln])
        o["lng"], o["lnb"], o["b_ln"] = lng, lnb, b_ln
        o["stats"] = sb("fstats", [128, 2, 6], F32); o["b_st"] = Buf()
        o["mv"] = sb("fmv", [128, 4], F32); o["b_mv"] = Buf()
        o["xn"] = sb("fxn", [128, D], F32); o["b_xn"] = Buf()
        o["ybuf"] = [sb("fy0", [128, D], F32)] * 2; _by = Buf(); o["b_y"] = [_by, _by]
        return o

    def ffn_ln_mod(self, o, l, xt, b_x, hT, b_hT, tl, hT32=None, b_hT32=None):
        C, B = self.C, self.B
        self.ln_stats(xt, b_x, o["mv"], o["b_mv"], o["mv"][:, 2:3], o["stats"], o["b_st"])
        self.V("tensor_scalar", [b_x, o["b_mv"]], [o["b_xn"]], out=o["xn"][:], in0=xt[:], scalar1=o["mv"][:, 0:1], scalar2=o["mv"][:, 2:3],
               op0=ALU.subtract, op1=ALU.mult)
        for hf in range(2):
            pp, bpp = o["pT"][hf], o["bpT"][hf]
            for k4 in range(4):
                self.TR(pp[:, k4 * 128:(k4 + 1) * 128], o["xn"][:, (hf * 4 + k4) * 128:(hf * 4 + k4 + 1) * 128], C["ident_f"][:],
                        [o["b_xn"], B["ident_f"]], [bpp])
            for k4 in range(4):
                k = hf * 4 + k4
                self.A(hT[:, k, tl * 128:(tl + 1) * 128], pp[:, k4 * 128:(k4 + 1) * 128], AF.Identity, [bpp, self.b_mod], [b_hT],
                       bias=self.modcol[:, l, 2, k:k + 1], scale=self.modcol[:, l, 3, k:k + 1])
                if hT32 is not None:
                    self.V("tensor_scalar", [bpp, self.b_mod], [b_hT32], out=hT32[:, k, :], in0=pp[:, k4 * 128:(k4 + 1) * 128],
                           scalar1=self.modcol[:, l, 3, k:k + 1], scalar2=self.modcol[:, l, 2, k:k + 1], op0=ALU.mult, op1=ALU.add)

    def ffn_finish_tile(self, o, yb, b_yb, dst_ap, b_dst, is_out):
        self.ln_stats(yb, b_yb, o["mv"], o["b_mv"], o["mv"][:, 2:3], o["stats"], o["b_st"])
        self.V("tensor_scalar", [b_yb, o["b_mv"]], [b_yb], out=yb[:], in0=yb[:], scalar1=o["mv"][:, 0:1], scalar2=o["mv"][:, 2:3],
               op0=ALU.subtract, op1=ALU.mult)
        self.G("tensor_tensor", [b_yb, o["b_ln"]], [b_yb], out=yb[:], in0=yb[:], in1=o["lng"][:], op=ALU.mult)
        self.G("tensor_tensor", [b_yb, o["b_ln"]], [b_yb], out=yb[:], in0=yb[:], in1=o["lnb"][:], op=ALU.add)
        self.S.dma(self.S.sp, dst_ap, yb[:], reads=[b_yb], writes=[b_dst], is_output=is_out)

    def ffn_dense(self, st, l, xin, bxin, xout, bxout, is_out):
        S, C, B, I = self.S, self.C, self.B, self.I
        sb = lambda n, s, d: self.sb(st, n, s, d)
        MM, A, V, G, LD = self.MM, self.A, self.V, self.G, self.LD
        j = l // 2
        NF = D_FF // 128
        o = self.ffn_common(st, l)
        wg = sb("wg", [128, 8, D_FF], BF16); wu = sb("wu", [128, 8, D_FF], BF16); wd = sb("wd", [128, NF, D], BF16)
        b_wg, b_wu, b_wd = Buf(), Buf(), Buf()
        for (a, b) in ((0, 1408), (1408, D_FF)):
            LD(wg[:, :, a:b], I["ffn_wg"][j, :, a:b].rearrange("(k p) n -> p k n", p=128), [b_wg], q=S.pool)
            LD(wu[:, :, a:b], I["ffn_wu"][j, :, a:b].rearrange("(k p) n -> p k n", p=128), [b_wu], q=S.pool)
        for (a, b) in ((0, 11), (11, NF)):
            LD(wd[:, a:b, :], I["ffn_wd"][j, a * 128:b * 128, :].rearrange("(k p) n -> p k n", p=128), [b_wd], q=S.pool)
        for k in range(NF):
            V("tensor_tensor", [b_wd, o["b_gbc"]], [b_wd], out=wd[:, k, :], in0=wd[:, k, :], in1=o["gbc"][:], op=ALU.mult)
        xt = [sb(f"fx{i}", [128, D], F32) for i in range(4)]; b_xt = [Buf() for _ in range(4)]
        hT = sb("fhT", [128, 8, 512], BF16); b_hT = Buf()
        hid = sb("hid", [128, NF, 512], BF16); b_hid = Buf()
        sg = [sb("fsg0", [128, 512], F32)] * 2; _bs = Buf(); b_sg = [_bs, _bs]
        ny = 0
        for gi in range(self.NG):
            for tl in range(4):
                t = gi * 4 + tl
                LD(xt[tl][:], xin[t * 128:(t + 1) * 128, :], [b_xt[tl]], R=[bxin[t]])
                self.ffn_ln_mod(o, l, xt[tl], b_xt[tl], hT, b_hT, tl)
            for fc in range(NF):
                for k in range(8):
                    MM(o["pA"][:, :], wg[:, k, fc * 128:(fc + 1) * 128], hT[:, k, :], k == 0, [b_wg, b_hT], [o["bpA"]])
                for k in range(8):
                    MM(o["pB"][:, :], wu[:, k, fc * 128:(fc + 1) * 128], hT[:, k, :], k == 0, [b_wu, b_hT], [o["bpB"]])
                si = fc % 2
                A(sg[si][:], o["pA"][:, :], AF.Silu, [o["bpA"]], [b_sg[si]])
                V("tensor_tensor", [b_sg[si], o["bpB"]], [b_hid], out=hid[:, fc, :], in0=sg[si][:], in1=o["pB"][:, :], op=ALU.mult)
            for tl in range(4):
                t = gi * 4 + tl
                yb, byb = o["ybuf"][ny % 2], o["b_y"][ny % 2]; ny += 1
                for hf in range(2):
                    pd, bpd = o["pD"][hf], o["bpD"][hf]
                    for fc in range(NF):
                        MM(pd[:, :], hid[:, fc, tl * 128:(tl + 1) * 128], wd[:, fc, hf * 512:(hf + 1) * 512], fc == 0, [b_hid, b_wd], [bpd])
                    V("scalar_tensor_tensor", [bpd, b_xt[tl]], [byb], out=yb[:, hf * 512:(hf + 1) * 512], in0=xt[tl][:, hf * 512:(hf + 1) * 512],
                      scalar=ALPHA, in1=pd[:, :], op0=ALU.mult, op1=ALU.add)
                self.ffn_finish_tile(o, yb, byb, xout[t * 128:(t + 1) * 128, :], bxout[t], is_out)
        S.barrier()

    def ffn_moe(self, st, l, xin, bxin, xout, bxout, is_out):
        S, C, B, I = self.S, self.C, self.B, self.I
        sb = lambda n, s, d: self.sb(st, n, s, d)
        MM, A, V, G, LD = self.MM, self.A, self.V, self.G, self.LD
        j = l // 2
        o = self.ffn_common(st, l)
        SGT = min(self.NT, 16)
        NSG = self.NT // SGT
        wr = sb("wr", [128, 8, NEXP], F32); b_wr = Buf()
        LD(wr[:], I["moe_wr"][j].rearrange("(k p) n -> p k n", p=128), [b_wr])
        brb = sb("brb", [128, NEXP], F32)
        LD(brb[:], I["moe_br"][j:j + 1, :].to_broadcast([128, NEXP]), [b_wr])
        hTa = sb("hTa", [128, 8, SGT * 128], BF16); b_hTa = [Buf() for _ in range(SGT // 4)]
        hT32 = sb("hT32", [128, 8, 128], F32); b_hT32 = Buf()
        accs = sb("accs", [128, SGT, D], F32); b_acc = [Buf() for _ in range(SGT)]
        gates = sb("gates", [128, SGT, NEXP], F32); b_gt = Buf()
        lg = sb("lg", [128, 40], F32); b_lg = Buf()
        xt = [sb(f"mx{i}", [128, D], F32) for i in range(2)]; b_xt = [Buf(), Buf()]
        wgc = [sb(f"wgc{i}", [128, 8, 512], BF16) for i in range(2)]
        wuc = [sb(f"wuc{i}", [128, 8, 512], BF16) for i in range(2)]
        wdc = [sb(f"wdc{i}", [128, 4, D], BF16) for i in range(2)]
        b_wc = [Buf(), Buf()]
        hid = sb("mhid", [128, 4, 512], BF16); b_hid = Buf()
        sg = [sb(f"msg{i}", [128, 512], F32) for i in range(2)]; b_sg = [Buf(), Buf()]
        nx = 0
        nw = 0
        for sgi in range(NSG):
            for tt in range(SGT):
                t = sgi * SGT + tt
                xi = nx % 2; nx += 1
                LD(xt[xi][:], xin[t * 128:(t + 1) * 128, :], [b_xt[xi]], R=[bxin[t]])
                self.ffn_ln_mod(o, l, xt[xi], b_xt[xi], hTa[:, :, (tt // 4) * 512:(tt // 4 + 1) * 512], b_hTa[tt // 4], tt % 4, hT32, b_hT32)
                for k in range(8):
                    MM(o["pR"][:, 0:NEXP], hT32[:, k, :], wr[:, k, :], k == 0, [b_hT32, b_wr], [o["bpR"]])
                V("tensor_tensor", [o["bpR"], b_wr], [b_lg], out=lg[:, 0:8], in0=o["pR"][:, 0:NEXP], in1=brb[:], op=ALU.add)
                V("max", [b_lg], [b_lg], out=lg[:, 8:16], in_=lg[:, 0:8])
                V("tensor_tensor", [b_lg], [b_lg], out=lg[:, 16:17], in0=lg[:, 9:10], in1=lg[:, 8:9], op=ALU.subtract)
                A(lg[:, 17:18], lg[:, 16:17], AF.Sigmoid, [b_lg], [b_lg])
                V("tensor_scalar", [b_lg], [b_lg], out=lg[:, 18:19], in0=lg[:, 17:18], scalar1=-1.0, scalar2=1.0, op0=ALU.mult, op1=ALU.add)
                V("tensor_scalar", [b_lg], [b_lg], out=lg[:, 24:32], in0=lg[:, 0:8], scalar1=lg[:, 8:9], scalar2=lg[:, 18:19], op0=ALU.is_equal, op1=ALU.mult)
                V("tensor_scalar", [b_lg], [b_lg], out=lg[:, 32:40], in0=lg[:, 0:8], scalar1=lg[:, 9:10], scalar2=lg[:, 17:18], op0=ALU.is_equal, op1=ALU.mult)
                V("tensor_tensor", [b_lg], [b_gt], out=gates[:, tt, :], in0=lg[:, 24:32], in1=lg[:, 32:40], op=ALU.add)
            import os
            MOES = int(os.environ.get("MOESTOP", "99"))
            for e in range(min(NEXP, MOES)):
                for fc in range(E_FF // 512 if MOES > 1 else 1):
                    wi = nw % 2; nw += 1
                    f0 = fc * 512
                    LD(wgc[wi][:], I["moe_wg"][j, e, :, f0:f0 + 512].rearrange("(k p) n -> p k n", p=128), [b_wc[wi]], q=S.pool)
                    LD(wuc[wi][:], I["moe_wu"][j, e, :, f0:f0 + 512].rearrange("(k p) n -> p k n", p=128), [b_wc[wi]], q=S.pool)
                    LD(wdc[wi][:], I["moe_wd"][j, e, f0:f0 + 512, :].rearrange("(k p) n -> p k n", p=128), [b_wc[wi]], q=S.pool)
                    for k in range(4):
                        G("tensor_tensor", [b_wc[wi], o["b_gbc"]], [b_wc[wi]], out=wdc[wi][:, k, :], in0=wdc[wi][:, k, :], in1=o["gbc"][:], op=ALU.mult)
                    first = (e == 0 and fc == 0)
                    for tg in range(SGT // 4):
                        hTg = hTa[:, :, tg * 512:(tg + 1) * 512]
                        for sc_ in range(4):
                            for k in range(8):
                                MM(o["pA"][:, :], wgc[wi][:, k, sc_ * 128:(sc_ + 1) * 128], hTg[:, k, :], k == 0, [b_wc[wi], b_hTa[tg]], [o["bpA"]])
                            for k in range(8):
                                MM(o["pB"][:, :], wuc[wi][:, k, sc_ * 128:(sc_ + 1) * 128], hTg[:, k, :], k == 0, [b_wc[wi], b_hTa[tg]], [o["bpB"]])
                            si = sc_ % 2
                            A(sg[si][:], o["pA"][:, :], AF.Silu, [o["bpA"]], [b_sg[si]])
                            V("tensor_tensor", [b_sg[si], o["bpB"]], [b_hid], out=hid[:, sc_, :], in0=sg[si][:], in1=o["pB"][:, :], op=ALU.mult)
                        for tl in range(4):
                            tt = tg * 4 + tl
                            for hf in range(2):
                                pd, bpd = o["pD"][hf], o["bpD"][hf]
                                for sc_ in range(4):
                                    MM(pd[:, :], hid[:, sc_, tl * 128:(tl + 1) * 128], wdc[wi][:, sc_, hf * 512:(hf + 1) * 512], sc_ == 0, [b_hid, b_wc[wi]], [bpd])
                                if first:
                                    V("tensor_scalar", [bpd, b_gt], [b_acc[tt]], out=accs[:, tt, hf * 512:(hf + 1) * 512], in0=pd[:, :],
                                      scalar1=gates[:, tt, e:e + 1], scalar2=None, op0=ALU.mult)
                                else:
                                    V("scalar_tensor_tensor", [bpd, b_gt, b_acc[tt]], [b_acc[tt]], out=accs[:, tt, hf * 512:(hf + 1) * 512], in0=pd[:, :],
                                      scalar=gates[:, tt, e:e + 1], in1=accs[:, tt, hf * 512:(hf + 1) * 512], op0=ALU.mult, op1=ALU.add)
            for tt in range(SGT):
                t = sgi * SGT + tt
                xi = nx % 2; nx += 1
                LD(xt[xi][:], xin[t * 128:(t + 1) * 128, :], [b_xt[xi]], R=[bxin[t]])
                V("scalar_tensor_tensor", [b_xt[xi], b_acc[tt]], [b_acc[tt]], out=accs[:, tt, :], in0=xt[xi][:], scalar=ALPHA, in1=accs[:, tt, :],
                  op0=ALU.mult, op1=ALU.add)
                self.ffn_finish_tile(o, accs[:, tt, :], b_acc[tt], xout[t * 128:(t + 1) * 128, :], bxout[t], is_out)
        S.barrier()


_PROG = {}
IN_KEYS = ["w_ada", "b_ada", "w_in", "nsa_cmp_pos", "nsa_cmp_w1", "nsa_cmp_w2", "dsa_kv_norm", "dsa_w_uk", "dsa_w_uv",
           "sgu_norm_g", "sgu_norm_b", "sgu_w", "sgu_b", "w_out", "ln1_g", "ln1_b", "ln2_g", "ln2_b",
           "ffn_w_gate", "ffn_w_up", "ffn_w_down", "moe_w_router", "moe_b_router", "moe_w_gate", "moe_w_up", "moe_w_down"]


def make_in_maps(inputs, ncores):
    shared = {}
    for k in IN_KEYS:
        a = np.ascontiguousarray(np.asarray(inputs[k], dtype=np.float32))
        if k in ("sgu_norm_g", "sgu_norm_b"):
            a = a.reshape(a.shape[0], -1)
        shared[k] = a
    x = np.asarray(inputs["x"], dtype=np.float32)
    c = np.asarray(inputs["c"], dtype=np.float32)
    pos = np.asarray(inputs["positions"], dtype=np.int32)
    maps = []
    for b in range(ncores):
        m = dict(shared)
        m["x"] = np.ascontiguousarray(x[b])
        m["c"] = np.ascontiguousarray(c[b:b + 1])
        m["pos"] = np.ascontiguousarray(pos[b:b + 1])
        maps.append(m)
    return maps


def kernel(**inputs):
    x = np.asarray(inputs["x"])
    Bn, SL, _ = x.shape
    key = (SL,)
    if key not in _PROG:
        _PROG[key] = Prog(SL)
    prog = _PROG[key]
    maps = make_in_maps(inputs, Bn)
    res = run_bass_kernel_spmd(prog.nc, maps, core_ids=list(range(Bn)))
    return np.stack([np.asarray(r["y"], dtype=np.float32) for r in res.results], axis=0)
```
